# Optimizing a Trainium2 kernel written in Bass

```python
import math
import jax, jax.numpy as jnp
from jax import lax
import numpy as np

D_MODEL = 1024
BATCH = 4
SEQ = 8192
DEPTH = 4

GRID_W = 64
CTX_LEN = 256
N_MIXERS = 2
A_HEADS = 4
A_DK = 128
A_DV = 256
N_DIR = 2
CHUNK = 64
D_FF = 2816
EPS = 1e-6
A_PROJ = 2 * A_HEADS * A_DK + A_HEADS * A_DV + D_MODEL + 2 * N_DIR * A_HEADS
N_A = sum(1 for i in range(DEPTH) if i % N_MIXERS == 0)
N_B = DEPTH - N_A

kernel_name = "hybrid_mlstm_shortconv_convglu_prefix_dit"


def rmsnorm(x, g):
    xf = x.astype(jnp.float32)
    y = xf * lax.rsqrt(jnp.mean(xf * xf, axis=-1, keepdims=True) + EPS)
    return (y * g.astype(jnp.float32)).astype(x.dtype)


def modulate(x, g, shift, scale):
    return rmsnorm(x, g) * (1 + scale) + shift


def conv3(x, w, axis):
    pad = [(0, 0)] * x.ndim
    pad[axis] = (1, 1)
    xp = jnp.pad(x, pad)
    n = x.shape[axis]
    sl = lambda s: lax.slice_in_dim(xp, s, s + n, axis=axis)
    return sl(0) * w[0] + sl(1) * w[1] + sl(2) * w[2]


def mlstm_inputs(h, w_in, b_gate):
    bn, t, _ = h.shape
    p = h @ w_in
    o1 = A_HEADS * A_DK
    o2 = 2 * o1
    o3 = o2 + A_HEADS * A_DV
    o4 = o3 + D_MODEL
    heads = lambda a, d: a.reshape(bn, t, A_HEADS, d).transpose(0, 2, 1, 3).astype(jnp.float32)
    q = heads(p[..., :o1], A_DK)
    k = heads(p[..., o1:o2], A_DK) * (A_DK ** -0.5)
    v = heads(p[..., o2:o3], A_DV)
    o = p[..., o3:o4]
    gates = (p[..., o4:] + b_gate).astype(jnp.float32)
    gates = gates.reshape(bn, t, 2, N_DIR, A_HEADS).transpose(2, 3, 0, 4, 1)
    it = gates[0]
    lf = jax.nn.log_sigmoid(gates[1])
    return q, k, v, it, lf, o


def mlstm_chunk_scan(q, k, v, it, lf, state, emit):
    bn, nh, t, _ = q.shape
    nc = t // CHUNK
    chunks = lambda a: jnp.moveaxis(a.reshape(a.shape[:2] + (nc, CHUNK) + a.shape[3:]), 2, 0)
    xs = (chunks(q), chunks(k), chunks(v), chunks(it), chunks(lf))
    mask = jnp.tril(jnp.ones((CHUNK, CHUNK), dtype=bool))

    def body(carry, inp):
        cm, nv, m = carry
        qc, kc, vc, ic, fc = inp
        b = jnp.cumsum(fc, axis=-1)
        g = b[..., -1]
        a = g[..., None] - b + ic
        m_new = jnp.maximum(g + m, jnp.max(a, axis=-1))
        decay = jnp.exp(g + m - m_new)
        wa = jnp.exp(a - m_new[..., None])
        c_new = decay[..., None, None] * cm + jnp.einsum('bhsv,bhsd->bhvd', wa[..., None] * vc, kc)
        n_new = decay[..., None] * nv + jnp.einsum('bhs,bhsd->bhd', wa, kc)
        if not emit:
            return (c_new, n_new, m_new), None
        dmat = jnp.where(mask, b[..., :, None] - b[..., None, :] + ic[..., None, :], -jnp.inf)
        inter = b + m[..., None]
        m_t = jnp.maximum(inter, jnp.max(dmat, axis=-1))
        w = jnp.exp(dmat - m_t[..., None]) * jnp.einsum('bhtd,bhsd->bhts', qc, kc)
        wi = jnp.exp(inter - m_t)
        num = jnp.einsum('bhts,bhsv->bhtv', w, vc) + wi[..., None] * jnp.einsum('bhvd,bhtd->bhtv', cm, qc)
        den = jnp.sum(w, axis=-1) + wi * jnp.einsum('bhd,bhtd->bht', nv, qc)
        h = num / jnp.maximum(jnp.abs(den), jnp.exp(-m_t))[..., None]
        return (c_new, n_new, m_new), h

    state, hs = lax.scan(body, state, xs)
    if not emit:
        return None, state
    return jnp.moveaxis(hs, 0, 2).reshape(bn, nh, t, A_DV), state


def mlstm_readout(h, o, gain, w_out):
    bn, nh, t, dv = h.shape
    hn = h * lax.rsqrt(jnp.mean(h * h, axis=-1, keepdims=True) + EPS)
    hn = hn.transpose(0, 2, 1, 3).reshape(bn, t, nh * dv) * gain.astype(jnp.float32)
    y = hn * jax.nn.sigmoid(o.astype(jnp.float32))
    return y.astype(o.dtype) @ w_out


def mlstm_mixer(h_lat, h_ctx, w_in, b_gate, head_gain, w_out, emit_ctx):
    ql, kl, vl, il, fl, ol = mlstm_inputs(h_lat, w_in, b_gate)
    qc, kc, vc, ic, fc, oc = mlstm_inputs(h_ctx, w_in, b_gate)
    bn = h_lat.shape[0]
    s0 = (jnp.zeros((bn, A_HEADS, A_DV, A_DK), jnp.float32),
          jnp.zeros((bn, A_HEADS, A_DK), jnp.float32),
          jnp.zeros((bn, A_HEADS), jnp.float32))
    rq = lambda a: jnp.flip(a, axis=2)
    rg = lambda a: jnp.flip(a, axis=-1)
    hc_f, s_f = mlstm_chunk_scan(qc, kc, vc, ic[0], fc[0], s0, emit_ctx)
    hl_f, _ = mlstm_chunk_scan(ql, kl, vl, il[0], fl[0], s_f, True)
    hc_b, s_b = mlstm_chunk_scan(rq(qc), rq(kc), rq(vc), rg(ic[1]), rg(fc[1]), s0, emit_ctx)
    hl_b, _ = mlstm_chunk_scan(rq(ql), rq(kl), rq(vl), rg(il[1]), rg(fl[1]), s_b, True)
    y = mlstm_readout(hl_f + rq(hl_b), ol, head_gain, w_out)
    yc = mlstm_readout(hc_f + rq(hc_b), oc, head_gain, w_out) if emit_ctx else None
    return y, yc


def shortconv_mixer(h, w_in, w_conv, w_out, conv_fn):
    bg, cg, xv = jnp.split(h @ w_in, 3, axis=-1)
    return (bg * conv_fn(cg * xv, w_conv)) @ w_out


def conv_glu(h, w_up, w_conv, b_conv, w_down, conv_fn):
    gate, val = jnp.split(h @ w_up, 2, axis=-1)
    gate = conv_fn(gate, w_conv) + b_conv
    return (jax.nn.silu(gate) * val) @ w_down


def setup_inputs(seed: int = 0) -> dict:
    key = jax.random.key(seed)
    ks = jax.random.split(key, 24)
    nrm = lambda k, s, sc: jax.random.normal(k, s, jnp.float32) * sc
    D = D_MODEL
    b_i = nrm(ks[10], (N_A, N_DIR, A_HEADS), 0.1)
    b_f = jnp.linspace(3.0, 6.0, A_HEADS, dtype=jnp.float32) + nrm(ks[11], (N_A, N_DIR, A_HEADS), 0.1)
    a_b_gate = jnp.stack([b_i, b_f], axis=1).reshape(N_A, 2 * N_DIR * A_HEADS)
    return {
        "x": nrm(ks[0], (BATCH, SEQ, D), 1.0),
        "c": nrm(ks[1], (BATCH, D), 1.0),
        "ctx": nrm(ks[2], (BATCH, CTX_LEN, D), 1.0),
        "c_ctx": nrm(ks[3], (D,), 1.0),
        "w_mod": nrm(ks[4], (DEPTH, D, 6 * D), 0.5 * D ** -0.5),
        "b_mod": nrm(ks[5], (DEPTH, 6 * D), 0.02),
        "g_mix": 1.0 + nrm(ks[6], (DEPTH, D), 0.05),
        "g_ffn": 1.0 + nrm(ks[7], (DEPTH, D), 0.05),
        "a_w_in": nrm(ks[8], (N_A, D, A_PROJ), D ** -0.5),
        "a_b_gate": a_b_gate,
        "a_head_gain": 1.0 + nrm(ks[9], (N_A, A_HEADS * A_DV), 0.05),
        "a_w_out": nrm(ks[12], (N_A, A_HEADS * A_DV, D), (A_HEADS * A_DV) ** -0.5),
        "b_w_in": nrm(ks[13], (N_B, D, 3 * D), D ** -0.5),
        "b_w_conv": nrm(ks[14], (N_B, 3, D), 3 ** -0.5),
        "b_w_out": nrm(ks[15], (N_B, D, D), D ** -0.5),
        "f_w_up": nrm(ks[16], (DEPTH, D, 2 * D_FF), D ** -0.5),
        "f_w_conv": nrm(ks[17], (DEPTH, 3, D_FF), 3 ** -0.5),
        "f_b_conv": nrm(ks[18], (DEPTH, D_FF), 0.02),
        "f_w_down": nrm(ks[19], (DEPTH, D_FF, D), D_FF ** -0.5),
        "g_final": 1.0 + nrm(ks[20], (D,), 0.05),
    }


def reference(x, c, ctx, c_ctx, w_mod, b_mod, g_mix, g_ffn, a_w_in, a_b_gate, a_head_gain,
              a_w_out, b_w_in, b_w_conv, b_w_out, f_w_up, f_w_conv, f_b_conv, f_w_down, g_final):
    bn, t, d = x.shape
    rows = t // GRID_W
    lat_row_conv = lambda a, w: conv3(a.reshape(bn, rows, GRID_W, a.shape[-1]), w, 2).reshape(a.shape)
    lat_col_conv = lambda a, w: conv3(a.reshape(bn, rows, GRID_W, a.shape[-1]), w, 1).reshape(a.shape)
    seq_conv = lambda a, w: conv3(a, w, 1)
    rec_layers = [i for i in range(DEPTH) if i % N_MIXERS == 0]
    last_rec = max(rec_layers) if rec_layers else -1

    for i in range(DEPTH):
        j = i // N_MIXERS
        ctx_read = i <= last_rec
        ctx_live = i < last_rec
        mod = jax.nn.silu(c) @ w_mod[i] + b_mod[i]
        sh1, sc1, ga1, sh2, sc2, ga2 = [m[:, None, :] for m in jnp.split(mod, 6, axis=-1)]
        if ctx_read:
            mod_c = jax.nn.silu(c_ctx) @ w_mod[i] + b_mod[i]
            csh1, csc1, cga1, csh2, csc2, cga2 = jnp.split(mod_c, 6, axis=-1)
            hc = modulate(ctx, g_mix[i], csh1, csc1)
        hx = modulate(x, g_mix[i], sh1, sc1)
        if i % N_MIXERS == 0:
            y, yc = mlstm_mixer(hx, hc, a_w_in[j], a_b_gate[j], a_head_gain[j], a_w_out[j], ctx_live)
        else:
            y = shortconv_mixer(hx, b_w_in[j], b_w_conv[j], b_w_out[j], lat_row_conv)
            yc = shortconv_mixer(hc, b_w_in[j], b_w_conv[j], b_w_out[j], seq_conv) if ctx_live else None
        x = x + ga1 * y
        hx = modulate(x, g_ffn[i], sh2, sc2)
        x = x + ga2 * conv_glu(hx, f_w_up[i], f_w_conv[i], f_b_conv[i], f_w_down[i], lat_col_conv)
        if ctx_live:
            ctx = ctx + cga1 * yc
            hc = modulate(ctx, g_ffn[i], csh2, csc2)
            ctx = ctx + cga2 * conv_glu(hc, f_w_up[i], f_w_conv[i], f_b_conv[i], f_w_down[i], seq_conv)

    return rmsnorm(x, g_final)
```

```python
import numpy as np
from contextlib import ExitStack
import ml_dtypes
import concourse.bass as bass
import concourse.mybir as mybir
from concourse.bass_utils import run_bass_kernel_spmd

F32 = mybir.dt.float32
BF16 = mybir.dt.bfloat16
AF = mybir.ActivationFunctionType
ALU = mybir.AluOpType

D = 1024
TFULL = 8192
T = 4096
TC = 256
TA = TC + T
DEPTH = 4
NH = 4
DK = 128
DV = 256
DVA = DV + 1
DFF = 2816
NJ = DFF // 128
GW = 64
CH = 64
NROW = T // GW
APROJ = 3088
EPS = 1e-6
NCH = TA // CH
SAME_ENG_SYNC = True


class Sched:
    ENGS = ("pe", "act", "dve", "pool", "sp")

    def __init__(self, nc, es, n_dma_sems=48, n_bg_sems=8):
        self.nc = nc
        self.eng = {"pe": nc.tensor, "act": nc.scalar, "dve": nc.vector,
                    "pool": nc.gpsimd, "sp": nc.sync}
        self.sems = []
        self.esem = {}
        for e in self.ENGS:
            self.esem[e] = len(self.sems)
            self.sems.append(es.enter_context(nc.semaphore("s_" + e)))
        self.dsem = []
        for i in range(n_dma_sems):
            self.dsem.append(len(self.sems))
            self.sems.append(es.enter_context(nc.semaphore("d%d" % i)))
        self.bsem = []
        for i in range(n_bg_sems):
            self.bsem.append(len(self.sems))
            self.sems.append(es.enter_context(nc.semaphore("b%d" % i)))
        self.brr = 0
        self.btarget = [0] * n_bg_sems
        self.bgev = {}
        self.ops = []
        self.cnt = {e: 0 for e in self.ENGS}
        self.waited = {e: {} for e in self.ENGS}
        self.pending = {e: {} for e in self.ENGS}
        self.rr = 0
        self.target = [0] * n_dma_sems
        self.n_ins = 0
        self.n_wait = 0

    def op(self, eng, meth, *args, r=(), w=(), **kw):
        self.ops.append(dict(eng=eng, meth=meth, args=args, kw=kw, r=tuple(r), w=tuple(w),
                             dma=False, signal=False, ev=None))

    def dma(self, eng, out, in_, r=(), w=(), bg=None):
        self.ops.append(dict(eng=eng, meth="dma_start", args=(), kw=dict(out=out, in_=in_),
                             r=tuple(r), w=tuple(w), dma=True, signal=True, ev=None, inc=16, bg=bg))

    def join_bg(self, group):
        for s, v in self.bgev.pop(group, []):
            for e in self.ENGS:
                if self.pending[e].get(s, 0) < v:
                    self.pending[e][s] = v

    def coll(self, ins, outs, groups, r=(), w=()):
        self.ops.append(dict(eng="pool", meth="collective_compute", args=("AllGather", ALU.bypass),
                             kw=dict(replica_groups=groups, ins=ins, outs=outs),
                             r=tuple(r), w=tuple(w), dma=True, signal=True, ev=None, inc=1))

    def flush(self):
        ops = self.ops
        self.ops = []
        state = {}
        deps = []
        last = {}
        for i, o in enumerate(ops):
            d = {}
            for r in o["r"]:
                st = state.get(r)
                if st is not None and st[0] is not None:
                    d[id(st[0])] = st[0]
            for r in o["w"]:
                st = state.get(r)
                if st is not None:
                    if st[0] is not None:
                        d[id(st[0])] = st[0]
                    for x in st[1].values():
                        d[id(x)] = x
                    for x in st[2]:
                        d[id(x)] = x
            for r in o["r"]:
                st = state.setdefault(r, [None, {}, []])
                if o["dma"]:
                    st[2].append(o)
                else:
                    st[1][o["eng"]] = o
            for r in o["w"]:
                st = state.setdefault(r, [None, {}, []])
                st[0] = o
                st[1] = {}
                st[2] = []
            d.pop(id(o), None)
            dd = []
            for oj in d.values():
                if (not oj["dma"]) and (not o["dma"]) and oj["eng"] == o["eng"] and \
                        (o["eng"] == "pe" or not SAME_ENG_SYNC):
                    continue
                dd.append(oj)
                oj["signal"] = True
            deps.append(dd)
            if not o["dma"]:
                last[o["eng"]] = o
        for o in last.values():
            o["signal"] = True
        nd = len(self.dsem)
        bar = {}
        for i, o in enumerate(ops):
            en = o["eng"]
            E = self.eng[en]
            need = dict(self.pending[en])
            self.pending[en] = {}
            for oj in deps[i]:
                s, v = oj["ev"]
                if need.get(s, 0) < v:
                    need[s] = v
            bgd = o["dma"] and o.get("bg") is not None
            if bgd:
                k = self.brr
                self.brr = (self.brr + 1) % len(self.bsem)
                s = self.bsem[k]
                if self.btarget[k] > 0 and need.get(s, 0) < self.btarget[k]:
                    need[s] = self.btarget[k]
            elif o["dma"]:
                k = self.rr
                self.rr = (self.rr + 1) % nd
                s = self.dsem[k]
                if self.target[k] > 0 and need.get(s, 0) < self.target[k]:
                    need[s] = self.target[k]
            for s, v in need.items():
                if self.waited[en].get(s, 0) < v:
                    E.wait_ge(self.sems[s], v)
                    self.waited[en][s] = v
                    self.n_wait += 1
            ins = getattr(E, o["meth"])(*o["args"], **o["kw"])
            self.n_ins += 1
            if bgd:
                self.btarget[k] += 16
                ins.then_inc(self.sems[self.bsem[k]], 16)
                o["ev"] = (self.bsem[k], self.btarget[k])
                self.bgev.setdefault(o["bg"], []).append(o["ev"])
            elif o["dma"]:
                self.target[k] += o["inc"]
                if o["inc"] == 16:
                    ins.then_inc(self.sems[self.dsem[k]], 16)
                else:
                    ins.then_inc(self.sems[self.dsem[k]])
                o["ev"] = (self.dsem[k], self.target[k])
                bar[self.dsem[k]] = self.target[k]
            elif o["signal"]:
                self.cnt[en] += 1
                ins.then_inc(self.sems[self.esem[en]], 1)
                o["ev"] = (self.esem[en], self.cnt[en])
                bar[self.esem[en]] = self.cnt[en]
        for e in self.ENGS:
            p = self.pending[e]
            for s, v in bar.items():
                if p.get(s, 0) < v:
                    p[s] = v

    def finish(self, eng="sp"):
        E = self.eng[eng]
        for s, v in self.pending[eng].items():
            if self.waited[eng].get(s, 0) < v:
                E.wait_ge(self.sems[s], v)
                self.waited[eng][s] = v


def _par_layout():
    off = {}
    o = 0
    for name, n in (("cc", 16), ("bmod", DEPTH * 48), ("gmix", DEPTH * 8), ("gffn", DEPTH * 8),
                    ("gfin", 8), ("bwc_l", 2 * 3 * 8), ("bwc_c", 2 * 3 * 8), ("fwc_l", DEPTH * 3 * NJ),
                    ("fwc_c", DEPTH * 3 * NJ), ("fbc", DEPTH * NJ), ("bgate", 2 * 16), ("sel", 2)):
        off[name] = (o, n)
        o += n
    return off, o


PAR_OFF, NPAR = _par_layout()
C_ONES, C_TRIF, C_TRIB, C_BLK0, C_BLK1, C_ID, C_MF, C_MB, C_TRL0, C_TRL1, C_ML0, C_ML1, NCONST = \
    0, 128, 256, 384, 512, 640, 768, 832, 896, 1024, 1152, 1216, 1280


def fm(v):
    v = np.asarray(v, np.float32)
    lead = v.shape[:-1]
    k = v.shape[-1] // 128
    a = v.reshape(lead + (k, 128))
    a = np.moveaxis(a, -1, 0)
    return np.ascontiguousarray(a)


def make_consts(odd):
    c = np.zeros((128, NCONST), np.float32)
    c[:, C_ONES:C_ONES + 128] = 1.0
    s = np.arange(128)[:, None]
    t = np.arange(128)[None, :]
    same = (s // 64) == (t // 64)
    c[:, C_TRIF:C_TRIF + 128] = (same & (s <= t))
    c[:, C_TRIB:C_TRIB + 128] = (same & (s >= t))
    c[:, C_BLK0:C_BLK0 + 128] = (s < 64)
    c[:, C_BLK1:C_BLK1 + 128] = (s >= 64)
    c[:, C_ID:C_ID + 128] = (s == t)
    s6 = np.arange(128)[:, None] % 64
    t6 = np.arange(64)[None, :]
    c[:, C_MF:C_MF + 64] = (s6 <= t6)
    c[:, C_MB:C_MB + 64] = (s6 >= t6)
    f0, f1 = (C_TRIB, C_TRIF) if odd else (C_TRIF, C_TRIB)
    c[:, C_TRL0:C_TRL0 + 128] = c[:, f0:f0 + 128]
    c[:, C_TRL1:C_TRL1 + 128] = c[:, f1:f1 + 128]
    m0, m1 = (C_MB, C_MF) if odd else (C_MF, C_MB)
    c[:, C_ML0:C_ML0 + 64] = c[:, m0:m0 + 64]
    c[:, C_ML1:C_ML1 + 64] = c[:, m1:m1 + 64]
    return c


def build(plan, final=True, ncores=8):
    nc = bass.Bass("TRN2", target_bir_lowering=False)

    def dram(name, shape, dtype, kind):
        return nc.dram_tensor(name, list(shape), dtype, kind=kind).ap()

    xT_in = dram("xT", [8, 128, T], F32, "ExternalInput")
    ctxT_in = dram("ctxT", [8, 128, TC], F32, "ExternalInput")
    par_in = dram("par", [128, NPAR], F32, "ExternalInput")
    const_in = dram("consts", [128, NCONST], F32, "ExternalInput")
    gain_in = dram("gain", [128, 2, D], F32, "ExternalInput")
    need_w = set()
    for kind, l in plan:
        need_w.add(("mod", l))
        if kind == "A":
            need_w.add(("a", l // 2))
        elif kind == "B":
            need_w.add(("b", l // 2))
        elif kind == "F":
            need_w.add(("f", l))
    Wi, Wb = {}, {}
    for kind, l in sorted(need_w):
        if kind == "mod":
            specs = [("w_mod", [D, 6 * D])]
        elif kind == "a":
            specs = [("a_w_in", [D, APROJ]), ("a_w_out", [D, D])]
        elif kind == "b":
            specs = [("b_w_in", [D, 3 * D]), ("b_w_out", [D, D])]
        else:
            specs = [("f_w_up", [D, 2 * DFF]), ("f_w_down", [DFF, D])]
        for nm, shp in specs:
            Wi[(nm, l)] = dram("%s_%d" % (nm, l), shp, F32, "ExternalInput")
            Wb[(nm, l)] = dram("%s_%d_b" % (nm, l), shp, BF16, "Internal")
    outT = dram("outT", [8, 128, T], F32, "ExternalOutput")
    ctx_out = dram("ctx_out", [8, 128, TC], F32, "ExternalOutput")
    XA = dram("XA", [8, 128, T], F32, "Internal")
    XB = dram("XB", [8, 128, T], F32, "Internal")
    QK = dram("QK", [8, 128, TA], BF16, "Internal")
    KT = dram("KT", [TA, NH * DK], F32, "Internal")
    VA = dram("VA", [TA, NH * DV], BF16, "Internal")
    SO = dram("SO", [TA, D], F32, "Internal")
    SC = dram("SC", [TA, 16], F32, "Internal")
    HD = [dram("HF", [TA, D], F32, "Internal"), dram("HB", [TA, D], F32, "Internal")]
    NST = NH * DV + NH
    csrc = nc.dram_tensor("csrc", [128, NST], F32)
    cdst = nc.dram_tensor("cdst", [256, NST], F32)
    hsrc = nc.dram_tensor("hsrc", [128, 8 * GW], F32)
    hdst = nc.dram_tensor("hdst", [256, 8 * GW], F32)
    PAIRS = [[2 * i, 2 * i + 1] for i in range(ncores // 2)]

    ges = ExitStack()
    with ges:
        S = Sched(nc, ges)

        uid = [0]

        def sb(es, name, shape, dtype):
            uid[0] += 1
            return es.enter_context(nc.sbuf_tensor("%s_u%d" % (name, uid[0]), list(shape), dtype))

        par = sb(ges, "par", [128, NPAR], F32)
        cst = sb(ges, "cst", [128, NCONST], F32)
        identb = sb(ges, "identb", [128, 128], BF16)
        MODL = sb(ges, "modl", [128, 6, 8, 2], F32)
        ctx = [sb(ges, "ctx%d" % k, [128, TC], F32) for k in range(8)]
        EG = sb(ges, "eg", [128, NCH, 8], F32)
        halo = sb(ges, "halo", [128, 8, GW], F32)
        crecv = sb(ges, "crecv", [128, NST], F32)
        osel, _ = PAR_OFF["sel"]
        sel0 = par[:, osel:osel + 1]
        sel1 = par[:, osel + 1:osel + 2]

        def exchange(src_d, dst_d, n, load_src, out_ap, tag):
            with ExitStack() as es:
                two = sb(es, "xch2", [128, 2, n], F32)
                tmpx = sb(es, "xcht", [128, n], F32)
                load_src(src_d.ap())
                S.coll([src_d.ap().opt()], [dst_d.ap().opt()], PAIRS, r=[tag + "src"], w=[tag + "dst"])
                S.dma("sp", two[:], dst_d.ap().rearrange("(r p) n -> p r n", p=128), r=[tag + "dst"], w=["xch2"])
                S.op("dve", "tensor_scalar", out=tmpx[:], in0=two[:, 0, :], scalar1=sel0, scalar2=None,
                     op0=ALU.mult, r=["xch2", "par"], w=["xcht"])
                S.op("dve", "scalar_tensor_tensor", out=out_ap, in0=two[:, 1, :], scalar=sel1, op0=ALU.mult,
                     in1=tmpx[:], op1=ALU.add, r=["xch2", "xcht", "par"], w=[tag + "recv"])
                S.flush()
        psf = ges.enter_context(nc.psum_tensor("psf", [128, 7, 512], F32))
        psb = ges.enter_context(nc.psum_tensor("psb", [128, 1024], BF16))

        def PS(b):
            return psf[:, b, :]

        def pcol(name, idx=0):
            o, n = PAR_OFF[name]
            return o + idx

        ones_f = cst[:, C_ONES:C_ONES + 128]

        S.dma("sp", par[:], par_in[:, :], w=["par"])
        S.dma("sp", cst[:], const_in[:, :], w=["cst"])
        for k in range(8):
            S.dma("sp", ctx[k][:], ctxT_in[k], w=[("ctx", k)])
        S.op("dve", "tensor_copy", out=identb[:], in_=cst[:, C_ID:C_ID + 128], r=["cst"], w=["identb"])
        def cast_rows(dst, src, nrows, bg):
            for r0 in range(0, nrows, 128):
                S.dma("pool", dst[r0:r0 + 128, :], src[r0:r0 + 128, :], bg=bg)

        wgroups = []
        for kind, l in plan:
            for g in (("mod", l), ({"A": "a", "B": "b", "F": "f", "M": "mod"}[kind], l if kind in ("F", "M") else l // 2)):
                if g not in wgroups:
                    wgroups.append(g)
        wnames = {"mod": ["w_mod"], "a": ["a_w_in", "a_w_out"], "b": ["b_w_in", "b_w_out"], "f": ["f_w_up", "f_w_down"]}
        joined = set()

        def need_weights(g):
            if g not in joined:
                joined.add(g)
                S.join_bg(g)

        for gi, g in enumerate(wgroups[:2]):
            for nm in wnames[g[0]]:
                key = (nm, g[1])
                cast_rows(Wb[key], Wi[key], Wi[key].shape[0], None)
            joined.add(g)
        S.flush()

        bg_done = [False]

        def issue_bg_casts():
            if bg_done[0]:
                return
            bg_done[0] = True
            for g in wgroups[2:]:
                for nm in wnames[g[0]]:
                    key = (nm, g[1])
                    cast_rows(Wb[key], Wi[key], Wi[key].shape[0], g)


        def mk_tmp(es, n):
            return {nm: sb(es, nm, [128, n], F32) for nm in ("sq0", "sq1", "std", "nt0", "nt1")}

        def norm_mod(xs, xregs, n, which, s_gs, s_sh, hx, tmp):
            pieces = [(c0, min(512, n - c0)) for c0 in range(0, n, 512)]
            for k in range(8):
                sq = tmp["sq%d" % (k % 2)]
                S.op("act", "activation", out=sq[:, 0:n], in_=xs[k], func=AF.Square,
                     r=[xregs[k]], w=[("sq", k % 2)])
                for pi, (c0, cn) in enumerate(pieces):
                    S.op("pe", "matmul", PS(6 - pi)[:, 0:cn], lhsT=ones_f, rhs=sq[:, c0:c0 + cn],
                         start=(k == 0), stop=(k == 7), r=[("sq", k % 2), "cst"], w=[("ps", 6 - pi)])
            std = tmp["std"]
            for pi, (c0, cn) in enumerate(pieces):
                S.op("act", "activation", out=std[:, c0:c0 + cn], in_=PS(6 - pi)[:, 0:cn], func=AF.Sqrt,
                     scale=1.0 / D, bias=EPS, r=[("ps", 6 - pi)], w=["std"])
            S.op("dve", "reciprocal", out=std[:, 0:n], in_=std[:, 0:n], r=["std"], w=["std"])
            for k in range(8):
                tt = tmp["nt%d" % (k % 2)]
                S.op("dve", "tensor_tensor", out=tt[:, 0:n], in0=xs[k], in1=std[:, 0:n], op=ALU.mult,
                     r=[xregs[k], "std"], w=[("nt", k % 2)])
                S.op("act", "activation", out=hx[k][:, 0:n], in_=tt[:, 0:n], func=AF.Identity,
                     scale=MODL[:, s_gs, k, which:which + 1], bias=MODL[:, s_sh, k, which:which + 1],
                     r=[("nt", k % 2), "mod"], w=[("hx", k)])

        def resid(out_ap, ps_ap, gcol, x_ap, r, w):
            S.op("dve", "scalar_tensor_tensor", out=out_ap, in0=ps_ap, scalar=gcol, op0=ALU.mult,
                 in1=x_ap, op1=ALU.add, r=r, w=w)

        def v3(ap, rowlen):
            return ap.rearrange("p (a b) -> p a b", b=rowlen)

        def phase_mod(l):
            with ExitStack() as es:
                scb = sb(es, "scb", [128, 8, 2], BF16)
                wm = [sb(es, "wm%d" % i, [128, 8, D], BF16) for i in range(6)]
                t1 = sb(es, "modt1", [128, 8, 2], F32)
                o, _ = PAR_OFF["cc"]
                S.op("act", "activation", out=scb[:].rearrange("p k w -> p (k w)"), in_=par[:, o:o + 16],
                     func=AF.Silu, r=["par"], w=["scb"])
                for s in range(6):
                    S.dma("sp", wm[s][:], Wb[("w_mod", l)][:, s * D:(s + 1) * D].rearrange("(k p) n -> p k n", p=128),
                          w=[("wm", s)])
                for s in range(6):
                    wt = wm[s]
                    pst = PS(s % 2)
                    for m in range(8):
                        for k in range(8):
                            S.op("pe", "matmul", pst[:, m * 2:m * 2 + 2], lhsT=wt[:, k, m * 128:(m + 1) * 128],
                                 rhs=scb[:, k, :], start=(k == 0), stop=(k == 7),
                                 r=[("wm", s), "scb"], w=[("ps", s % 2)])
                    ob, _ = PAR_OFF["bmod"]
                    bcol = par[:, ob + l * 48 + s * 8: ob + l * 48 + s * 8 + 8]
                    S.op("dve", "tensor_tensor", out=MODL[:, s, :, :],
                         in0=pst[:, 0:16].rearrange("p (m w) -> p m w", w=2),
                         in1=bcol.unsqueeze(2).to_broadcast([128, 8, 2]), op=ALU.add,
                         r=[("ps", s % 2), "par"], w=["mod"])
                for s, gname in ((1, "gmix"), (4, "gffn")):
                    og, _ = PAR_OFF[gname]
                    gcol = par[:, og + l * 8: og + l * 8 + 8]
                    S.op("dve", "tensor_scalar", out=t1[:], in0=MODL[:, s, :, :], scalar1=1.0, scalar2=None,
                         op0=ALU.add, r=["mod"], w=["modt1"])
                    S.op("dve", "tensor_tensor", out=MODL[:, s, :, :], in0=t1[:],
                         in1=gcol.unsqueeze(2).to_broadcast([128, 8, 2]), op=ALU.mult,
                         r=["modt1", "par"], w=["mod"])
                S.flush()

        def phase_B(l, src, dst, do_ctx):
            j = l // 2
            with ExitStack() as es:
                xw = sb(es, "xw", [128, 8, 512], F32)
                xo = sb(es, "xo", [128, 8, 512], F32)
                hx = [sb(es, "hx%d" % k, [128, 512], BF16) for k in range(8)]
                tmp = mk_tmp(es, 512)
                win = sb(es, "win", [128, 8, 3 * D], BF16)
                wout = sb(es, "wout", [128, 8, D], BF16)
                xv_sb = [sb(es, "xv%d" % i, [128, 512], F32) for i in range(2)]
                m_sb = [sb(es, "m%d" % i, [128, 512], F32) for i in range(2)]
                acc = [sb(es, "acc%d" % i, [128, 512], F32) for i in range(2)]
                z = [sb(es, "z%d" % k, [128, 512], BF16) for k in range(8)]
                for pc in range(3):
                    S.dma("sp", win[:, :, pc * D:(pc + 1) * D],
                          Wb[("b_w_in", j)][:, pc * D:(pc + 1) * D].rearrange("(k p) n -> p k n", p=128), w=[("win", pc)])
                S.dma("sp", wout[:], Wb[("b_w_out", j)].rearrange("(k p) n -> p k n", p=128), w=["wout"])
                def tile(xs, xregs, n, rowlen, which, outs, oregs):
                    ow, _ = PAR_OFF["bwc_c" if which == 1 else "bwc_l"]
                    norm_mod(xs, xregs, n, which, 1, 0, hx, tmp)
                    for c in range(8):
                        pr = c % 2
                        pb, pc_, pv = PS(3 * pr), PS(3 * pr + 1), PS(3 * pr + 2)
                        for gi, pt in enumerate((pb, pc_, pv)):
                            for k in range(8):
                                S.op("pe", "matmul", pt[:, 0:n],
                                     lhsT=win[:, k, gi * D + c * 128: gi * D + (c + 1) * 128], rhs=hx[k][:, 0:n],
                                     start=(k == 0), stop=(k == 7),
                                     r=[("win", gi), ("hx", k)], w=[("ps", 3 * pr + gi)])
                        S.op("act", "activation", out=xv_sb[pr][:, 0:n], in_=pv[:, 0:n], func=AF.Identity,
                             r=[("ps", 3 * pr + 2)], w=[("xv", pr)])
                        S.op("dve", "tensor_tensor", out=m_sb[pr][:, 0:n], in0=pc_[:, 0:n], in1=xv_sb[pr][:, 0:n],
                             op=ALU.mult, r=[("ps", 3 * pr + 1), ("xv", pr)], w=[("m", pr)])
                        w0 = par[:, ow + (j * 3 + 0) * 8 + c: ow + (j * 3 + 0) * 8 + c + 1]
                        w1 = par[:, ow + (j * 3 + 1) * 8 + c: ow + (j * 3 + 1) * 8 + c + 1]
                        w2 = par[:, ow + (j * 3 + 2) * 8 + c: ow + (j * 3 + 2) * 8 + c + 1]
                        S.op("act", "activation", out=acc[pr][:, 0:n], in_=m_sb[pr][:, 0:n], func=AF.Identity,
                             scale=w1, r=[("m", pr), "par"], w=[("acc", pr)])
                        a3 = v3(acc[pr][:, 0:n], rowlen)
                        m3 = v3(m_sb[pr][:, 0:n], rowlen)
                        S.op("dve", "scalar_tensor_tensor", out=a3[:, :, 1:rowlen], in0=m3[:, :, 0:rowlen - 1],
                             scalar=w0, op0=ALU.mult, in1=a3[:, :, 1:rowlen], op1=ALU.add,
                             r=[("m", pr), ("acc", pr), "par"], w=[("acc", pr)])
                        S.op("dve", "scalar_tensor_tensor", out=a3[:, :, 0:rowlen - 1], in0=m3[:, :, 1:rowlen],
                             scalar=w2, op0=ALU.mult, in1=a3[:, :, 0:rowlen - 1], op1=ALU.add,
                             r=[("m", pr), ("acc", pr), "par"], w=[("acc", pr)])
                        S.op("dve", "tensor_tensor", out=z[c][:, 0:n], in0=pb[:, 0:n], in1=acc[pr][:, 0:n],
                             op=ALU.mult, r=[("ps", 3 * pr), ("acc", pr)], w=[("z", c)])
                    for mo in range(8):
                        pt = PS(mo % 6)
                        for c in range(8):
                            S.op("pe", "matmul", pt[:, 0:n], lhsT=wout[:, c, mo * 128:(mo + 1) * 128],
                                 rhs=z[c][:, 0:n], start=(c == 0), stop=(c == 7),
                                 r=["wout", ("z", c)], w=[("ps", mo % 6)])
                        resid(outs[mo], pt[:, 0:n], MODL[:, 2, mo, which:which + 1], xs[mo],
                              r=[("ps", mo % 6), "mod", xregs[mo]], w=[oregs[mo]])

                if do_ctx:
                    tile([ctx[k][:, :] for k in range(8)], [("ctx", k) for k in range(8)], TC, TC, 1,
                         [ctx[k][:, :] for k in range(8)], [("ctx", k) for k in range(8)])
                for i in range(T // 512):
                    t0 = i * 512
                    S.dma("sp", xw[:], src[:, :, t0:t0 + 512].rearrange("k p t -> p k t"), w=["xw"])
                    tile([xw[:, k, :] for k in range(8)], ["xw"] * 8, 512, GW, 0,
                         [xo[:, k, :] for k in range(8)], ["xo"] * 8)
                    S.dma("sp", dst[:, :, t0:t0 + 512].rearrange("k p t -> p k t"), xo[:], r=["xo"])
                S.flush()

        def phase_F(l, src, dst, do_ctx):
            with ExitStack() as es:
                NW = 640
                xw = sb(es, "xw", [128, 8, NW], F32)
                xo = sb(es, "xo", [128, 8, 512], F32)
                hx = [sb(es, "hx%d" % k, [128, NW], BF16) for k in range(8)]
                tmp = mk_tmp(es, NW)
                wg = [sb(es, "wg%d" % i, [128, 8, 256], BF16) for i in range(2)]
                wv = [sb(es, "wv%d" % i, [128, 8, 256], BF16) for i in range(2)]
                wdn = sb(es, "wdn", [128, NJ, D], BF16)
                acc = [sb(es, "acc%d" % i, [128, 512], F32) for i in range(2)]
                sil = [sb(es, "sil%d" % i, [128, 512], F32) for i in range(2)]
                act = [sb(es, "act%d" % jj, [128, 512], BF16) for jj in range(NJ)]
                for jb in range(0, NJ, 11):
                    S.dma("sp", wdn[:, jb:jb + 11, :],
                          Wb[("f_w_down", l)][jb * 128:(jb + 11) * 128, :].rearrange("(j p) n -> p j n", p=128),
                          w=[("wdn", jb)])
                obc, _ = PAR_OFF["fbc"]

                def tile(xs, xregs, nw, co, n, shift, which, outs, oregs):
                    owc, _ = PAR_OFF["fwc_c" if which == 1 else "fwc_l"]
                    lo_ok = co >= shift
                    hi_ok = nw >= co + n + shift
                    norm_mod(xs, xregs, nw, which, 4, 3, hx, tmp)
                    gp = [(c0, min(512, nw - c0)) for c0 in range(0, nw, 512)]
                    for jj in range(NJ):
                        pr = jj % 2
                        if jj % 2 == 0:
                            wb = (jj // 2) % 2
                            S.dma("sp", wg[wb][:], Wb[("f_w_up", l)][:, jj * 128: jj * 128 + 256].rearrange(
                                "(k p) n -> p k n", p=128), w=[("wg", wb)])
                            S.dma("sp", wv[wb][:], Wb[("f_w_up", l)][:, DFF + jj * 128: DFF + jj * 128 + 256].rearrange(
                                "(k p) n -> p k n", p=128), w=[("wv", wb)])
                        wb = (jj // 2) % 2
                        wo_ = (jj % 2) * 128
                        gflat = psf[:, 2 * pr:2 * pr + 2, :].rearrange("p b c -> p (b c)")
                        pv = PS(4 + pr)
                        for pi, (c0, cn) in enumerate(gp):
                            for k in range(8):
                                S.op("pe", "matmul", PS(2 * pr + pi)[:, 0:cn], lhsT=wg[wb][:, k, wo_:wo_ + 128],
                                     rhs=hx[k][:, c0:c0 + cn], start=(k == 0), stop=(k == 7),
                                     r=[("wg", wb), ("hx", k)], w=[("ps", 2 * pr + pi)])
                        for k in range(8):
                            S.op("pe", "matmul", pv[:, 0:n], lhsT=wv[wb][:, k, wo_:wo_ + 128],
                                 rhs=hx[k][:, co:co + n], start=(k == 0), stop=(k == 7),
                                 r=[("wv", wb), ("hx", k)], w=[("ps", 4 + pr)])
                        greg = [("ps", 2 * pr), ("ps", 2 * pr + 1)]
                        w0 = par[:, owc + (l * 3 + 0) * NJ + jj: owc + (l * 3 + 0) * NJ + jj + 1]
                        w1 = par[:, owc + (l * 3 + 1) * NJ + jj: owc + (l * 3 + 1) * NJ + jj + 1]
                        w2 = par[:, owc + (l * 3 + 2) * NJ + jj: owc + (l * 3 + 2) * NJ + jj + 1]
                        bc = par[:, obc + l * NJ + jj: obc + l * NJ + jj + 1]
                        a = acc[pr]
                        S.op("dve", "tensor_scalar", out=a[:, 0:n], in0=gflat[:, co:co + n], scalar1=w1, scalar2=None,
                             op0=ALU.mult, r=greg + ["par"], w=[("acc", pr)])
                        if lo_ok:
                            o0, o1, s0 = 0, n, co - shift
                        else:
                            o0, o1, s0 = shift, n, co
                        S.op("dve", "scalar_tensor_tensor", out=a[:, o0:o1], in0=gflat[:, s0:s0 + (o1 - o0)],
                             scalar=w0, op0=ALU.mult, in1=a[:, o0:o1], op1=ALU.add,
                             r=greg + ["par", ("acc", pr)], w=[("acc", pr)])
                        if hi_ok:
                            o0, o1 = 0, n
                        else:
                            o0, o1 = 0, n - shift
                        S.op("dve", "scalar_tensor_tensor", out=a[:, o0:o1],
                             in0=gflat[:, co + shift:co + shift + (o1 - o0)],
                             scalar=w2, op0=ALU.mult, in1=a[:, o0:o1], op1=ALU.add,
                             r=greg + ["par", ("acc", pr)], w=[("acc", pr)])
                        S.op("act", "activation", out=sil[pr][:, 0:n], in_=a[:, 0:n], func=AF.Silu, bias=bc,
                             r=[("acc", pr), "par"], w=[("sil", pr)])
                        S.op("dve", "tensor_tensor", out=act[jj][:, 0:n], in0=pv[:, 0:n], in1=sil[pr][:, 0:n],
                             op=ALU.mult, r=[("ps", 4 + pr), ("sil", pr)], w=[("act", jj)])
                    for mo in range(8):
                        pt = PS(mo % 4)
                        for jj in range(NJ):
                            S.op("pe", "matmul", pt[:, 0:n], lhsT=wdn[:, jj, mo * 128:(mo + 1) * 128],
                                 rhs=act[jj][:, 0:n], start=(jj == 0), stop=(jj == NJ - 1),
                                 r=[("wdn", 0), ("wdn", 11), ("act", jj)], w=[("ps", mo % 4)])
                        resid(outs[mo], pt[:, 0:n], MODL[:, 5, mo, which:which + 1], xs[mo][:, co:co + n],
                              r=[("ps", mo % 4), "mod", xregs[mo]], w=[oregs[mo]])

                if do_ctx:
                    tile([ctx[k][:, :] for k in range(8)], [("ctx", k) for k in range(8)], TC, 0, TC, 1, 1,
                         [ctx[k][:, :] for k in range(8)], [("ctx", k) for k in range(8)])
                for i in range(NROW // 8):
                    r0 = i * 8
                    wlo, whi = max(0, r0 - 1), min(NROW, r0 + 9)
                    nw = (whi - wlo) * GW
                    co = (r0 - wlo) * GW
                    S.dma("sp", xw[:, :, 0:nw], src[:, :, wlo * GW:whi * GW].rearrange("k p t -> p k t"), w=["xw"])
                    if r0 + 9 > NROW:
                        S.op("act", "activation", out=xw[:, :, nw:nw + GW], in_=halo[:], func=AF.Identity,
                             r=["hrecv"], w=["xw"])
                        nw += GW
                    tile([xw[:, k, 0:nw] for k in range(8)], ["xw"] * 8, nw, co, 512, GW, 0,
                         [xo[:, k, :] for k in range(8)], ["xo"] * 8)
                    S.dma("sp", dst[:, :, r0 * GW:r0 * GW + 512].rearrange("k p t -> p k t"), xo[:], r=["xo"])
                S.flush()

        def phase_A1(l, src, emit_ctx):
            j = l // 2
            with ExitStack() as es:
                xw = sb(es, "xw", [128, 8, 512], F32)
                hx = [sb(es, "hx%d" % k, [128, 512], BF16) for k in range(8)]
                tmp = mk_tmp(es, 512)
                win = sb(es, "win", [128, 8, APROJ], BF16)
                qkb = sb(es, "qkb", [128, 8, 512], BF16)
                kt_sb = [sb(es, "kt%d" % i, [128, NH * DK], F32) for i in range(2)]
                va_sb = [sb(es, "va%d" % i, [128, NH * DV], BF16) for i in range(2)]
                so_sb = [sb(es, "so%d" % i, [128, D], F32) for i in range(2)]
                g_sb = [sb(es, "g%d" % i, [128, 16], F32) for i in range(2)]
                sp_sb = [sb(es, "sp%d" % i, [128, 8], F32) for i in range(2)]
                u_sb = [sb(es, "u%d" % i, [128, 8], F32) for i in range(2)]
                sc_sb = [sb(es, "sc%d" % i, [128, 16], F32) for i in range(2)]
                for pc, (c0, c1) in enumerate(((0, 1024), (1024, 2048), (2048, APROJ))):
                    S.dma("sp", win[:, :, c0:c1], Wb[("a_w_in", j)][:, c0:c1].rearrange("(k p) n -> p k n", p=128),
                          w=[("win", pc)])
                wreg = [("win", 0), ("win", 1), ("win", 2)]
                obg, _ = PAR_OFF["bgate"]
                bg = par[:, obg + j * 16: obg + j * 16 + 16]
                stc = [0]

                def tile(xs, xregs, n, which, ta0, do_o):
                    tr0, tr1 = (C_TRIF, C_TRIB) if which == 1 else (C_TRL0, C_TRL1)
                    norm_mod(xs, xregs, n, which, 1, 0, hx, tmp)
                    for m in range(8):
                        pt = PS(m % 2)
                        for k in range(8):
                            S.op("pe", "matmul", pt[:, 0:n], lhsT=win[:, k, m * 128:(m + 1) * 128], rhs=hx[k][:, 0:n],
                                 start=(k == 0), stop=(k == 7), r=wreg + [("hx", k)], w=[("ps", m % 2)])
                        S.op("act", "activation", out=qkb[:, m, 0:n], in_=pt[:, 0:n], func=AF.Identity,
                             scale=(1.0 if m < 4 else DK ** -0.5), r=[("ps", m % 2)], w=["qkb"])
                    S.dma("sp", QK[:, :, ta0:ta0 + n].rearrange("k p t -> p k t"), qkb[:, :, 0:n], r=["qkb"])
                    for s in range(n // 128):
                        st = stc[0] % 2
                        stc[0] += 1
                        tsl = slice(s * 128, (s + 1) * 128)
                        row0 = ta0 + s * 128

                        def proj(bank, c0, cn):
                            for k in range(8):
                                S.op("pe", "matmul", PS(bank)[:, 0:cn], lhsT=hx[k][:, tsl], rhs=win[:, k, c0:c0 + cn],
                                     start=(k == 0), stop=(k == 7), r=wreg + [("hx", k)], w=[("ps", bank)])
                        proj(2, 512, 512)
                        S.op("act", "activation", out=kt_sb[st][:], in_=PS(2)[:, :], func=AF.Identity,
                             scale=DK ** -0.5, r=[("ps", 2)], w=[("kt", st)])
                        S.dma("sp", KT[row0:row0 + 128, :], kt_sb[st][:], r=[("kt", st)])
                        for pi in range(2):
                            proj(3 + pi, 1024 + pi * 512, 512)
                            S.op("act", "activation", out=va_sb[st][:, pi * 512:(pi + 1) * 512], in_=PS(3 + pi)[:, :],
                                 func=AF.Identity, r=[("ps", 3 + pi)], w=[("va", st)])
                        S.dma("sp", VA[row0:row0 + 128, :], va_sb[st][:], r=[("va", st)])
                        if do_o:
                            for pi in range(2):
                                proj(2 + 2 * pi, 2048 + pi * 512, 512)
                                S.op("act", "activation", out=so_sb[st][:, pi * 512:(pi + 1) * 512],
                                     in_=PS(2 + 2 * pi)[:, :], func=AF.Sigmoid, r=[("ps", 2 + 2 * pi)],
                                     w=[("so", st)])
                            S.dma("sp", SO[row0:row0 + 128, :], so_sb[st][:], r=[("so", st)])
                        proj(5, 3072, 16)
                        S.op("dve", "tensor_tensor", out=g_sb[st][:], in0=PS(5)[:, 0:16], in1=bg, op=ALU.add,
                             r=[("ps", 5), "par"], w=[("g", st)])
                        S.op("act", "activation", out=sp_sb[st][:], in_=g_sb[st][:, 8:16], func=AF.Exp, scale=-1.0,
                             r=[("g", st)], w=[("sp", st)])
                        S.op("act", "activation", out=sp_sb[st][:], in_=sp_sb[st][:], func=AF.Ln, bias=1.0, scale=1.0,
                             r=[("sp", st)], w=[("sp", st)])
                        S.op("pe", "matmul", PS(5)[:, 32:36], lhsT=cst[:, tr0:tr0 + 128], rhs=sp_sb[st][:, 0:4],
                             start=True, stop=True, r=[("sp", st), "cst"], w=[("ps", 5)])
                        S.op("pe", "matmul", PS(5)[:, 36:40], lhsT=cst[:, tr1:tr1 + 128], rhs=sp_sb[st][:, 4:8],
                             start=True, stop=True, r=[("sp", st), "cst"], w=[("ps", 5)])
                        S.op("pe", "matmul", PS(5)[:, 64:72], lhsT=cst[:, C_BLK0:C_BLK0 + 128], rhs=sp_sb[st][:, 0:8],
                             start=True, stop=True, r=[("sp", st), "cst"], w=[("ps", 5)])
                        S.op("pe", "matmul", PS(5)[:, 72:80], lhsT=cst[:, C_BLK1:C_BLK1 + 128], rhs=sp_sb[st][:, 0:8],
                             start=True, stop=True, r=[("sp", st), "cst"], w=[("ps", 5)])
                        S.op("dve", "tensor_tensor", out=u_sb[st][:], in0=PS(5)[:, 32:40], in1=g_sb[st][:, 0:8],
                             op=ALU.add, r=[("ps", 5), ("g", st)], w=[("u", st)])
                        S.op("act", "activation", out=sc_sb[st][:, 0:8], in_=u_sb[st][:], func=AF.Exp,
                             r=[("u", st)], w=[("sc", st)])
                        S.op("act", "activation", out=sc_sb[st][:, 8:16], in_=PS(5)[:, 32:40], func=AF.Exp,
                             r=[("ps", 5)], w=[("sc", st)])
                        ch0 = row0 // CH
                        S.op("act", "activation", out=EG[:, ch0:ch0 + 2, :].rearrange("p c g -> p (c g)"),
                             in_=PS(5)[:, 64:80], func=AF.Exp, scale=-1.0, r=[("ps", 5)], w=["eg"])
                        S.dma("sp", SC[row0:row0 + 128, :], sc_sb[st][:], r=[("sc", st)])

                tile([ctx[k][:, :] for k in range(8)], [("ctx", k) for k in range(8)], TC, 1, 0, emit_ctx)
                for i in range(T // 512):
                    t0 = i * 512
                    S.dma("sp", xw[:], src[:, :, t0:t0 + 512].rearrange("k p t -> p k t"), w=["xw"])
                    tile([xw[:, k, :] for k in range(8)], ["xw"] * 8, 512, 0, TC + t0, True)
                S.flush()

        def phase_scan(d, emit_ctx):
            with ExitStack() as es:
                qk_t = [sb(es, "qkt%d" % i, [128, 8, 512], BF16) for i in range(2)]
                kt_t = [sb(es, "ktt%d" % i, [64, 8, NH * DK], F32) for i in range(2)]
                va_t = [sb(es, "vat%d" % i, [64, 8, NH * DV], BF16) for i in range(2)]
                sc_t = [sb(es, "sct%d" % i, [64, 8, 16], F32) for i in range(2)]
                hout = [sb(es, "hout%d" % i, [64, 8, D], F32) for i in range(2)]
                Ct = sb(es, "Ct", [128, NH, DV], F32)
                nst = sb(es, "nst", [128, NH], F32)
                Cb = [sb(es, "Cb%d" % i, [128, NH, DV], BF16) for i in range(2)]
                nb_ = [sb(es, "nb%d" % i, [128, NH], BF16) for i in range(2)]
                ntmp = sb(es, "ntmp", [128, NH], F32)
                pT = [sb(es, "pT%d" % i, [64, NH, 64], BF16) for i in range(2)]
                ks = [sb(es, "ks%d" % i, [64, NH * DK], BF16) for i in range(2)]
                absb = sb(es, "absb", [64, NH], F32)
                den = sb(es, "den", [64, NH], F32)
                onesb = sb(es, "onesb", [128, 2], BF16)
                S.op("dve", "memset", Ct[:], 0.0, w=[("Ct", h) for h in range(NH)])
                S.op("dve", "memset", nst[:], 0.0, w=["nst"])
                S.op("dve", "memset", Cb[0][:], 0.0, w=[("Cb", 0, h) for h in range(NH)])
                S.op("dve", "memset", nb_[0][:], 0.0, w=[("nb", 0)])
                S.op("dve", "memset", onesb[:], 1.0, w=["onesb"])
                issue_bg_casts()
                mc_ctx = C_MF if d == 0 else C_MB
                mc_lat = C_ML0 if d == 0 else C_ML1
                blocks = [(0, 4, True)] + [(TC + i * 512, 8, False) for i in range(T // 512)]
                if d == 1:
                    blocks = ([blocks[0]] if emit_ctx else []) + blocks[1:][::-1]
                chunks = []
                for bi, (ta0, nb, is_ctx) in enumerate(blocks):
                    order = list(range(nb)) if d == 0 else list(range(nb))[::-1]
                    for ci in order:
                        chunks.append(dict(bi=bi, bf=bi % 2, ta0=ta0, nb=nb, is_ctx=is_ctx, ci=ci,
                                           chg=ta0 // CH + ci, emit=((not is_ctx) or emit_ctx),
                                           first=(ci == order[0]), last=(ci == order[-1])))
                ones4 = cst[:, C_ONES:C_ONES + NH]
                SB = (0, 6)
                st = dict(prev=None, injected=(d == 0))

                def load_block(bi):
                    if bi >= len(blocks):
                        return
                    ta0, nb, _ = blocks[bi]
                    bf, ntok = bi % 2, nb * 64
                    if True:
                        S.dma("sp", qk_t[bf][:, :, 0:ntok], QK[:, :, ta0:ta0 + ntok].rearrange("k p t -> p k t"),
                              w=[("qkt", bf)])
                        S.dma("sp", kt_t[bf][:, 0:nb, :], KT[ta0:ta0 + ntok, :].rearrange("(c s) d -> s c d", s=64),
                              w=[("ktt", bf)])
                        S.dma("sp", va_t[bf][:, 0:nb, :], VA[ta0:ta0 + ntok, :].rearrange("(c s) d -> s c d", s=64),
                              w=[("vat", bf)])
                        S.dma("sp", sc_t[bf][:, 0:nb, :], SC[ta0:ta0 + ntok, :].rearrange("(c s) d -> s c d", s=64),
                              w=[("sct", bf)])

                def P1(ch, q):
                    bf, ci = ch["bf"], ch["ci"]
                    tsl = slice(ci * 64, (ci + 1) * 64)
                    if ch["emit"]:
                        for h in range(NH):
                            S.op("pe", "matmul", PS(SB[q % 2])[0:64, h * 64:(h + 1) * 64], lhsT=qk_t[bf][:, 4 + h, tsl],
                                 rhs=qk_t[bf][:, h, tsl], start=True, stop=True,
                                 r=[("qkt", bf)], w=[("ps", SB[q % 2])])
                    S.op("dve", "tensor_tensor", out=ks[q % 2][:].rearrange("p (h d) -> p h d", h=NH),
                         in0=kt_t[bf][:, ci, :].rearrange("p (h d) -> p h d", h=NH),
                         in1=sc_t[bf][:, ci, d * 4:d * 4 + 4].unsqueeze(2).to_broadcast([64, NH, DK]), op=ALU.mult,
                         r=[("ktt", bf), ("sct", bf)], w=[("ks", q % 2)])

                def P2(ch, q):
                    bf, ci = ch["bf"], ch["ci"]
                    mcol = mc_ctx if ch["is_ctx"] else mc_lat
                    mask = cst[0:64, mcol:mcol + 64]
                    if ch["emit"]:
                        for h in range(NH):
                            S.op("dve", "scalar_tensor_tensor", out=pT[q % 2][:, h, :],
                                 in0=PS(SB[q % 2])[0:64, h * 64:(h + 1) * 64],
                                 scalar=sc_t[bf][:, ci, d * 4 + h:d * 4 + h + 1],
                                 op0=ALU.mult, in1=mask, op1=ALU.mult,
                                 r=[("ps", SB[q % 2]), ("sct", bf), "cst"], w=[("pT", q % 2, h)])
                    for h in range(NH):
                        pu = PS(4 + h // 2)[:, (h % 2) * 256:(h % 2) * 256 + 256]
                        S.op("pe", "matmul", pu, lhsT=ks[q % 2][:, h * DK:(h + 1) * DK],
                             rhs=va_t[bf][:, ci, h * DV:(h + 1) * DV], start=True, stop=True,
                             r=[("ks", q % 2), ("vat", bf)], w=[("ps", 4 + h // 2)])
                    for h in range(NH):
                        S.op("pe", "matmul", PS(3)[:, 8 + h:9 + h], lhsT=ks[q % 2][:, h * DK:(h + 1) * DK],
                             rhs=onesb[0:64, 0:1], start=True, stop=True, r=[("ks", q % 2), "onesb"], w=[("psn",)])

                def P3(ch, q):
                    bf, ci, chg = ch["bf"], ch["ci"], ch["chg"]
                    tsl = slice(ci * 64, (ci + 1) * 64)
                    cur, nxt = q % 2, (q + 1) % 2
                    if not ch["is_ctx"] and not st["injected"]:
                        st["injected"] = True
                        st["prev"] = "ones"
                        S.op("dve", "tensor_copy", out=Ct[:].rearrange("p h v -> p (h v)"), in_=crecv[:, 0:NH * DV],
                             r=["crecv"], w=[("Ct", h) for h in range(NH)])
                        S.op("dve", "tensor_copy", out=nst[:], in_=crecv[:, NH * DV:NST], r=["crecv"], w=["nst"])
                        S.op("act", "activation", out=Cb[cur][:].rearrange("p h v -> p (h v)"),
                             in_=crecv[:, 0:NH * DV], func=AF.Identity, r=["crecv"], w=[("Cb", cur, h) for h in range(NH)])
                        S.op("act", "activation", out=nb_[cur][:], in_=crecv[:, NH * DV:NST], func=AF.Identity,
                             r=["crecv"], w=[("nb", cur)])
                    use_ones = (st["prev"] == "ones")
                    pgl = chg if (st["prev"] is None or use_ones) else st["prev"]
                    st["prev"] = chg
                    if ch["emit"]:
                        for h in range(NH):
                            pa = PS(1 + h // 2)[0:64, (h % 2) * 256:(h % 2) * 256 + 256]
                            S.op("pe", "matmul", pa, lhsT=pT[cur][:, h, :], rhs=va_t[bf][:, ci, h * DV:(h + 1) * DV],
                                 start=True, stop=False, r=[("pT", cur, h), ("vat", bf)], w=[("ps", 1 + h // 2)])
                            S.op("pe", "matmul", pa, lhsT=qk_t[bf][:, h, tsl], rhs=Cb[cur][:, h, :],
                                 start=False, stop=True, r=[("qkt", bf), ("Cb", cur, h)], w=[("ps", 1 + h // 2)])
                        for h in range(NH):
                            pbn = PS(3)[0:64, h:h + 1]
                            S.op("pe", "matmul", pbn, lhsT=pT[cur][:, h, :], rhs=onesb[0:64, 0:1],
                                 start=True, stop=False, r=[("pT", cur, h), "onesb"], w=[("psb4",)])
                            S.op("pe", "matmul", pbn, lhsT=qk_t[bf][:, h, tsl], rhs=nb_[cur][:, h:h + 1],
                                 start=False, stop=True, r=[("qkt", bf), ("nb", cur)], w=[("psb4",)])
                    egp = ones4 if use_ones else EG[:, pgl, d * 4:d * 4 + 4]
                    egc = EG[:, chg, d * 4:d * 4 + 4]
                    for h in range(NH):
                        pu = PS(4 + h // 2)[:, (h % 2) * 256:(h % 2) * 256 + 256]
                        S.op("dve", "scalar_tensor_tensor", out=Ct[:, h, :], in0=Ct[:, h, :],
                             scalar=(cst[:, C_ONES:C_ONES + 1] if use_ones else EG[:, pgl, d * 4 + h:d * 4 + h + 1]),
                             op0=ALU.mult, in1=pu, op1=ALU.add,
                             r=[("Ct", h), "eg", ("ps", 4 + h // 2)], w=[("Ct", h)])
                        S.op("act", "activation", out=Cb[nxt][:, h, :], in_=Ct[:, h, :], func=AF.Identity,
                             scale=EG[:, chg, d * 4 + h:d * 4 + h + 1], r=[("Ct", h), "eg"], w=[("Cb", nxt, h)])
                    S.op("dve", "tensor_tensor", out=ntmp[:], in0=nst[:], in1=egp, op=ALU.mult,
                         r=["nst", "eg"], w=["ntmp"])
                    S.op("dve", "tensor_tensor", out=nst[:], in0=PS(3)[:, 8:8 + NH], in1=ntmp[:], op=ALU.add,
                         r=[("psn",), "ntmp"], w=["nst"])
                    S.op("dve", "tensor_tensor", out=nb_[nxt][:], in0=nst[:], in1=egc, op=ALU.mult,
                         r=["nst", "eg"], w=[("nb", nxt)])

                def P4(ch, q):
                    bf, ci = ch["bf"], ch["ci"]
                    if not ch["emit"]:
                        return
                    ho = hout[ch["bi"] % 2]
                    S.op("act", "activation", out=absb[:], in_=PS(3)[0:64, 0:NH], func=AF.Abs,
                         r=[("psb4",)], w=["absb"])
                    S.op("dve", "tensor_tensor", out=den[:], in0=absb[:], in1=sc_t[bf][:, ci, 8 + d * 4:12 + d * 4],
                         op=ALU.max, r=["absb", ("sct", bf)], w=["den"])
                    S.op("dve", "reciprocal", out=den[:], in_=den[:], r=["den"], w=["den"])
                    for h in range(NH):
                        pa = PS(1 + h // 2)[0:64, (h % 2) * 256:(h % 2) * 256 + 256]
                        S.op("act", "activation", out=ho[:, ci, h * DV:(h + 1) * DV], in_=pa, func=AF.Identity,
                             scale=den[:, h:h + 1], r=[("ps", 1 + h // 2), "den"], w=[("hout", ch["bi"] % 2, h)])
                    if ch["last"]:
                        ta0, ntok, nb = ch["ta0"], ch["nb"] * 64, ch["nb"]
                        S.dma("sp", HD[d][ta0:ta0 + ntok, :].rearrange("(c s) v -> s c v", s=64), ho[:, 0:nb, :],
                              r=[("hout", ch["bi"] % 2, h) for h in range(NH)])

                load_block(0)
                load_block(1)
                P1(chunks[0], 0)
                P2(chunks[0], 0)
                for q, ch in enumerate(chunks):
                    P3(ch, q)
                    if q + 1 < len(chunks):
                        P1(chunks[q + 1], q + 1)
                    P4(ch, q)
                    if ch["last"]:
                        load_block(ch["bi"] + 2)
                    if q + 1 < len(chunks):
                        P2(chunks[q + 1], q + 1)
                if d == 0:
                    csend = sb(es, "csend", [128, NST], F32)
                    lastc = st["prev"]
                    for h in range(NH):
                        S.op("act", "activation", out=csend[:, h * DV:(h + 1) * DV], in_=Ct[:, h, :], func=AF.Identity,
                             scale=EG[:, lastc, d * 4 + h:d * 4 + h + 1], r=[("Ct", h), "eg"], w=["csend"])
                    S.op("dve", "tensor_tensor", out=csend[:, NH * DV:NST], in0=nst[:],
                         in1=EG[:, lastc, d * 4:d * 4 + 4], op=ALU.mult, r=["nst", "eg"], w=["csend"])
                    S.dma("sp", csrc.ap(), csend[:], r=["csend"], w=["csrc"])
                S.flush()
            if d == 0:
                exchange(csrc, cdst, NST, lambda a: None, crecv[:], "c")

        def phase_A5(l, src, dst, do_ctx):
            j = l // 2
            with ExitStack() as es:
                xw = sb(es, "xw", [128, 8, 512], F32)
                xo = sb(es, "xo", [128, 8, 512], F32)
                hf = [sb(es, "hf%d" % i, [128, D], F32) for i in range(2)]
                hb = [sb(es, "hb%d" % i, [128, D], F32) for i in range(2)]
                so = [sb(es, "so%d" % i, [128, D], F32) for i in range(2)]
                hs = sb(es, "hs", [128, D], F32)
                junk = sb(es, "junk", [128, DV], BF16)
                ss = sb(es, "ss", [128, NH], F32)
                yb = [sb(es, "yb%d" % i, [128, D], BF16) for i in range(2)]
                gain = sb(es, "gaint", [128, D], F32)
                yT = sb(es, "yT", [128, 8, 512], BF16)
                wout = sb(es, "wout", [128, 8, D], BF16)
                S.dma("sp", gain[:], gain_in[:, j, :], w=["gain"])
                S.dma("sp", wout[:], Wb[("a_w_out", j)].rearrange("(k p) n -> p k n", p=128), w=["wout"])
                stc = [0]

                def tile(xs, xregs, n, which, ta0, outs, oregs):
                    for s in range(n // 128):
                        st = stc[0] % 2
                        stc[0] += 1
                        row0 = ta0 + s * 128
                        S.dma("sp", hf[st][:], HD[0][row0:row0 + 128, :], w=[("hf", st)])
                        S.dma("sp", hb[st][:], HD[1][row0:row0 + 128, :], w=[("hb", st)])
                        S.dma("sp", so[st][:], SO[row0:row0 + 128, :], w=[("so", st)])
                        S.op("pool", "tensor_tensor", out=hs[:], in0=hf[st][:], in1=hb[st][:], op=ALU.add,
                             r=[("hf", st), ("hb", st)], w=["hs"])
                        for h in range(NH):
                            S.op("act", "activation", out=junk[:], in_=hs[:, h * DV:(h + 1) * DV], func=AF.Square,
                                 accum_out=ss[:, h:h + 1], r=["hs"], w=["junk", "ss"])
                        S.op("act", "activation", out=ss[:], in_=ss[:], func=AF.Sqrt, scale=1.0 / DV, bias=EPS,
                             r=["ss"], w=["ss"])
                        S.op("dve", "reciprocal", out=ss[:], in_=ss[:], r=["ss"], w=["ss"])
                        S.op("pool", "tensor_tensor", out=so[st][:], in0=so[st][:], in1=gain[:], op=ALU.mult,
                             r=[("so", st), "gain"], w=[("so", st)])
                        for h in range(NH):
                            S.op("dve", "scalar_tensor_tensor", out=yb[st][:, h * DV:(h + 1) * DV],
                                 in0=hs[:, h * DV:(h + 1) * DV], scalar=ss[:, h:h + 1], op0=ALU.mult,
                                 in1=so[st][:, h * DV:(h + 1) * DV], op1=ALU.mult,
                                 r=["hs", "ss", ("so", st)], w=[("yb", st)])
                        for c in range(8):
                            S.op("pe", "transpose", psb[:, c * 128:(c + 1) * 128], yb[st][:, c * 128:(c + 1) * 128],
                                 identb[:], r=[("yb", st), "identb"], w=["psb"])
                        S.op("act", "activation", out=yT[:, :, s * 128:(s + 1) * 128],
                             in_=psb[:].rearrange("p (c t) -> p c t", c=8), func=AF.Identity, r=["psb"], w=["yT"])
                    for mo in range(8):
                        pt = PS(mo % 4)
                        for c in range(8):
                            S.op("pe", "matmul", pt[:, 0:n], lhsT=wout[:, c, mo * 128:(mo + 1) * 128], rhs=yT[:, c, 0:n],
                                 start=(c == 0), stop=(c == 7), r=["wout", "yT"], w=[("ps", mo % 4)])
                        resid(outs[mo], pt[:, 0:n], MODL[:, 2, mo, which:which + 1], xs[mo],
                              r=[("ps", mo % 4), "mod", xregs[mo]], w=[oregs[mo]])

                if do_ctx:
                    tile([ctx[k][:, :] for k in range(8)], [("ctx", k) for k in range(8)], TC, 1, 0,
                         [ctx[k][:, :] for k in range(8)], [("ctx", k) for k in range(8)])
                for i in range(T // 512):
                    t0 = i * 512
                    S.dma("sp", xw[:], src[:, :, t0:t0 + 512].rearrange("k p t -> p k t"), w=["xw"])
                    tile([xw[:, k, :] for k in range(8)], ["xw"] * 8, 512, 0, TC + t0,
                         [xo[:, k, :] for k in range(8)], ["xo"] * 8)
                    S.dma("sp", dst[:, :, t0:t0 + 512].rearrange("k p t -> p k t"), xo[:], r=["xo"])
                S.flush()

        def phase_final(src):
            with ExitStack() as es:
                xw = sb(es, "xw", [128, 8, 512], F32)
                xo = sb(es, "xo", [128, 8, 512], F32)
                tmp = mk_tmp(es, 512)
                og, _ = PAR_OFF["gfin"]
                for i in range(T // 512):
                    t0 = i * 512
                    S.dma("sp", xw[:], src[:, :, t0:t0 + 512].rearrange("k p t -> p k t"), w=["xw"])
                    for k in range(8):
                        sq = tmp["sq%d" % (k % 2)]
                        S.op("act", "activation", out=sq[:], in_=xw[:, k, :], func=AF.Square, r=["xw"], w=[("sq", k % 2)])
                        S.op("pe", "matmul", PS(6)[:, :], lhsT=ones_f, rhs=sq[:], start=(k == 0), stop=(k == 7),
                             r=[("sq", k % 2), "cst"], w=[("ps", 6)])
                    std = tmp["std"]
                    S.op("act", "activation", out=std[:], in_=PS(6)[:, :], func=AF.Sqrt, scale=1.0 / D, bias=EPS,
                         r=[("ps", 6)], w=["std"])
                    S.op("dve", "reciprocal", out=std[:], in_=std[:], r=["std"], w=["std"])
                    for k in range(8):
                        S.op("dve", "scalar_tensor_tensor", out=xo[:, k, :], in0=xw[:, k, :],
                             scalar=par[:, og + k:og + k + 1], op0=ALU.mult, in1=std[:], op1=ALU.mult,
                             r=["xw", "par", "std"], w=["xo"])
                    S.dma("sp", outT[:, :, t0:t0 + 512].rearrange("k p t -> p k t"), xo[:], r=["xo"])
                S.flush()

        def halo_exchange(xsrc):
            def load(a):
                S.dma("sp", a.rearrange("p (k t) -> p k t", k=8),
                      xsrc[:, :, T - GW:T].rearrange("k p t -> p k t"), w=["hsrc"])
            exchange(hsrc, hdst, 8 * GW, load, halo[:].rearrange("p k t -> p (k t)"), "h")

        cur = xT_in
        last_mod = None
        for kind, l in plan:
            if last_mod != l:
                need_weights(("mod", l))
                phase_mod(l)
                last_mod = l
            ctx_live = l < 2
            if kind == "A":
                need_weights(("a", l // 2))
                phase_A1(l, cur, ctx_live)
                phase_scan(0, ctx_live)
                phase_scan(1, ctx_live)
                phase_A5(l, cur, XB, ctx_live)
                cur = XB
            elif kind == "B":
                need_weights(("b", l // 2))
                phase_B(l, cur, XB, ctx_live)
                cur = XB
            elif kind == "F":
                need_weights(("f", l))
                halo_exchange(cur)
                phase_F(l, cur, XA, ctx_live)
                cur = XA
            elif kind == "M":
                pass
        if final:
            phase_final(cur)
        else:
            for k in range(8):
                S.dma("sp", outT[k], cur[k])
        for k in range(8):
            S.dma("sp", ctx_out[k], ctx[k][:], r=[("ctx", k)])
        S.flush()
        S.finish("sp")
        print("sched: ins=%d waits=%d cnt=%s" % (S.n_ins, S.n_wait, S.cnt))
    return nc


FULL_PLAN = [("A", 0), ("F", 0), ("B", 1), ("F", 1), ("A", 2), ("F", 2), ("B", 3), ("F", 3)]
_NC_CACHE = {}


def pack_par(b, odd, c, c_ctx, b_mod, g_mix, g_ffn, g_final, b_w_conv, f_w_conv, f_b_conv, a_b_gate):
    par = np.zeros((128, NPAR), np.float32)

    def put(name, arr):
        o, n = PAR_OFF[name]
        a = np.asarray(arr, np.float32).reshape(128, -1)
        assert a.shape[1] == n, (name, a.shape, n)
        par[:, o:o + n] = a
    bwc = np.asarray(b_w_conv, np.float32)
    fwc = np.asarray(f_w_conv, np.float32)
    put("cc", np.stack([fm(c[b]), fm(c_ctx)], axis=-1))
    put("bmod", fm(b_mod))
    put("gmix", fm(g_mix))
    put("gffn", fm(g_ffn))
    put("gfin", fm(g_final))
    put("bwc_l", fm(bwc))
    put("bwc_c", fm(bwc[:, ::-1] if odd else bwc))
    put("fwc_l", fm(fwc[:, ::-1] if odd else fwc))
    put("fwc_c", fm(fwc[:, ::-1] if odd else fwc))
    put("fbc", fm(f_b_conv))
    put("bgate", np.broadcast_to(gate_perm(np.asarray(a_b_gate, np.float32), odd).reshape(1, 32), (128, 32)))
    put("sel", np.broadcast_to(np.array([[1.0, 0.0]] if odd else [[0.0, 1.0]], np.float32), (128, 2)))
    return par


def gate_perm(g, odd):
    if not odd:
        return g
    sh = g.shape
    return np.ascontiguousarray(g.reshape(sh[:-1] + (2, 2, NH))[..., ::-1, :].reshape(sh))


def core_tokens(x_b, odd):
    r = np.asarray(x_b, np.float32).reshape(TFULL // GW, GW, D)
    r = r[NROW:][::-1] if odd else r[:NROW]
    return np.ascontiguousarray(r.reshape(T, D).T.reshape(8, 128, T))


def make_in_maps(inputs, cores, plan=None):
    maps = []
    gain = np.ascontiguousarray(np.broadcast_to(
        np.asarray(inputs["a_head_gain"], np.float32)[None], (128, 2, D)))
    wcache = {}
    for b, odd in cores:
        cx = np.asarray(inputs["ctx"][b], np.float32)
        if odd:
            cx = cx[::-1]
        m = {
            "xT": core_tokens(inputs["x"][b], odd),
            "ctxT": np.ascontiguousarray(cx.T.reshape(8, 128, TC)),
            "par": pack_par(b, odd, inputs["c"], inputs["c_ctx"], inputs["b_mod"], inputs["g_mix"], inputs["g_ffn"],
                            inputs["g_final"], inputs["b_w_conv"], inputs["f_w_conv"], inputs["f_b_conv"],
                            inputs["a_b_gate"]),
            "consts": make_consts(odd),
            "gain": gain,
        }
        for kind, l in (plan if plan is not None else FULL_PLAN):
            m["w_mod_%d" % l] = np.ascontiguousarray(inputs["w_mod"][l], np.float32)
            if kind == "A":
                j = l // 2
                key = ("awin", j, odd)
                if key not in wcache:
                    w = np.array(inputs["a_w_in"][j], np.float32)
                    w[:, 3072:] = gate_perm(w[:, 3072:], odd)
                    wcache[key] = w
                m["a_w_in_%d" % j] = wcache[key]
                m["a_w_out_%d" % j] = np.ascontiguousarray(inputs["a_w_out"][j], np.float32)
            elif kind == "B":
                m["b_w_in_%d" % (l // 2)] = np.ascontiguousarray(inputs["b_w_in"][l // 2], np.float32)
                m["b_w_out_%d" % (l // 2)] = np.ascontiguousarray(inputs["b_w_out"][l // 2], np.float32)
            elif kind == "F":
                m["f_w_up_%d" % l] = np.ascontiguousarray(inputs["f_w_up"][l], np.float32)
                m["f_w_down_%d" % l] = np.ascontiguousarray(inputs["f_w_down"][l], np.float32)
        maps.append(m)
    return maps


def assemble(results, cores, nb):
    out = np.empty((nb, TFULL, D), np.float32)
    for res, (b, odd) in zip(results, cores):
        o = res["outT"].reshape(D, T).T.reshape(NROW, GW, D)
        if odd:
            out[b, T:] = o[::-1].reshape(T, D)
        else:
            out[b, :T] = o.reshape(T, D)
    return out


def kernel(**inputs):
    key = "full"
    if key not in _NC_CACHE:
        _NC_CACHE[key] = build(FULL_PLAN, final=True, ncores=8)
    nc = _NC_CACHE[key]
    cores = [(b, odd) for b in range(4) for odd in (0, 1)]
    in_maps = make_in_maps(inputs, cores)
    res = run_bass_kernel_spmd(nc, in_maps, core_ids=list(range(8)))
    return assemble(res.results, cores, 4)
```

```python
import numpy as np
from contextlib import ExitStack
import ml_dtypes
import concourse.bass as bass
import concourse.mybir as mybir
from concourse.bass_utils import run_bass_kernel_spmd

F32 = mybir.dt.float32
BF16 = mybir.dt.bfloat16
AF = mybir.ActivationFunctionType
ALU = mybir.AluOpType

D = 1024
TFULL = 8192
T = 4096
TC = 256
TA = TC + T
DEPTH = 4
NH = 4
DK = 128
DV = 256
DVA = DV + 1
DFF = 2816
NJ = DFF // 128
GW = 64
CH = 64
NROW = T // GW
APROJ = 3088
EPS = 1e-6
NCH = TA // CH
SAME_ENG_SYNC = True


class Sched:
    ENGS = ("pe", "act", "dve", "pool", "sp")

    def __init__(self, nc, es, n_dma_sems=48, n_bg_sems=8):
        self.nc = nc
        self.eng = {"pe": nc.tensor, "act": nc.scalar, "dve": nc.vector,
                    "pool": nc.gpsimd, "sp": nc.sync}
        self.sems = []
        self.esem = {}
        for e in self.ENGS:
            self.esem[e] = len(self.sems)
            self.sems.append(es.enter_context(nc.semaphore("s_" + e)))
        self.dsem = []
        for i in range(n_dma_sems):
            self.dsem.append(len(self.sems))
            self.sems.append(es.enter_context(nc.semaphore("d%d" % i)))
        self.bsem = []
        for i in range(n_bg_sems):
            self.bsem.append(len(self.sems))
            self.sems.append(es.enter_context(nc.semaphore("b%d" % i)))
        self.brr = 0
        self.btarget = [0] * n_bg_sems
        self.bgev = {}
        self.ops = []
        self.cnt = {e: 0 for e in self.ENGS}
        self.waited = {e: {} for e in self.ENGS}
        self.pending = {e: {} for e in self.ENGS}
        self.rr = 0
        self.target = [0] * n_dma_sems
        self.n_ins = 0
        self.n_wait = 0

    def op(self, eng, meth, *args, r=(), w=(), **kw):
        self.ops.append(dict(eng=eng, meth=meth, args=args, kw=kw, r=tuple(r), w=tuple(w),
                             dma=False, signal=False, ev=None))

    def dma(self, eng, out, in_, r=(), w=(), bg=None):
        self.ops.append(dict(eng=eng, meth="dma_start", args=(), kw=dict(out=out, in_=in_),
                             r=tuple(r), w=tuple(w), dma=True, signal=True, ev=None, inc=16, bg=bg))

    def join_bg(self, group):
        for s, v in self.bgev.pop(group, []):
            for e in self.ENGS:
                if self.pending[e].get(s, 0) < v:
                    self.pending[e][s] = v

    def coll(self, ins, outs, groups, r=(), w=()):
        self.ops.append(dict(eng="pool", meth="collective_compute", args=("AllGather", ALU.bypass),
                             kw=dict(replica_groups=groups, ins=ins, outs=outs),
                             r=tuple(r), w=tuple(w), dma=True, signal=True, ev=None, inc=1))

    def flush(self):
        ops = self.ops
        self.ops = []
        state = {}
        deps = []
        last = {}
        for i, o in enumerate(ops):
            d = {}
            for r in o["r"]:
                st = state.get(r)
                if st is not None and st[0] is not None:
                    d[id(st[0])] = st[0]
            for r in o["w"]:
                st = state.get(r)
                if st is not None:
                    if st[0] is not None:
                        d[id(st[0])] = st[0]
                    for x in st[1].values():
                        d[id(x)] = x
                    for x in st[2]:
                        d[id(x)] = x
            for r in o["r"]:
                st = state.setdefault(r, [None, {}, []])
                if o["dma"]:
                    st[2].append(o)
                else:
                    st[1][o["eng"]] = o
            for r in o["w"]:
                st = state.setdefault(r, [None, {}, []])
                st[0] = o
                st[1] = {}
                st[2] = []
            d.pop(id(o), None)
            dd = []
            for oj in d.values():
                if (not oj["dma"]) and (not o["dma"]) and oj["eng"] == o["eng"] and \
                        (o["eng"] == "pe" or not SAME_ENG_SYNC):
                    continue
                dd.append(oj)
                oj["signal"] = True
            deps.append(dd)
            if not o["dma"]:
                last[o["eng"]] = o
        for o in last.values():
            o["signal"] = True
        nd = len(self.dsem)
        bar = {}
        for i, o in enumerate(ops):
            en = o["eng"]
            E = self.eng[en]
            need = dict(self.pending[en])
            self.pending[en] = {}
            for oj in deps[i]:
                s, v = oj["ev"]
                if need.get(s, 0) < v:
                    need[s] = v
            bgd = o["dma"] and o.get("bg") is not None
            if bgd:
                k = self.brr
                self.brr = (self.brr + 1) % len(self.bsem)
                s = self.bsem[k]
                if self.btarget[k] > 0 and need.get(s, 0) < self.btarget[k]:
                    need[s] = self.btarget[k]
            elif o["dma"]:
                k = self.rr
                self.rr = (self.rr + 1) % nd
                s = self.dsem[k]
                if self.target[k] > 0 and need.get(s, 0) < self.target[k]:
                    need[s] = self.target[k]
            for s, v in need.items():
                if self.waited[en].get(s, 0) < v:
                    E.wait_ge(self.sems[s], v)
                    self.waited[en][s] = v
                    self.n_wait += 1
            ins = getattr(E, o["meth"])(*o["args"], **o["kw"])
            self.n_ins += 1
            if bgd:
                self.btarget[k] += 16
                ins.then_inc(self.sems[self.bsem[k]], 16)
                o["ev"] = (self.bsem[k], self.btarget[k])
                self.bgev.setdefault(o["bg"], []).append(o["ev"])
            elif o["dma"]:
                self.target[k] += o["inc"]
                if o["inc"] == 16:
                    ins.then_inc(self.sems[self.dsem[k]], 16)
                else:
                    ins.then_inc(self.sems[self.dsem[k]])
                o["ev"] = (self.dsem[k], self.target[k])
                bar[self.dsem[k]] = self.target[k]
            elif o["signal"]:
                self.cnt[en] += 1
                ins.then_inc(self.sems[self.esem[en]], 1)
                o["ev"] = (self.esem[en], self.cnt[en])
                bar[self.esem[en]] = self.cnt[en]
        for e in self.ENGS:
            p = self.pending[e]
            for s, v in bar.items():
                if p.get(s, 0) < v:
                    p[s] = v

    def finish(self, eng="sp"):
        E = self.eng[eng]
        for s, v in self.pending[eng].items():
            if self.waited[eng].get(s, 0) < v:
                E.wait_ge(self.sems[s], v)
                self.waited[eng][s] = v


def _par_layout():
    off = {}
    o = 0
    for name, n in (("cc", 16), ("bmod", DEPTH * 48), ("gmix", DEPTH * 8), ("gffn", DEPTH * 8),
                    ("gfin", 8), ("bwc_l", 2 * 3 * 8), ("bwc_c", 2 * 3 * 8), ("fwc_l", DEPTH * 3 * NJ),
                    ("fwc_c", DEPTH * 3 * NJ), ("fbc", DEPTH * NJ), ("bgate", 2 * 16), ("sel", 2)):
        off[name] = (o, n)
        o += n
    return off, o


PAR_OFF, NPAR = _par_layout()
C_ONES, C_TRIF, C_TRIB, C_BLK0, C_BLK1, C_ID, C_MF, C_MB, C_TRL0, C_TRL1, C_ML0, C_ML1, NCONST = \
    0, 128, 256, 384, 512, 640, 768, 832, 896, 1024, 1152, 1216, 1280


def fm(v):
    v = np.asarray(v, np.float32)
    lead = v.shape[:-1]
    k = v.shape[-1] // 128
    a = v.reshape(lead + (k, 128))
    a = np.moveaxis(a, -1, 0)
    return np.ascontiguousarray(a)


def make_consts(odd):
    c = np.zeros((128, NCONST), np.float32)
    c[:, C_ONES:C_ONES + 128] = 1.0
    s = np.arange(128)[:, None]
    t = np.arange(128)[None, :]
    same = (s // 64) == (t // 64)
    c[:, C_TRIF:C_TRIF + 128] = (same & (s <= t))
    c[:, C_TRIB:C_TRIB + 128] = (same & (s >= t))
    c[:, C_BLK0:C_BLK0 + 128] = (s < 64)
    c[:, C_BLK1:C_BLK1 + 128] = (s >= 64)
    c[:, C_ID:C_ID + 128] = (s == t)
    s6 = np.arange(128)[:, None] % 64
    t6 = np.arange(64)[None, :]
    c[:, C_MF:C_MF + 64] = (s6 <= t6)
    c[:, C_MB:C_MB + 64] = (s6 >= t6)
    f0, f1 = (C_TRIB, C_TRIF) if odd else (C_TRIF, C_TRIB)
    c[:, C_TRL0:C_TRL0 + 128] = c[:, f0:f0 + 128]
    c[:, C_TRL1:C_TRL1 + 128] = c[:, f1:f1 + 128]
    m0, m1 = (C_MB, C_MF) if odd else (C_MF, C_MB)
    c[:, C_ML0:C_ML0 + 64] = c[:, m0:m0 + 64]
    c[:, C_ML1:C_ML1 + 64] = c[:, m1:m1 + 64]
    return c


def build(plan, final=True, ncores=8):
    nc = bass.Bass("TRN2", target_bir_lowering=False)

    def dram(name, shape, dtype, kind):
        return nc.dram_tensor(name, list(shape), dtype, kind=kind).ap()

    xT_in = dram("xT", [8, 128, T], F32, "ExternalInput")
    ctxT_in = dram("ctxT", [8, 128, TC], F32, "ExternalInput")
    par_in = dram("par", [128, NPAR], F32, "ExternalInput")
    const_in = dram("consts", [128, NCONST], F32, "ExternalInput")
    gain_in = dram("gain", [128, 2, D], F32, "ExternalInput")
    need_w = set()
    for kind, l in plan:
        need_w.add(("mod", l))
        if kind == "A":
            need_w.add(("a", l // 2))
        elif kind == "B":
            need_w.add(("b", l // 2))
        elif kind == "F":
            need_w.add(("f", l))
    Wi, Wb = {}, {}
    for kind, l in sorted(need_w):
        if kind == "mod":
            specs = [("w_mod", [D, 6 * D])]
        elif kind == "a":
            specs = [("a_w_in", [D, APROJ]), ("a_w_out", [D, D])]
        elif kind == "b":
            specs = [("b_w_in", [D, 3 * D]), ("b_w_out", [D, D])]
        else:
            specs = [("f_w_up", [D, 2 * DFF]), ("f_w_down", [DFF, D])]
        for nm, shp in specs:
            Wi[(nm, l)] = dram("%s_%d" % (nm, l), shp, F32, "ExternalInput")
            if nm == "f_w_up":
                Wb[(nm, l)] = dram("%s_%d_b" % (nm, l), shp, BF16, "Internal")
    outT = dram("outT", [8, 128, T], F32, "ExternalOutput")
    ctx_out = dram("ctx_out", [8, 128, TC], F32, "ExternalOutput")
    XA = dram("XA", [8, 128, T], F32, "Internal")
    XB = dram("XB", [8, 128, T], F32, "Internal")
    QK = dram("QK", [8, 128, TA], BF16, "Internal")
    KT = dram("KT", [TA, NH * DK], F32, "Internal")
    VA = dram("VA", [TA, NH * DV], BF16, "Internal")
    SO = dram("SO", [TA, D], F32, "Internal")
    SC = dram("SC", [TA, 16], F32, "Internal")
    HD = [dram("HF", [TA, D], F32, "Internal"), dram("HB", [TA, D], F32, "Internal")]
    NST = NH * DV + NH
    csrc = nc.dram_tensor("csrc", [128, NST], F32)
    cdst = nc.dram_tensor("cdst", [256, NST], F32)
    hsrc = nc.dram_tensor("hsrc", [128, 8 * GW], F32)
    hdst = nc.dram_tensor("hdst", [256, 8 * GW], F32)
    PAIRS = [[2 * i, 2 * i + 1] for i in range(ncores // 2)]

    ges = ExitStack()
    with ges:
        S = Sched(nc, ges)

        uid = [0]

        def sb(es, name, shape, dtype):
            uid[0] += 1
            return es.enter_context(nc.sbuf_tensor("%s_u%d" % (name, uid[0]), list(shape), dtype))

        par = sb(ges, "par", [128, NPAR], F32)
        cst = sb(ges, "cst", [128, NCONST], F32)
        identb = sb(ges, "identb", [128, 128], BF16)
        MODL = sb(ges, "modl", [128, 6, 8, 2], F32)
        ctx = [sb(ges, "ctx%d" % k, [128, TC], F32) for k in range(8)]
        EG = sb(ges, "eg", [128, NCH, 8], F32)
        halo = sb(ges, "halo", [128, 8, GW], F32)
        crecv = sb(ges, "crecv", [128, NST], F32)
        osel, _ = PAR_OFF["sel"]
        sel0 = par[:, osel:osel + 1]
        sel1 = par[:, osel + 1:osel + 2]

        def exchange(src_d, dst_d, n, load_src, out_ap, tag):
            with ExitStack() as es:
                two = sb(es, "xch2", [128, 2, n], F32)
                tmpx = sb(es, "xcht", [128, n], F32)
                load_src(src_d.ap())
                S.coll([src_d.ap().opt()], [dst_d.ap().opt()], PAIRS, r=[tag + "src"], w=[tag + "dst"])
                S.dma("sp", two[:], dst_d.ap().rearrange("(r p) n -> p r n", p=128), r=[tag + "dst"], w=["xch2"])
                S.op("dve", "tensor_scalar", out=tmpx[:], in0=two[:, 0, :], scalar1=sel0, scalar2=None,
                     op0=ALU.mult, r=["xch2", "par"], w=["xcht"])
                S.op("dve", "scalar_tensor_tensor", out=out_ap, in0=two[:, 1, :], scalar=sel1, op0=ALU.mult,
                     in1=tmpx[:], op1=ALU.add, r=["xch2", "xcht", "par"], w=[tag + "recv"])
                S.flush()
        psf = ges.enter_context(nc.psum_tensor("psf", [128, 7, 512], F32))
        psb = ges.enter_context(nc.psum_tensor("psb", [128, 1024], BF16))

        def PS(b):
            return psf[:, b, :]

        def pcol(name, idx=0):
            o, n = PAR_OFF[name]
            return o + idx

        onesb128 = sb(ges, "onesb128", [128, 128], BF16)
        ones_f = onesb128[:]

        S.dma("sp", par[:], par_in[:, :], w=["par"])
        S.dma("sp", cst[:], const_in[:, :], w=["cst"])
        for k in range(8):
            S.dma("sp", ctx[k][:], ctxT_in[k], w=[("ctx", k)])
        S.op("dve", "tensor_copy", out=identb[:], in_=cst[:, C_ID:C_ID + 128], r=["cst"], w=["identb"])
        S.op("dve", "tensor_copy", out=onesb128[:], in_=cst[:, C_ONES:C_ONES + 128], r=["cst"], w=["onesb128"])
        def cast_rows(dst, src, nrows, bg):
            for r0 in range(0, nrows, 128):
                S.dma("pool", dst[r0:r0 + 128, :], src[r0:r0 + 128, :], bg=bg)

        fgroups = []
        for kind, l in plan:
            if kind == "F" and ("f", l) not in fgroups:
                fgroups.append(("f", l))
        joined = set()

        def need_weights(g):
            if g[0] == "f" and g not in joined:
                issue_bg_casts()
                joined.add(g)
                S.join_bg(g)

        S.flush()
        bg_done = [False]

        def issue_bg_casts():
            if bg_done[0]:
                return
            bg_done[0] = True
            for g in fgroups:
                key = ("f_w_up", g[1])
                cast_rows(Wb[key], Wi[key], Wi[key].shape[0], g)

        def mk_tmp(es, n):
            t = {nm: sb(es, nm, [128, n], F32) for nm in ("std", "nt0", "nt1")}
            t["sq0"] = sb(es, "sq0", [128, n], BF16)
            t["sq1"] = sb(es, "sq1", [128, n], BF16)
            return t

        def norm_mod(xs, xregs, n, which, s_gs, s_sh, hx, tmp):
            pieces = [(c0, min(512, n - c0)) for c0 in range(0, n, 512)]
            for k in range(8):
                sq = tmp["sq%d" % (k % 2)]
                S.op("act", "activation", out=sq[:, 0:n], in_=xs[k], func=AF.Square,
                     r=[xregs[k]], w=[("sq", k % 2)])
                for pi, (c0, cn) in enumerate(pieces):
                    S.op("pe", "matmul", PS(6 - pi)[:, 0:cn], lhsT=ones_f, rhs=sq[:, c0:c0 + cn],
                         start=(k == 0), stop=(k == 7), r=[("sq", k % 2), "cst"], w=[("ps", 6 - pi)])
            std = tmp["std"]
            for pi, (c0, cn) in enumerate(pieces):
                S.op("act", "activation", out=std[:, c0:c0 + cn], in_=PS(6 - pi)[:, 0:cn], func=AF.Sqrt,
                     scale=1.0 / D, bias=EPS, r=[("ps", 6 - pi)], w=["std"])
            S.op("dve", "reciprocal", out=std[:, 0:n], in_=std[:, 0:n], r=["std"], w=["std"])
            for k in range(8):
                tt = tmp["nt%d" % (k % 2)]
                S.op("dve", "tensor_tensor", out=tt[:, 0:n], in0=xs[k], in1=std[:, 0:n], op=ALU.mult,
                     r=[xregs[k], "std"], w=[("nt", k % 2)])
                S.op("act", "activation", out=hx[k][:, 0:n], in_=tt[:, 0:n], func=AF.Identity,
                     scale=MODL[:, s_gs, k, which:which + 1], bias=MODL[:, s_sh, k, which:which + 1],
                     r=[("nt", k % 2), "mod"], w=[("hx", k)])

        def resid(out_ap, ps_ap, gcol, x_ap, r, w):
            S.op("dve", "scalar_tensor_tensor", out=out_ap, in0=ps_ap, scalar=gcol, op0=ALU.mult,
                 in1=x_ap, op1=ALU.add, r=r, w=w)

        def v3(ap, rowlen):
            return ap.rearrange("p (a b) -> p a b", b=rowlen)

        def phase_mod(l):
            with ExitStack() as es:
                scb = sb(es, "scb", [128, 8, 2], BF16)
                wm = [sb(es, "wm%d" % i, [128, 8, D], BF16) for i in range(6)]
                t1 = sb(es, "modt1", [128, 8, 2], F32)
                o, _ = PAR_OFF["cc"]
                S.op("act", "activation", out=scb[:].rearrange("p k w -> p (k w)"), in_=par[:, o:o + 16],
                     func=AF.Silu, r=["par"], w=["scb"])
                for s in range(6):
                    S.dma("pool", wm[s][:], Wi[("w_mod", l)][:, s * D:(s + 1) * D].rearrange("(k p) n -> p k n", p=128),
                          w=[("wm", s)])
                for s in range(6):
                    wt = wm[s]
                    pst = PS(s % 2)
                    for m in range(8):
                        for k in range(8):
                            S.op("pe", "matmul", pst[:, m * 2:m * 2 + 2], lhsT=wt[:, k, m * 128:(m + 1) * 128],
                                 rhs=scb[:, k, :], start=(k == 0), stop=(k == 7),
                                 r=[("wm", s), "scb"], w=[("ps", s % 2)])
                    ob, _ = PAR_OFF["bmod"]
                    bcol = par[:, ob + l * 48 + s * 8: ob + l * 48 + s * 8 + 8]
                    S.op("dve", "tensor_tensor", out=MODL[:, s, :, :],
                         in0=pst[:, 0:16].rearrange("p (m w) -> p m w", w=2),
                         in1=bcol.unsqueeze(2).to_broadcast([128, 8, 2]), op=ALU.add,
                         r=[("ps", s % 2), "par"], w=["mod"])
                for s, gname in ((1, "gmix"), (4, "gffn")):
                    og, _ = PAR_OFF[gname]
                    gcol = par[:, og + l * 8: og + l * 8 + 8]
                    S.op("dve", "tensor_scalar", out=t1[:], in0=MODL[:, s, :, :], scalar1=1.0, scalar2=None,
                         op0=ALU.add, r=["mod"], w=["modt1"])
                    S.op("dve", "tensor_tensor", out=MODL[:, s, :, :], in0=t1[:],
                         in1=gcol.unsqueeze(2).to_broadcast([128, 8, 2]), op=ALU.mult,
                         r=["modt1", "par"], w=["mod"])
                S.flush()

        def phase_B(l, src, dst, do_ctx):
            j = l // 2
            with ExitStack() as es:
                xw = sb(es, "xw", [128, 8, 512], F32)
                xo = sb(es, "xo", [128, 8, 512], F32)
                hx = [sb(es, "hx%d" % k, [128, 512], BF16) for k in range(8)]
                tmp = mk_tmp(es, 512)
                win = sb(es, "win", [128, 8, 3 * D], BF16)
                wout = sb(es, "wout", [128, 8, D], BF16)
                xv_sb = [sb(es, "xv%d" % i, [128, 512], F32) for i in range(2)]
                m_sb = [sb(es, "m%d" % i, [128, 512], F32) for i in range(2)]
                acc = [sb(es, "acc%d" % i, [128, 512], F32) for i in range(2)]
                z = [sb(es, "z%d" % k, [128, 512], BF16) for k in range(8)]
                for pc in range(3):
                    S.dma("pool", win[:, :, pc * D:(pc + 1) * D],
                          Wi[("b_w_in", j)][:, pc * D:(pc + 1) * D].rearrange("(k p) n -> p k n", p=128), w=[("win", pc)])
                S.dma("pool", wout[:], Wi[("b_w_out", j)].rearrange("(k p) n -> p k n", p=128), w=["wout"])
                def tile(xs, xregs, n, rowlen, which, outs, oregs):
                    ow, _ = PAR_OFF["bwc_c" if which == 1 else "bwc_l"]
                    norm_mod(xs, xregs, n, which, 1, 0, hx, tmp)
                    for c in range(8):
                        pr = c % 2
                        pb, pc_, pv = PS(3 * pr), PS(3 * pr + 1), PS(3 * pr + 2)
                        for gi, pt in enumerate((pb, pc_, pv)):
                            for k in range(8):
                                S.op("pe", "matmul", pt[:, 0:n],
                                     lhsT=win[:, k, gi * D + c * 128: gi * D + (c + 1) * 128], rhs=hx[k][:, 0:n],
                                     start=(k == 0), stop=(k == 7),
                                     r=[("win", gi), ("hx", k)], w=[("ps", 3 * pr + gi)])
                        S.op("act", "activation", out=xv_sb[pr][:, 0:n], in_=pv[:, 0:n], func=AF.Identity,
                             r=[("ps", 3 * pr + 2)], w=[("xv", pr)])
                        S.op("dve", "tensor_tensor", out=m_sb[pr][:, 0:n], in0=pc_[:, 0:n], in1=xv_sb[pr][:, 0:n],
                             op=ALU.mult, r=[("ps", 3 * pr + 1), ("xv", pr)], w=[("m", pr)])
                        w0 = par[:, ow + (j * 3 + 0) * 8 + c: ow + (j * 3 + 0) * 8 + c + 1]
                        w1 = par[:, ow + (j * 3 + 1) * 8 + c: ow + (j * 3 + 1) * 8 + c + 1]
                        w2 = par[:, ow + (j * 3 + 2) * 8 + c: ow + (j * 3 + 2) * 8 + c + 1]
                        S.op("act", "activation", out=acc[pr][:, 0:n], in_=m_sb[pr][:, 0:n], func=AF.Identity,
                             scale=w1, r=[("m", pr), "par"], w=[("acc", pr)])
                        a3 = v3(acc[pr][:, 0:n], rowlen)
                        m3 = v3(m_sb[pr][:, 0:n], rowlen)
                        S.op("dve", "scalar_tensor_tensor", out=a3[:, :, 1:rowlen], in0=m3[:, :, 0:rowlen - 1],
                             scalar=w0, op0=ALU.mult, in1=a3[:, :, 1:rowlen], op1=ALU.add,
                             r=[("m", pr), ("acc", pr), "par"], w=[("acc", pr)])
                        S.op("dve", "scalar_tensor_tensor", out=a3[:, :, 0:rowlen - 1], in0=m3[:, :, 1:rowlen],
                             scalar=w2, op0=ALU.mult, in1=a3[:, :, 0:rowlen - 1], op1=ALU.add,
                             r=[("m", pr), ("acc", pr), "par"], w=[("acc", pr)])
                        S.op("dve", "tensor_tensor", out=z[c][:, 0:n], in0=pb[:, 0:n], in1=acc[pr][:, 0:n],
                             op=ALU.mult, r=[("ps", 3 * pr), ("acc", pr)], w=[("z", c)])
                    for mo in range(8):
                        pt = PS(mo % 6)
                        for c in range(8):
                            S.op("pe", "matmul", pt[:, 0:n], lhsT=wout[:, c, mo * 128:(mo + 1) * 128],
                                 rhs=z[c][:, 0:n], start=(c == 0), stop=(c == 7),
                                 r=["wout", ("z", c)], w=[("ps", mo % 6)])
                        resid(outs[mo], pt[:, 0:n], MODL[:, 2, mo, which:which + 1], xs[mo],
                              r=[("ps", mo % 6), "mod", xregs[mo]], w=[oregs[mo]])

                if do_ctx:
                    tile([ctx[k][:, :] for k in range(8)], [("ctx", k) for k in range(8)], TC, TC, 1,
                         [ctx[k][:, :] for k in range(8)], [("ctx", k) for k in range(8)])
                for i in range(T // 512):
                    t0 = i * 512
                    S.dma("sp", xw[:], src[:, :, t0:t0 + 512].rearrange("k p t -> p k t"), w=["xw"])
                    tile([xw[:, k, :] for k in range(8)], ["xw"] * 8, 512, GW, 0,
                         [xo[:, k, :] for k in range(8)], ["xo"] * 8)
                    S.dma("sp", dst[:, :, t0:t0 + 512].rearrange("k p t -> p k t"), xo[:], r=["xo"])
                S.flush()

        def phase_F(l, src, dst, do_ctx):
            with ExitStack() as es:
                NW = 640
                xw = sb(es, "xw", [128, 8, NW], F32)
                xo = sb(es, "xo", [128, 8, 512], F32)
                hx = [sb(es, "hx%d" % k, [128, NW], BF16) for k in range(8)]
                tmp = mk_tmp(es, NW)
                wg = [sb(es, "wg%d" % i, [128, 8, 256], BF16) for i in range(2)]
                wv = [sb(es, "wv%d" % i, [128, 8, 256], BF16) for i in range(2)]
                wdn = sb(es, "wdn", [128, NJ, D], BF16)
                acc = [sb(es, "acc%d" % i, [128, 512], F32) for i in range(2)]
                sil = [sb(es, "sil%d" % i, [128, 512], F32) for i in range(2)]
                act = [sb(es, "act%d" % jj, [128, 512], BF16) for jj in range(NJ)]
                for jb in range(0, NJ, 11):
                    S.dma("pool", wdn[:, jb:jb + 11, :],
                          Wi[("f_w_down", l)][jb * 128:(jb + 11) * 128, :].rearrange("(j p) n -> p j n", p=128),
                          w=[("wdn", jb)])
                obc, _ = PAR_OFF["fbc"]

                def tile(xs, xregs, nw, co, n, shift, which, outs, oregs):
                    owc, _ = PAR_OFF["fwc_c" if which == 1 else "fwc_l"]
                    lo_ok = co >= shift
                    hi_ok = nw >= co + n + shift
                    norm_mod(xs, xregs, nw, which, 4, 3, hx, tmp)
                    gp = [(c0, min(512, nw - c0)) for c0 in range(0, nw, 512)]
                    for jj in range(NJ):
                        pr = jj % 2
                        if jj % 2 == 0:
                            wb = (jj // 2) % 2
                            S.dma("sp", wg[wb][:], Wb[("f_w_up", l)][:, jj * 128: jj * 128 + 256].rearrange(
                                "(k p) n -> p k n", p=128), w=[("wg", wb)])
                            S.dma("sp", wv[wb][:], Wb[("f_w_up", l)][:, DFF + jj * 128: DFF + jj * 128 + 256].rearrange(
                                "(k p) n -> p k n", p=128), w=[("wv", wb)])
                        wb = (jj // 2) % 2
                        wo_ = (jj % 2) * 128
                        gflat = psf[:, 2 * pr:2 * pr + 2, :].rearrange("p b c -> p (b c)")
                        pv = PS(4 + pr)
                        for pi, (c0, cn) in enumerate(gp):
                            for k in range(8):
                                S.op("pe", "matmul", PS(2 * pr + pi)[:, 0:cn], lhsT=wg[wb][:, k, wo_:wo_ + 128],
                                     rhs=hx[k][:, c0:c0 + cn], start=(k == 0), stop=(k == 7),
                                     r=[("wg", wb), ("hx", k)], w=[("ps", 2 * pr + pi)])
                        for k in range(8):
                            S.op("pe", "matmul", pv[:, 0:n], lhsT=wv[wb][:, k, wo_:wo_ + 128],
                                 rhs=hx[k][:, co:co + n], start=(k == 0), stop=(k == 7),
                                 r=[("wv", wb), ("hx", k)], w=[("ps", 4 + pr)])
                        greg = [("ps", 2 * pr), ("ps", 2 * pr + 1)]
                        w0 = par[:, owc + (l * 3 + 0) * NJ + jj: owc + (l * 3 + 0) * NJ + jj + 1]
                        w1 = par[:, owc + (l * 3 + 1) * NJ + jj: owc + (l * 3 + 1) * NJ + jj + 1]
                        w2 = par[:, owc + (l * 3 + 2) * NJ + jj: owc + (l * 3 + 2) * NJ + jj + 1]
                        bc = par[:, obc + l * NJ + jj: obc + l * NJ + jj + 1]
                        a = acc[pr]
                        S.op("dve", "tensor_scalar", out=a[:, 0:n], in0=gflat[:, co:co + n], scalar1=w1, scalar2=None,
                             op0=ALU.mult, r=greg + ["par"], w=[("acc", pr)])
                        if lo_ok:
                            o0, o1, s0 = 0, n, co - shift
                        else:
                            o0, o1, s0 = shift, n, co
                        S.op("dve", "scalar_tensor_tensor", out=a[:, o0:o1], in0=gflat[:, s0:s0 + (o1 - o0)],
                             scalar=w0, op0=ALU.mult, in1=a[:, o0:o1], op1=ALU.add,
                             r=greg + ["par", ("acc", pr)], w=[("acc", pr)])
                        if hi_ok:
                            o0, o1 = 0, n
                        else:
                            o0, o1 = 0, n - shift
                        S.op("dve", "scalar_tensor_tensor", out=a[:, o0:o1],
                             in0=gflat[:, co + shift:co + shift + (o1 - o0)],
                             scalar=w2, op0=ALU.mult, in1=a[:, o0:o1], op1=ALU.add,
                             r=greg + ["par", ("acc", pr)], w=[("acc", pr)])
                        S.op("act", "activation", out=sil[pr][:, 0:n], in_=a[:, 0:n], func=AF.Silu, bias=bc,
                             r=[("acc", pr), "par"], w=[("sil", pr)])
                        S.op("dve", "tensor_tensor", out=act[jj][:, 0:n], in0=pv[:, 0:n], in1=sil[pr][:, 0:n],
                             op=ALU.mult, r=[("ps", 4 + pr), ("sil", pr)], w=[("act", jj)])
                    for mo in range(8):
                        pt = PS(mo % 4)
                        for jj in range(NJ):
                            S.op("pe", "matmul", pt[:, 0:n], lhsT=wdn[:, jj, mo * 128:(mo + 1) * 128],
                                 rhs=act[jj][:, 0:n], start=(jj == 0), stop=(jj == NJ - 1),
                                 r=[("wdn", 0), ("wdn", 11), ("act", jj)], w=[("ps", mo % 4)])
                        resid(outs[mo], pt[:, 0:n], MODL[:, 5, mo, which:which + 1], xs[mo][:, co:co + n],
                              r=[("ps", mo % 4), "mod", xregs[mo]], w=[oregs[mo]])

                if do_ctx:
                    tile([ctx[k][:, :] for k in range(8)], [("ctx", k) for k in range(8)], TC, 0, TC, 1, 1,
                         [ctx[k][:, :] for k in range(8)], [("ctx", k) for k in range(8)])
                for i in range(NROW // 8):
                    r0 = i * 8
                    wlo, whi = max(0, r0 - 1), min(NROW, r0 + 9)
                    nw = (whi - wlo) * GW
                    co = (r0 - wlo) * GW
                    S.dma("sp", xw[:, :, 0:nw], src[:, :, wlo * GW:whi * GW].rearrange("k p t -> p k t"), w=["xw"])
                    if r0 + 9 > NROW:
                        S.op("act", "activation", out=xw[:, :, nw:nw + GW], in_=halo[:], func=AF.Identity,
                             r=["hrecv"], w=["xw"])
                        nw += GW
                    tile([xw[:, k, 0:nw] for k in range(8)], ["xw"] * 8, nw, co, 512, GW, 0,
                         [xo[:, k, :] for k in range(8)], ["xo"] * 8)
                    S.dma("sp", dst[:, :, r0 * GW:r0 * GW + 512].rearrange("k p t -> p k t"), xo[:], r=["xo"])
                S.flush()

        def phase_A1(l, src, emit_ctx):
            j = l // 2
            with ExitStack() as es:
                xw = sb(es, "xw", [128, 8, 512], F32)
                hx = [sb(es, "hx%d" % k, [128, 512], BF16) for k in range(8)]
                tmp = mk_tmp(es, 512)
                win = sb(es, "win", [128, 8, APROJ], BF16)
                qkb = sb(es, "qkb", [128, 8, 512], BF16)
                kt_sb = [sb(es, "kt%d" % i, [128, NH * DK], F32) for i in range(2)]
                va_sb = [sb(es, "va%d" % i, [128, NH * DV], BF16) for i in range(2)]
                so_sb = [sb(es, "so%d" % i, [128, D], F32) for i in range(2)]
                g_sb = [sb(es, "g%d" % i, [128, 16], F32) for i in range(2)]
                sp_sb = [sb(es, "sp%d" % i, [128, 8], F32) for i in range(2)]
                u_sb = [sb(es, "u%d" % i, [128, 8], F32) for i in range(2)]
                sc_sb = [sb(es, "sc%d" % i, [128, 16], F32) for i in range(2)]
                for pc, (c0, c1) in enumerate(((0, 1024), (1024, 2048), (2048, APROJ))):
                    S.dma("pool", win[:, :, c0:c1], Wi[("a_w_in", j)][:, c0:c1].rearrange("(k p) n -> p k n", p=128),
                          w=[("win", pc)])
                wreg = [("win", 0), ("win", 1), ("win", 2)]
                obg, _ = PAR_OFF["bgate"]
                bg = par[:, obg + j * 16: obg + j * 16 + 16]
                stc = [0]
                pendB = []

                def tile(xs, xregs, n, which, ta0, do_o):
                    tr0, tr1 = (C_TRIF, C_TRIB) if which == 1 else (C_TRL0, C_TRL1)
                    norm_mod(xs, xregs, n, which, 1, 0, hx, tmp)
                    for m in range(8):
                        pt = PS(m % 2)
                        for k in range(8):
                            S.op("pe", "matmul", pt[:, 0:n], lhsT=win[:, k, m * 128:(m + 1) * 128], rhs=hx[k][:, 0:n],
                                 start=(k == 0), stop=(k == 7), r=wreg + [("hx", k)], w=[("ps", m % 2)])
                        S.op("act", "activation", out=qkb[:, m, 0:n], in_=pt[:, 0:n], func=AF.Identity,
                             scale=(1.0 if m < 4 else DK ** -0.5), r=[("ps", m % 2)], w=["qkb"])
                    S.dma("sp", QK[:, :, ta0:ta0 + n].rearrange("k p t -> p k t"), qkb[:, :, 0:n], r=["qkb"])
                    for s in range(n // 128):
                        st = stc[0] % 2
                        stc[0] += 1
                        tsl = slice(s * 128, (s + 1) * 128)
                        row0 = ta0 + s * 128

                        def proj(bank, c0, cn):
                            for k in range(8):
                                S.op("pe", "matmul", PS(bank)[:, 0:cn], lhsT=hx[k][:, tsl], rhs=win[:, k, c0:c0 + cn],
                                     start=(k == 0), stop=(k == 7), r=wreg + [("hx", k)], w=[("ps", bank)])
                        proj(2, 512, 512)
                        S.op("act", "activation", out=kt_sb[st][:], in_=PS(2)[:, :], func=AF.Identity,
                             scale=DK ** -0.5, r=[("ps", 2)], w=[("kt", st)])
                        S.dma("sp", KT[row0:row0 + 128, :], kt_sb[st][:], r=[("kt", st)])
                        for pi in range(2):
                            proj(3 + pi, 1024 + pi * 512, 512)
                            S.op("act", "activation", out=va_sb[st][:, pi * 512:(pi + 1) * 512], in_=PS(3 + pi)[:, :],
                                 func=AF.Identity, r=[("ps", 3 + pi)], w=[("va", st)])
                        S.dma("sp", VA[row0:row0 + 128, :], va_sb[st][:], r=[("va", st)])
                        if do_o:
                            for pi in range(2):
                                proj(2 + 2 * pi, 2048 + pi * 512, 512)
                                S.op("act", "activation", out=so_sb[st][:, pi * 512:(pi + 1) * 512],
                                     in_=PS(2 + 2 * pi)[:, :], func=AF.Sigmoid, r=[("ps", 2 + 2 * pi)],
                                     w=[("so", st)])
                            S.dma("sp", SO[row0:row0 + 128, :], so_sb[st][:], r=[("so", st)])
                        proj(5, 3072, 16)
                        while pendB:
                            pendB.pop(0)()
                        S.op("dve", "tensor_tensor", out=g_sb[st][:], in0=PS(5)[:, 0:16], in1=bg, op=ALU.add,
                             r=[("ps", 5), "par"], w=[("g", st)])
                        S.op("act", "activation", out=sp_sb[st][:], in_=g_sb[st][:, 8:16], func=AF.Exp, scale=-1.0,
                             r=[("g", st)], w=[("sp", st)])
                        S.op("act", "activation", out=sp_sb[st][:], in_=sp_sb[st][:], func=AF.Ln, bias=1.0, scale=1.0,
                             r=[("sp", st)], w=[("sp", st)])
                        def partB(st=st, row0=row0, tr0=tr0, tr1=tr1):
                            S.op("pe", "matmul", PS(6)[:, 32:36], lhsT=cst[:, tr0:tr0 + 128], rhs=sp_sb[st][:, 0:4],
                                 start=True, stop=True, r=[("sp", st), "cst"], w=[("ps", 6)])
                            S.op("pe", "matmul", PS(6)[:, 36:40], lhsT=cst[:, tr1:tr1 + 128], rhs=sp_sb[st][:, 4:8],
                                 start=True, stop=True, r=[("sp", st), "cst"], w=[("ps", 6)])
                            S.op("pe", "matmul", PS(6)[:, 64:72], lhsT=cst[:, C_BLK0:C_BLK0 + 128], rhs=sp_sb[st][:, 0:8],
                                 start=True, stop=True, r=[("sp", st), "cst"], w=[("ps", 6)])
                            S.op("pe", "matmul", PS(6)[:, 72:80], lhsT=cst[:, C_BLK1:C_BLK1 + 128], rhs=sp_sb[st][:, 0:8],
                                 start=True, stop=True, r=[("sp", st), "cst"], w=[("ps", 6)])
                            S.op("dve", "tensor_tensor", out=u_sb[st][:], in0=PS(6)[:, 32:40], in1=g_sb[st][:, 0:8],
                                 op=ALU.add, r=[("ps", 6), ("g", st)], w=[("u", st)])
                            S.op("act", "activation", out=sc_sb[st][:, 0:8], in_=u_sb[st][:], func=AF.Exp,
                                 r=[("u", st)], w=[("sc", st)])
                            S.op("act", "activation", out=sc_sb[st][:, 8:16], in_=PS(6)[:, 32:40], func=AF.Exp,
                                 r=[("ps", 6)], w=[("sc", st)])
                            ch0 = row0 // CH
                            S.op("act", "activation", out=EG[:, ch0:ch0 + 2, :].rearrange("p c g -> p (c g)"),
                                 in_=PS(6)[:, 64:80], func=AF.Exp, scale=-1.0, r=[("ps", 6)], w=["eg"])
                            S.dma("sp", SC[row0:row0 + 128, :], sc_sb[st][:], r=[("sc", st)])
                        pendB.append(partB)
                    while pendB:
                        pendB.pop(0)()

                tile([ctx[k][:, :] for k in range(8)], [("ctx", k) for k in range(8)], TC, 1, 0, emit_ctx)
                for i in range(T // 512):
                    t0 = i * 512
                    S.dma("sp", xw[:], src[:, :, t0:t0 + 512].rearrange("k p t -> p k t"), w=["xw"])
                    tile([xw[:, k, :] for k in range(8)], ["xw"] * 8, 512, 0, TC + t0, True)
                S.flush()

        def phase_scan(d, emit_ctx):
            with ExitStack() as es:
                qk_t = [sb(es, "qkt%d" % i, [128, 8, 512], BF16) for i in range(2)]
                kt_t = [sb(es, "ktt%d" % i, [64, 8, NH * DK], F32) for i in range(2)]
                va_t = [sb(es, "vat%d" % i, [64, 8, NH * DV], BF16) for i in range(2)]
                sc_t = [sb(es, "sct%d" % i, [64, 8, 16], F32) for i in range(2)]
                hout = [sb(es, "hout%d" % i, [64, 8, D], F32) for i in range(2)]
                Ct = sb(es, "Ct", [128, NH, DV], F32)
                nst = sb(es, "nst", [128, NH], F32)
                Cb = [sb(es, "Cb%d" % i, [128, NH, DV], BF16) for i in range(2)]
                nb_ = [sb(es, "nb%d" % i, [128, NH], BF16) for i in range(2)]
                ntmp = sb(es, "ntmp", [128, NH], F32)
                pT = [sb(es, "pT%d" % i, [64, NH, 64], BF16) for i in range(2)]
                ks = [sb(es, "ks%d" % i, [64, NH * DK], BF16) for i in range(2)]
                absb = sb(es, "absb", [64, NH], F32)
                den = sb(es, "den", [64, NH], F32)
                onesb = sb(es, "onesb", [128, 2], BF16)
                S.op("dve", "memset", Ct[:], 0.0, w=[("Ct", h) for h in range(NH)])
                S.op("dve", "memset", nst[:], 0.0, w=["nst"])
                S.op("dve", "memset", Cb[0][:], 0.0, w=[("Cb", 0, h) for h in range(NH)])
                S.op("dve", "memset", nb_[0][:], 0.0, w=[("nb", 0)])
                S.op("dve", "memset", onesb[:], 1.0, w=["onesb"])
                issue_bg_casts()
                mc_ctx = C_MF if d == 0 else C_MB
                mc_lat = C_ML0 if d == 0 else C_ML1
                blocks = [(0, 4, True)] + [(TC + i * 512, 8, False) for i in range(T // 512)]
                if d == 1:
                    blocks = ([blocks[0]] if emit_ctx else []) + blocks[1:][::-1]
                chunks = []
                for bi, (ta0, nb, is_ctx) in enumerate(blocks):
                    order = list(range(nb)) if d == 0 else list(range(nb))[::-1]
                    for ci in order:
                        chunks.append(dict(bi=bi, bf=bi % 2, ta0=ta0, nb=nb, is_ctx=is_ctx, ci=ci,
                                           chg=ta0 // CH + ci, emit=((not is_ctx) or emit_ctx),
                                           first=(ci == order[0]), last=(ci == order[-1])))
                ones4 = cst[:, C_ONES:C_ONES + NH]
                SB = (0, 6)
                st = dict(prev=None, injected=(d == 0))

                def load_block(bi):
                    if bi >= len(blocks):
                        return
                    ta0, nb, _ = blocks[bi]
                    bf, ntok = bi % 2, nb * 64
                    if True:
                        S.dma("sp", qk_t[bf][:, :, 0:ntok], QK[:, :, ta0:ta0 + ntok].rearrange("k p t -> p k t"),
                              w=[("qkt", bf)])
                        S.dma("sp", kt_t[bf][:, 0:nb, :], KT[ta0:ta0 + ntok, :].rearrange("(c s) d -> s c d", s=64),
                              w=[("ktt", bf)])
                        S.dma("sp", va_t[bf][:, 0:nb, :], VA[ta0:ta0 + ntok, :].rearrange("(c s) d -> s c d", s=64),
                              w=[("vat", bf)])
                        S.dma("sp", sc_t[bf][:, 0:nb, :], SC[ta0:ta0 + ntok, :].rearrange("(c s) d -> s c d", s=64),
                              w=[("sct", bf)])

                def P1(ch, q):
                    bf, ci = ch["bf"], ch["ci"]
                    tsl = slice(ci * 64, (ci + 1) * 64)
                    if ch["emit"]:
                        for h in range(NH):
                            S.op("pe", "matmul", PS(SB[q % 2])[0:64, h * 64:(h + 1) * 64], lhsT=qk_t[bf][:, 4 + h, tsl],
                                 rhs=qk_t[bf][:, h, tsl], start=True, stop=True,
                                 r=[("qkt", bf)], w=[("ps", SB[q % 2])])
                    S.op("dve", "tensor_tensor", out=ks[q % 2][:].rearrange("p (h d) -> p h d", h=NH),
                         in0=kt_t[bf][:, ci, :].rearrange("p (h d) -> p h d", h=NH),
                         in1=sc_t[bf][:, ci, d * 4:d * 4 + 4].unsqueeze(2).to_broadcast([64, NH, DK]), op=ALU.mult,
                         r=[("ktt", bf), ("sct", bf)], w=[("ks", q % 2)])

                def P2(ch, q):
                    bf, ci = ch["bf"], ch["ci"]
                    mcol = mc_ctx if ch["is_ctx"] else mc_lat
                    mask = cst[0:64, mcol:mcol + 64]
                    if ch["emit"]:
                        for h in range(NH):
                            S.op("dve", "scalar_tensor_tensor", out=pT[q % 2][:, h, :],
                                 in0=PS(SB[q % 2])[0:64, h * 64:(h + 1) * 64],
                                 scalar=sc_t[bf][:, ci, d * 4 + h:d * 4 + h + 1],
                                 op0=ALU.mult, in1=mask, op1=ALU.mult,
                                 r=[("ps", SB[q % 2]), ("sct", bf), "cst"], w=[("pT", q % 2, h)])
                    for h in range(NH):
                        pu = PS(4 + h // 2)[:, (h % 2) * 256:(h % 2) * 256 + 256]
                        S.op("pe", "matmul", pu, lhsT=ks[q % 2][:, h * DK:(h + 1) * DK],
                             rhs=va_t[bf][:, ci, h * DV:(h + 1) * DV], start=True, stop=True,
                             r=[("ks", q % 2), ("vat", bf)], w=[("ps", 4 + h // 2)])
                    for h in range(NH):
                        S.op("pe", "matmul", PS(3)[:, 8 + h:9 + h], lhsT=ks[q % 2][:, h * DK:(h + 1) * DK],
                             rhs=onesb[0:64, 0:1], start=True, stop=True, r=[("ks", q % 2), "onesb"], w=[("psn",)])

                def P3(ch, q):
                    bf, ci, chg = ch["bf"], ch["ci"], ch["chg"]
                    tsl = slice(ci * 64, (ci + 1) * 64)
                    cur, nxt = q % 2, (q + 1) % 2
                    if not ch["is_ctx"] and not st["injected"]:
                        st["injected"] = True
                        st["prev"] = "ones"
                        S.op("dve", "tensor_copy", out=Ct[:].rearrange("p h v -> p (h v)"), in_=crecv[:, 0:NH * DV],
                             r=["crecv"], w=[("Ct", h) for h in range(NH)])
                        S.op("dve", "tensor_copy", out=nst[:], in_=crecv[:, NH * DV:NST], r=["crecv"], w=["nst"])
                        S.op("act", "activation", out=Cb[cur][:].rearrange("p h v -> p (h v)"),
                             in_=crecv[:, 0:NH * DV], func=AF.Identity, r=["crecv"], w=[("Cb", cur, h) for h in range(NH)])
                        S.op("act", "activation", out=nb_[cur][:], in_=crecv[:, NH * DV:NST], func=AF.Identity,
                             r=["crecv"], w=[("nb", cur)])
                    use_ones = (st["prev"] == "ones")
                    pgl = chg if (st["prev"] is None or use_ones) else st["prev"]
                    st["prev"] = chg
                    if ch["emit"]:
                        for h in range(NH):
                            pa = PS(1 + h // 2)[0:64, (h % 2) * 256:(h % 2) * 256 + 256]
                            S.op("pe", "matmul", pa, lhsT=pT[cur][:, h, :], rhs=va_t[bf][:, ci, h * DV:(h + 1) * DV],
                                 start=True, stop=False, r=[("pT", cur, h), ("vat", bf)], w=[("ps", 1 + h // 2)])
                            S.op("pe", "matmul", pa, lhsT=qk_t[bf][:, h, tsl], rhs=Cb[cur][:, h, :],
                                 start=False, stop=True, r=[("qkt", bf), ("Cb", cur, h)], w=[("ps", 1 + h // 2)])
                        for h in range(NH):
                            pbn = PS(3)[0:64, h:h + 1]
                            S.op("pe", "matmul", pbn, lhsT=pT[cur][:, h, :], rhs=onesb[0:64, 0:1],
                                 start=True, stop=False, r=[("pT", cur, h), "onesb"], w=[("psb4",)])
                            S.op("pe", "matmul", pbn, lhsT=qk_t[bf][:, h, tsl], rhs=nb_[cur][:, h:h + 1],
                                 start=False, stop=True, r=[("qkt", bf), ("nb", cur)], w=[("psb4",)])
                    egp = ones4 if use_ones else EG[:, pgl, d * 4:d * 4 + 4]
                    egc = EG[:, chg, d * 4:d * 4 + 4]
                    for h in range(NH):
                        pu = PS(4 + h // 2)[:, (h % 2) * 256:(h % 2) * 256 + 256]
                        S.op("dve", "scalar_tensor_tensor", out=Ct[:, h, :], in0=Ct[:, h, :],
                             scalar=(cst[:, C_ONES:C_ONES + 1] if use_ones else EG[:, pgl, d * 4 + h:d * 4 + h + 1]),
                             op0=ALU.mult, in1=pu, op1=ALU.add,
                             r=[("Ct", h), "eg", ("ps", 4 + h // 2)], w=[("Ct", h)])
                        S.op("act", "activation", out=Cb[nxt][:, h, :], in_=Ct[:, h, :], func=AF.Identity,
                             scale=EG[:, chg, d * 4 + h:d * 4 + h + 1], r=[("Ct", h), "eg"], w=[("Cb", nxt, h)])
                    S.op("dve", "tensor_tensor", out=ntmp[:], in0=nst[:], in1=egp, op=ALU.mult,
                         r=["nst", "eg"], w=["ntmp"])
                    S.op("dve", "tensor_tensor", out=nst[:], in0=PS(3)[:, 8:8 + NH], in1=ntmp[:], op=ALU.add,
                         r=[("psn",), "ntmp"], w=["nst"])
                    S.op("dve", "tensor_tensor", out=nb_[nxt][:], in0=nst[:], in1=egc, op=ALU.mult,
                         r=["nst", "eg"], w=[("nb", nxt)])

                def P4(ch, q):
                    bf, ci = ch["bf"], ch["ci"]
                    if not ch["emit"]:
                        return
                    ho = hout[ch["bi"] % 2]
                    S.op("act", "activation", out=absb[:], in_=PS(3)[0:64, 0:NH], func=AF.Abs,
                         r=[("psb4",)], w=["absb"])
                    S.op("dve", "tensor_tensor", out=den[:], in0=absb[:], in1=sc_t[bf][:, ci, 8 + d * 4:12 + d * 4],
                         op=ALU.max, r=["absb", ("sct", bf)], w=["den"])
                    S.op("dve", "reciprocal", out=den[:], in_=den[:], r=["den"], w=["den"])
                    for h in range(NH):
                        pa = PS(1 + h // 2)[0:64, (h % 2) * 256:(h % 2) * 256 + 256]
                        S.op("act", "activation", out=ho[:, ci, h * DV:(h + 1) * DV], in_=pa, func=AF.Identity,
                             scale=den[:, h:h + 1], r=[("ps", 1 + h // 2), "den"], w=[("hout", ch["bi"] % 2, h)])
                    if ch["last"]:
                        ta0, ntok, nb = ch["ta0"], ch["nb"] * 64, ch["nb"]
                        S.dma("sp", HD[d][ta0:ta0 + ntok, :].rearrange("(c s) v -> s c v", s=64), ho[:, 0:nb, :],
                              r=[("hout", ch["bi"] % 2, h) for h in range(NH)])

                load_block(0)
                load_block(1)
                P1(chunks[0], 0)
                P2(chunks[0], 0)
                for q, ch in enumerate(chunks):
                    P3(ch, q)
                    if q + 1 < len(chunks):
                        P1(chunks[q + 1], q + 1)
                    P4(ch, q)
                    if ch["last"]:
                        load_block(ch["bi"] + 2)
                    if q + 1 < len(chunks):
                        P2(chunks[q + 1], q + 1)
                if d == 0:
                    csend = sb(es, "csend", [128, NST], F32)
                    lastc = st["prev"]
                    for h in range(NH):
                        S.op("act", "activation", out=csend[:, h * DV:(h + 1) * DV], in_=Ct[:, h, :], func=AF.Identity,
                             scale=EG[:, lastc, d * 4 + h:d * 4 + h + 1], r=[("Ct", h), "eg"], w=["csend"])
                    S.op("dve", "tensor_tensor", out=csend[:, NH * DV:NST], in0=nst[:],
                         in1=EG[:, lastc, d * 4:d * 4 + 4], op=ALU.mult, r=["nst", "eg"], w=["csend"])
                    S.dma("sp", csrc.ap(), csend[:], r=["csend"], w=["csrc"])
                S.flush()
            if d == 0:
                exchange(csrc, cdst, NST, lambda a: None, crecv[:], "c")

        def phase_A5(l, src, dst, do_ctx):
            j = l // 2
            with ExitStack() as es:
                xw = sb(es, "xw", [128, 8, 512], F32)
                xo = sb(es, "xo", [128, 8, 512], F32)
                hf = [sb(es, "hf%d" % i, [128, D], F32) for i in range(2)]
                hb = [sb(es, "hb%d" % i, [128, D], F32) for i in range(2)]
                so = [sb(es, "so%d" % i, [128, D], F32) for i in range(2)]
                hs = sb(es, "hs", [128, D], F32)
                junk = sb(es, "junk", [128, DV], BF16)
                ss = sb(es, "ss", [128, NH], F32)
                yb = [sb(es, "yb%d" % i, [128, D], BF16) for i in range(2)]
                gain = sb(es, "gaint", [128, D], F32)
                yT = sb(es, "yT", [128, 8, 512], BF16)
                wout = sb(es, "wout", [128, 8, D], BF16)
                S.dma("sp", gain[:], gain_in[:, j, :], w=["gain"])
                S.dma("pool", wout[:], Wi[("a_w_out", j)].rearrange("(k p) n -> p k n", p=128), w=["wout"])
                stc = [0]

                def tile(xs, xregs, n, which, ta0, outs, oregs):
                    for s in range(n // 128):
                        st = stc[0] % 2
                        stc[0] += 1
                        row0 = ta0 + s * 128
                        S.dma("sp", hf[st][:], HD[0][row0:row0 + 128, :], w=[("hf", st)])
                        S.dma("sp", hb[st][:], HD[1][row0:row0 + 128, :], w=[("hb", st)])
                        S.dma("sp", so[st][:], SO[row0:row0 + 128, :], w=[("so", st)])
                        S.op("pool", "tensor_tensor", out=hs[:], in0=hf[st][:], in1=hb[st][:], op=ALU.add,
                             r=[("hf", st), ("hb", st)], w=["hs"])
                        for h in range(NH):
                            S.op("act", "activation", out=junk[:], in_=hs[:, h * DV:(h + 1) * DV], func=AF.Square,
                                 accum_out=ss[:, h:h + 1], r=["hs"], w=["junk", "ss"])
                        S.op("act", "activation", out=ss[:], in_=ss[:], func=AF.Sqrt, scale=1.0 / DV, bias=EPS,
                             r=["ss"], w=["ss"])
                        S.op("dve", "reciprocal", out=ss[:], in_=ss[:], r=["ss"], w=["ss"])
                        S.op("pool", "tensor_tensor", out=so[st][:], in0=so[st][:], in1=gain[:], op=ALU.mult,
                             r=[("so", st), "gain"], w=[("so", st)])
                        for h in range(NH):
                            S.op("dve", "scalar_tensor_tensor", out=yb[st][:, h * DV:(h + 1) * DV],
                                 in0=hs[:, h * DV:(h + 1) * DV], scalar=ss[:, h:h + 1], op0=ALU.mult,
                                 in1=so[st][:, h * DV:(h + 1) * DV], op1=ALU.mult,
                                 r=["hs", "ss", ("so", st)], w=[("yb", st)])
                        for c in range(8):
                            S.op("pe", "transpose", psb[:, c * 128:(c + 1) * 128], yb[st][:, c * 128:(c + 1) * 128],
                                 identb[:], r=[("yb", st), "identb"], w=["psb"])
                        S.op("act", "activation", out=yT[:, :, s * 128:(s + 1) * 128],
                             in_=psb[:].rearrange("p (c t) -> p c t", c=8), func=AF.Identity, r=["psb"], w=["yT"])
                    for mo in range(8):
                        pt = PS(mo % 4)
                        for c in range(8):
                            S.op("pe", "matmul", pt[:, 0:n], lhsT=wout[:, c, mo * 128:(mo + 1) * 128], rhs=yT[:, c, 0:n],
                                 start=(c == 0), stop=(c == 7), r=["wout", "yT"], w=[("ps", mo % 4)])
                        resid(outs[mo], pt[:, 0:n], MODL[:, 2, mo, which:which + 1], xs[mo],
                              r=[("ps", mo % 4), "mod", xregs[mo]], w=[oregs[mo]])

                if do_ctx:
                    tile([ctx[k][:, :] for k in range(8)], [("ctx", k) for k in range(8)], TC, 1, 0,
                         [ctx[k][:, :] for k in range(8)], [("ctx", k) for k in range(8)])
                for i in range(T // 512):
                    t0 = i * 512
                    S.dma("sp", xw[:], src[:, :, t0:t0 + 512].rearrange("k p t -> p k t"), w=["xw"])
                    tile([xw[:, k, :] for k in range(8)], ["xw"] * 8, 512, 0, TC + t0,
                         [xo[:, k, :] for k in range(8)], ["xo"] * 8)
                    S.dma("sp", dst[:, :, t0:t0 + 512].rearrange("k p t -> p k t"), xo[:], r=["xo"])
                S.flush()

        def phase_final(src):
            with ExitStack() as es:
                xw = sb(es, "xw", [128, 8, 512], F32)
                xo = sb(es, "xo", [128, 8, 512], F32)
                tmp = mk_tmp(es, 512)
                og, _ = PAR_OFF["gfin"]
                for i in range(T // 512):
                    t0 = i * 512
                    S.dma("sp", xw[:], src[:, :, t0:t0 + 512].rearrange("k p t -> p k t"), w=["xw"])
                    for k in range(8):
                        sq = tmp["sq%d" % (k % 2)]
                        S.op("act", "activation", out=sq[:], in_=xw[:, k, :], func=AF.Square, r=["xw"], w=[("sq", k % 2)])
                        S.op("pe", "matmul", PS(6)[:, :], lhsT=ones_f, rhs=sq[:], start=(k == 0), stop=(k == 7),
                             r=[("sq", k % 2), "cst"], w=[("ps", 6)])
                    std = tmp["std"]
                    S.op("act", "activation", out=std[:], in_=PS(6)[:, :], func=AF.Sqrt, scale=1.0 / D, bias=EPS,
                         r=[("ps", 6)], w=["std"])
                    S.op("dve", "reciprocal", out=std[:], in_=std[:], r=["std"], w=["std"])
                    for k in range(8):
                        S.op("dve", "scalar_tensor_tensor", out=xo[:, k, :], in0=xw[:, k, :],
                             scalar=par[:, og + k:og + k + 1], op0=ALU.mult, in1=std[:], op1=ALU.mult,
                             r=["xw", "par", "std"], w=["xo"])
                    S.dma("sp", outT[:, :, t0:t0 + 512].rearrange("k p t -> p k t"), xo[:], r=["xo"])
                S.flush()

        def halo_exchange(xsrc):
            def load(a):
                S.dma("sp", a.rearrange("p (k t) -> p k t", k=8),
                      xsrc[:, :, T - GW:T].rearrange("k p t -> p k t"), w=["hsrc"])
            exchange(hsrc, hdst, 8 * GW, load, halo[:].rearrange("p k t -> p (k t)"), "h")

        cur = xT_in
        last_mod = None
        for kind, l in plan:
            if last_mod != l:
                phase_mod(l)
                last_mod = l
            ctx_live = l < 2
            if kind == "A":
                phase_A1(l, cur, ctx_live)
                phase_scan(0, ctx_live)
                phase_scan(1, ctx_live)
                phase_A5(l, cur, XB, ctx_live)
                cur = XB
            elif kind == "B":
                phase_B(l, cur, XB, ctx_live)
                cur = XB
            elif kind == "F":
                need_weights(("f", l))
                halo_exchange(cur)
                phase_F(l, cur, XA, ctx_live)
                cur = XA
            elif kind == "M":
                pass
        if final:
            phase_final(cur)
        else:
            for k in range(8):
                S.dma("sp", outT[k], cur[k])
        for k in range(8):
            S.dma("sp", ctx_out[k], ctx[k][:], r=[("ctx", k)])
        S.flush()
        S.finish("sp")
        print("sched: ins=%d waits=%d cnt=%s" % (S.n_ins, S.n_wait, S.cnt))
    return nc


FULL_PLAN = [("A", 0), ("F", 0), ("B", 1), ("F", 1), ("A", 2), ("F", 2), ("B", 3), ("F", 3)]
_NC_CACHE = {}


def pack_par(b, odd, c, c_ctx, b_mod, g_mix, g_ffn, g_final, b_w_conv, f_w_conv, f_b_conv, a_b_gate):
    par = np.zeros((128, NPAR), np.float32)

    def put(name, arr):
        o, n = PAR_OFF[name]
        a = np.asarray(arr, np.float32).reshape(128, -1)
        assert a.shape[1] == n, (name, a.shape, n)
        par[:, o:o + n] = a
    bwc = np.asarray(b_w_conv, np.float32)
    fwc = np.asarray(f_w_conv, np.float32)
    put("cc", np.stack([fm(c[b]), fm(c_ctx)], axis=-1))
    put("bmod", fm(b_mod))
    put("gmix", fm(g_mix))
    put("gffn", fm(g_ffn))
    put("gfin", fm(g_final))
    put("bwc_l", fm(bwc))
    put("bwc_c", fm(bwc[:, ::-1] if odd else bwc))
    put("fwc_l", fm(fwc[:, ::-1] if odd else fwc))
    put("fwc_c", fm(fwc[:, ::-1] if odd else fwc))
    put("fbc", fm(f_b_conv))
    put("bgate", np.broadcast_to(gate_perm(np.asarray(a_b_gate, np.float32), odd).reshape(1, 32), (128, 32)))
    put("sel", np.broadcast_to(np.array([[1.0, 0.0]] if odd else [[0.0, 1.0]], np.float32), (128, 2)))
    return par


def gate_perm(g, odd):
    if not odd:
        return g
    sh = g.shape
    return np.ascontiguousarray(g.reshape(sh[:-1] + (2, 2, NH))[..., ::-1, :].reshape(sh))


def core_tokens(x_b, odd):
    r = np.asarray(x_b, np.float32).reshape(TFULL // GW, GW, D)
    r = r[NROW:][::-1] if odd else r[:NROW]
    return np.ascontiguousarray(r.reshape(T, D).T.reshape(8, 128, T))


def make_in_maps(inputs, cores, plan=None):
    maps = []
    gain = np.ascontiguousarray(np.broadcast_to(
        np.asarray(inputs["a_head_gain"], np.float32)[None], (128, 2, D)))
    wcache = {}
    for b, odd in cores:
        cx = np.asarray(inputs["ctx"][b], np.float32)
        if odd:
            cx = cx[::-1]
        m = {
            "xT": core_tokens(inputs["x"][b], odd),
            "ctxT": np.ascontiguousarray(cx.T.reshape(8, 128, TC)),
            "par": pack_par(b, odd, inputs["c"], inputs["c_ctx"], inputs["b_mod"], inputs["g_mix"], inputs["g_ffn"],
                            inputs["g_final"], inputs["b_w_conv"], inputs["f_w_conv"], inputs["f_b_conv"],
                            inputs["a_b_gate"]),
            "consts": make_consts(odd),
            "gain": gain,
        }
        for kind, l in (plan if plan is not None else FULL_PLAN):
            m["w_mod_%d" % l] = np.ascontiguousarray(inputs["w_mod"][l], np.float32)
            if kind == "A":
                j = l // 2
                key = ("awin", j, odd)
                if key not in wcache:
                    w = np.array(inputs["a_w_in"][j], np.float32)
                    w[:, 3072:] = gate_perm(w[:, 3072:], odd)
                    wcache[key] = w
                m["a_w_in_%d" % j] = wcache[key]
                m["a_w_out_%d" % j] = np.ascontiguousarray(inputs["a_w_out"][j], np.float32)
            elif kind == "B":
                m["b_w_in_%d" % (l // 2)] = np.ascontiguousarray(inputs["b_w_in"][l // 2], np.float32)
                m["b_w_out_%d" % (l // 2)] = np.ascontiguousarray(inputs["b_w_out"][l // 2], np.float32)
            elif kind == "F":
                m["f_w_up_%d" % l] = np.ascontiguousarray(inputs["f_w_up"][l], np.float32)
                m["f_w_down_%d" % l] = np.ascontiguousarray(inputs["f_w_down"][l], np.float32)
        maps.append(m)
    return maps


def assemble(results, cores, nb):
    out = np.empty((nb, TFULL, D), np.float32)
    for res, (b, odd) in zip(results, cores):
        o = res["outT"].reshape(D, T).T.reshape(NROW, GW, D)
        if odd:
            out[b, T:] = o[::-1].reshape(T, D)
        else:
            out[b, :T] = o.reshape(T, D)
    return out


def kernel(**inputs):
    key = "full"
    if key not in _NC_CACHE:
        _NC_CACHE[key] = build(FULL_PLAN, final=True, ncores=8)
    nc = _NC_CACHE[key]
    cores = [(b, odd) for b in range(4) for odd in (0, 1)]
    in_maps = make_in_maps(inputs, cores)
    res = run_bass_kernel_spmd(nc, in_maps, core_ids=list(range(8)))
    return assemble(res.results, cores, 4)
```

```python
import numpy as np
from contextlib import ExitStack
import ml_dtypes
import concourse.bass as bass
import concourse.mybir as mybir
from concourse.bass_utils import run_bass_kernel_spmd

F32 = mybir.dt.float32
BF16 = mybir.dt.bfloat16
AF = mybir.ActivationFunctionType
ALU = mybir.AluOpType

D = 1024
TFULL = 8192
T = 4096
TC = 256
TA = TC + T
DEPTH = 4
NH = 4
DK = 128
DV = 256
DVA = DV + 1
DFF = 2816
NJ = DFF // 128
GW = 64
CH = 64
NROW = T // GW
APROJ = 3088
EPS = 1e-6
NCH = TA // CH
SAME_ENG_SYNC = True


class Sched:
    ENGS = ("pe", "act", "dve", "pool", "sp")

    def __init__(self, nc, es, n_dma_sems=48, n_bg_sems=8):
        self.nc = nc
        self.eng = {"pe": nc.tensor, "act": nc.scalar, "dve": nc.vector,
                    "pool": nc.gpsimd, "sp": nc.sync}
        self.sems = []
        self.esem = {}
        for e in self.ENGS:
            self.esem[e] = len(self.sems)
            self.sems.append(es.enter_context(nc.semaphore("s_" + e)))
        self.dsem = []
        for i in range(n_dma_sems):
            self.dsem.append(len(self.sems))
            self.sems.append(es.enter_context(nc.semaphore("d%d" % i)))
        self.bsem = []
        for i in range(n_bg_sems):
            self.bsem.append(len(self.sems))
            self.sems.append(es.enter_context(nc.semaphore("b%d" % i)))
        self.brr = 0
        self.btarget = [0] * n_bg_sems
        self.bgev = {}
        self.ops = []
        self.cnt = {e: 0 for e in self.ENGS}
        self.waited = {e: {} for e in self.ENGS}
        self.pending = {e: {} for e in self.ENGS}
        self.rr = 0
        self.target = [0] * n_dma_sems
        self.n_ins = 0
        self.n_wait = 0

    def op(self, eng, meth, *args, r=(), w=(), **kw):
        self.ops.append(dict(eng=eng, meth=meth, args=args, kw=kw, r=tuple(r), w=tuple(w),
                             dma=False, signal=False, ev=None))

    def dma(self, eng, out, in_, r=(), w=(), bg=None):
        self.ops.append(dict(eng=eng, meth="dma_start", args=(), kw=dict(out=out, in_=in_),
                             r=tuple(r), w=tuple(w), dma=True, signal=True, ev=None, inc=16, bg=bg))

    def join_bg(self, group):
        for s, v in self.bgev.pop(group, []):
            for e in self.ENGS:
                if self.pending[e].get(s, 0) < v:
                    self.pending[e][s] = v

    def coll(self, ins, outs, groups, r=(), w=()):
        self.ops.append(dict(eng="pool", meth="collective_compute", args=("AllGather", ALU.bypass),
                             kw=dict(replica_groups=groups, ins=ins, outs=outs),
                             r=tuple(r), w=tuple(w), dma=True, signal=True, ev=None, inc=1))

    def flush(self):
        ops = self.ops
        self.ops = []
        state = {}
        deps = []
        last = {}
        for i, o in enumerate(ops):
            d = {}
            for r in o["r"]:
                st = state.get(r)
                if st is not None and st[0] is not None:
                    d[id(st[0])] = st[0]
            for r in o["w"]:
                st = state.get(r)
                if st is not None:
                    if st[0] is not None:
                        d[id(st[0])] = st[0]
                    for x in st[1].values():
                        d[id(x)] = x
                    for x in st[2]:
                        d[id(x)] = x
            for r in o["r"]:
                st = state.setdefault(r, [None, {}, []])
                if o["dma"]:
                    st[2].append(o)
                else:
                    st[1][o["eng"]] = o
            for r in o["w"]:
                st = state.setdefault(r, [None, {}, []])
                st[0] = o
                st[1] = {}
                st[2] = []
            d.pop(id(o), None)
            dd = []
            for oj in d.values():
                if (not oj["dma"]) and (not o["dma"]) and oj["eng"] == o["eng"] and \
                        (o["eng"] == "pe" or not SAME_ENG_SYNC):
                    continue
                dd.append(oj)
                oj["signal"] = True
            deps.append(dd)
            if not o["dma"]:
                last[o["eng"]] = o
        for o in last.values():
            o["signal"] = True
        nd = len(self.dsem)
        bar = {}
        for i, o in enumerate(ops):
            en = o["eng"]
            E = self.eng[en]
            need = dict(self.pending[en])
            self.pending[en] = {}
            for oj in deps[i]:
                s, v = oj["ev"]
                if need.get(s, 0) < v:
                    need[s] = v
            bgd = o["dma"] and o.get("bg") is not None
            if bgd:
                k = self.brr
                self.brr = (self.brr + 1) % len(self.bsem)
                s = self.bsem[k]
                if self.btarget[k] > 0 and need.get(s, 0) < self.btarget[k]:
                    need[s] = self.btarget[k]
            elif o["dma"]:
                k = self.rr
                self.rr = (self.rr + 1) % nd
                s = self.dsem[k]
                if self.target[k] > 0 and need.get(s, 0) < self.target[k]:
                    need[s] = self.target[k]
            for s, v in need.items():
                if self.waited[en].get(s, 0) < v:
                    E.wait_ge(self.sems[s], v)
                    self.waited[en][s] = v
                    self.n_wait += 1
            ins = getattr(E, o["meth"])(*o["args"], **o["kw"])
            self.n_ins += 1
            if bgd:
                self.btarget[k] += 16
                ins.then_inc(self.sems[self.bsem[k]], 16)
                o["ev"] = (self.bsem[k], self.btarget[k])
                self.bgev.setdefault(o["bg"], []).append(o["ev"])
            elif o["dma"]:
                self.target[k] += o["inc"]
                if o["inc"] == 16:
                    ins.then_inc(self.sems[self.dsem[k]], 16)
                else:
                    ins.then_inc(self.sems[self.dsem[k]])
                o["ev"] = (self.dsem[k], self.target[k])
                bar[self.dsem[k]] = self.target[k]
            elif o["signal"]:
                self.cnt[en] += 1
                ins.then_inc(self.sems[self.esem[en]], 1)
                o["ev"] = (self.esem[en], self.cnt[en])
                bar[self.esem[en]] = self.cnt[en]
        for e in self.ENGS:
            p = self.pending[e]
            for s, v in bar.items():
                if p.get(s, 0) < v:
                    p[s] = v

    def finish(self, eng="sp"):
        E = self.eng[eng]
        for s, v in self.pending[eng].items():
            if self.waited[eng].get(s, 0) < v:
                E.wait_ge(self.sems[s], v)
                self.waited[eng][s] = v


def _par_layout():
    off = {}
    o = 0
    for name, n in (("cc", 16), ("bmod", DEPTH * 48), ("gmix", DEPTH * 8), ("gffn", DEPTH * 8),
                    ("gfin", 8), ("bwc_l", 2 * 3 * 8), ("bwc_c", 2 * 3 * 8), ("fwc_l", DEPTH * 3 * NJ),
                    ("fwc_c", DEPTH * 3 * NJ), ("fbc", DEPTH * NJ), ("bgate", 2 * 16), ("sel", 2)):
        off[name] = (o, n)
        o += n
    return off, o


PAR_OFF, NPAR = _par_layout()
C_ONES, C_TRIF, C_TRIB, C_BLK0, C_BLK1, C_ID, C_MF, C_MB, C_TRL0, C_TRL1, C_ML0, C_ML1, NCONST = \
    0, 128, 256, 384, 512, 640, 768, 832, 896, 1024, 1152, 1216, 1280


def fm(v):
    v = np.asarray(v, np.float32)
    lead = v.shape[:-1]
    k = v.shape[-1] // 128
    a = v.reshape(lead + (k, 128))
    a = np.moveaxis(a, -1, 0)
    return np.ascontiguousarray(a)


def make_consts(odd):
    c = np.zeros((128, NCONST), np.float32)
    c[:, C_ONES:C_ONES + 128] = 1.0
    s = np.arange(128)[:, None]
    t = np.arange(128)[None, :]
    same = (s // 64) == (t // 64)
    c[:, C_TRIF:C_TRIF + 128] = (same & (s <= t))
    c[:, C_TRIB:C_TRIB + 128] = (same & (s >= t))
    c[:, C_BLK0:C_BLK0 + 128] = (s < 64)
    c[:, C_BLK1:C_BLK1 + 128] = (s >= 64)
    c[:, C_ID:C_ID + 128] = (s == t)
    s6 = np.arange(128)[:, None] % 64
    t6 = np.arange(64)[None, :]
    c[:, C_MF:C_MF + 64] = (s6 <= t6)
    c[:, C_MB:C_MB + 64] = (s6 >= t6)
    f0, f1 = (C_TRIB, C_TRIF) if odd else (C_TRIF, C_TRIB)
    c[:, C_TRL0:C_TRL0 + 128] = c[:, f0:f0 + 128]
    c[:, C_TRL1:C_TRL1 + 128] = c[:, f1:f1 + 128]
    m0, m1 = (C_MB, C_MF) if odd else (C_MF, C_MB)
    c[:, C_ML0:C_ML0 + 64] = c[:, m0:m0 + 64]
    c[:, C_ML1:C_ML1 + 64] = c[:, m1:m1 + 64]
    return c


def build(plan, final=True, ncores=8):
    nc = bass.Bass("TRN2", target_bir_lowering=False)

    def dram(name, shape, dtype, kind):
        return nc.dram_tensor(name, list(shape), dtype, kind=kind).ap()

    xT_in = dram("xT", [8, 128, T], F32, "ExternalInput")
    ctxT_in = dram("ctxT", [8, 128, TC], F32, "ExternalInput")
    par_in = dram("par", [128, NPAR], F32, "ExternalInput")
    const_in = dram("consts", [128, NCONST], F32, "ExternalInput")
    gain_in = dram("gain", [128, 2, D], F32, "ExternalInput")
    need_w = set()
    for kind, l in plan:
        need_w.add(("mod", l))
        if kind == "A":
            need_w.add(("a", l // 2))
        elif kind == "B":
            need_w.add(("b", l // 2))
        elif kind == "F":
            need_w.add(("f", l))
    Wi, Wb = {}, {}
    for kind, l in sorted(need_w):
        if kind == "mod":
            specs = [("w_mod", [D, 6 * D])]
        elif kind == "a":
            specs = [("a_w_in", [D, APROJ]), ("a_w_out", [D, D])]
        elif kind == "b":
            specs = [("b_w_in", [D, 3 * D]), ("b_w_out", [D, D])]
        else:
            specs = [("f_w_up", [D, 2 * DFF]), ("f_w_down", [DFF, D])]
        for nm, shp in specs:
            Wi[(nm, l)] = dram("%s_%d" % (nm, l), shp, F32, "ExternalInput")
            if nm == "f_w_up":
                Wb[(nm, l)] = dram("%s_%d_b" % (nm, l), shp, BF16, "Internal")
    outT = dram("outT", [8, 128, T], F32, "ExternalOutput")
    ctx_out = dram("ctx_out", [8, 128, TC], F32, "ExternalOutput")
    XA = dram("XA", [8, 128, T], F32, "Internal")
    XB = dram("XB", [8, 128, T], F32, "Internal")
    QK = dram("QK", [8, 128, TA], BF16, "Internal")
    KT = dram("KT", [TA, NH * DK], F32, "Internal")
    VA = dram("VA", [TA, NH * DV], BF16, "Internal")
    SO = dram("SO", [TA, D], F32, "Internal")
    SC = dram("SC", [TA, 16], F32, "Internal")
    HD = [dram("HF", [TA, D], F32, "Internal"), dram("HB", [TA, D], F32, "Internal")]
    NST = NH * DV + NH
    csrc = nc.dram_tensor("csrc", [128, NST], F32)
    cdst = nc.dram_tensor("cdst", [256, NST], F32)
    hsrc = nc.dram_tensor("hsrc", [128, 8 * GW], F32)
    hdst = nc.dram_tensor("hdst", [256, 8 * GW], F32)
    PAIRS = [[2 * i, 2 * i + 1] for i in range(ncores // 2)]

    ges = ExitStack()
    with ges:
        S = Sched(nc, ges)

        uid = [0]

        def sb(es, name, shape, dtype):
            uid[0] += 1
            return es.enter_context(nc.sbuf_tensor("%s_u%d" % (name, uid[0]), list(shape), dtype))

        par = sb(ges, "par", [128, NPAR], F32)
        cst = sb(ges, "cst", [128, NCONST], F32)
        identb = sb(ges, "identb", [128, 128], BF16)
        MODL = sb(ges, "modl", [128, 6, 8, 2], F32)
        ctx = [sb(ges, "ctx%d" % k, [128, TC], F32) for k in range(8)]
        EG = sb(ges, "eg", [128, NCH, 8], F32)
        halo = sb(ges, "halo", [128, 8, GW], F32)
        crecv = sb(ges, "crecv", [128, NST], F32)
        osel, _ = PAR_OFF["sel"]
        sel0 = par[:, osel:osel + 1]
        sel1 = par[:, osel + 1:osel + 2]

        def exchange(src_d, dst_d, n, load_src, out_ap, tag):
            with ExitStack() as es:
                two = sb(es, "xch2", [128, 2, n], F32)
                tmpx = sb(es, "xcht", [128, n], F32)
                load_src(src_d.ap())
                S.coll([src_d.ap().opt()], [dst_d.ap().opt()], PAIRS, r=[tag + "src"], w=[tag + "dst"])
                S.dma("sp", two[:], dst_d.ap().rearrange("(r p) n -> p r n", p=128), r=[tag + "dst"], w=["xch2"])
                S.op("dve", "tensor_scalar", out=tmpx[:], in0=two[:, 0, :], scalar1=sel0, scalar2=None,
                     op0=ALU.mult, r=["xch2", "par"], w=["xcht"])
                S.op("dve", "scalar_tensor_tensor", out=out_ap, in0=two[:, 1, :], scalar=sel1, op0=ALU.mult,
                     in1=tmpx[:], op1=ALU.add, r=["xch2", "xcht", "par"], w=[tag + "recv"])
                S.flush()
        psf = ges.enter_context(nc.psum_tensor("psf", [128, 7, 512], F32))
        psb = ges.enter_context(nc.psum_tensor("psb", [128, 1024], BF16))

        def PS(b):
            return psf[:, b, :]

        def pcol(name, idx=0):
            o, n = PAR_OFF[name]
            return o + idx

        onesb128 = sb(ges, "onesb128", [128, 128], BF16)
        ones_f = onesb128[:]

        S.dma("sp", par[:], par_in[:, :], w=["par"])
        S.dma("sp", cst[:], const_in[:, :], w=["cst"])
        for k in range(8):
            S.dma("sp", ctx[k][:], ctxT_in[k], w=[("ctx", k)])
        S.op("dve", "tensor_copy", out=identb[:], in_=cst[:, C_ID:C_ID + 128], r=["cst"], w=["identb"])
        S.op("dve", "tensor_copy", out=onesb128[:], in_=cst[:, C_ONES:C_ONES + 128], r=["cst"], w=["onesb128"])
        def cast_rows(dst, src, nrows, bg):
            for r0 in range(0, nrows, 128):
                S.dma("pool", dst[r0:r0 + 128, :], src[r0:r0 + 128, :], bg=bg)

        fgroups = []
        for kind, l in plan:
            if kind == "F" and ("f", l) not in fgroups:
                fgroups.append(("f", l))
        joined = set()

        def need_weights(g):
            if g[0] == "f" and g not in joined:
                issue_bg_casts()
                joined.add(g)
                S.join_bg(g)

        S.flush()
        bg_done = [False]

        def issue_bg_casts():
            if bg_done[0]:
                return
            bg_done[0] = True
            for g in fgroups:
                key = ("f_w_up", g[1])
                cast_rows(Wb[key], Wi[key], Wi[key].shape[0], g)

        def mk_tmp(es, n):
            t = {nm: sb(es, nm, [128, n], F32) for nm in ("std", "nt0", "nt1")}
            t["sq0"] = sb(es, "sq0", [128, n], BF16)
            t["sq1"] = sb(es, "sq1", [128, n], BF16)
            return t

        def norm_mod(xs, xregs, n, which, s_gs, s_sh, hx, tmp):
            pieces = [(c0, min(512, n - c0)) for c0 in range(0, n, 512)]
            for k in range(8):
                sq = tmp["sq%d" % (k % 2)]
                S.op("act", "activation", out=sq[:, 0:n], in_=xs[k], func=AF.Square,
                     r=[xregs[k]], w=[("sq", k % 2)])
                for pi, (c0, cn) in enumerate(pieces):
                    S.op("pe", "matmul", PS(6 - pi)[:, 0:cn], lhsT=ones_f, rhs=sq[:, c0:c0 + cn],
                         start=(k == 0), stop=(k == 7), r=[("sq", k % 2), "cst"], w=[("ps", 6 - pi)])
            std = tmp["std"]
            for pi, (c0, cn) in enumerate(pieces):
                S.op("act", "activation", out=std[:, c0:c0 + cn], in_=PS(6 - pi)[:, 0:cn], func=AF.Sqrt,
                     scale=1.0 / D, bias=EPS, r=[("ps", 6 - pi)], w=["std"])
            S.op("dve", "reciprocal", out=std[:, 0:n], in_=std[:, 0:n], r=["std"], w=["std"])
            for k in range(8):
                tt = tmp["nt%d" % (k % 2)]
                S.op("dve", "tensor_tensor", out=tt[:, 0:n], in0=xs[k], in1=std[:, 0:n], op=ALU.mult,
                     r=[xregs[k], "std"], w=[("nt", k % 2)])
                S.op("act", "activation", out=hx[k][:, 0:n], in_=tt[:, 0:n], func=AF.Identity,
                     scale=MODL[:, s_gs, k, which:which + 1], bias=MODL[:, s_sh, k, which:which + 1],
                     r=[("nt", k % 2), "mod"], w=[("hx", k)])

        def resid(out_ap, ps_ap, gcol, x_ap, r, w):
            S.op("dve", "scalar_tensor_tensor", out=out_ap, in0=ps_ap, scalar=gcol, op0=ALU.mult,
                 in1=x_ap, op1=ALU.add, r=r, w=w)

        def v3(ap, rowlen):
            return ap.rearrange("p (a b) -> p a b", b=rowlen)

        def phase_mod(l):
            with ExitStack() as es:
                scb = sb(es, "scb", [128, 8, 2], BF16)
                wm = [sb(es, "wm%d" % i, [128, 8, D], BF16) for i in range(6)]
                t1 = sb(es, "modt1", [128, 8, 2], F32)
                o, _ = PAR_OFF["cc"]
                S.op("act", "activation", out=scb[:].rearrange("p k w -> p (k w)"), in_=par[:, o:o + 16],
                     func=AF.Silu, r=["par"], w=["scb"])
                for s in range(6):
                    S.dma("pool", wm[s][:], Wi[("w_mod", l)][:, s * D:(s + 1) * D].rearrange("(k p) n -> p k n", p=128),
                          w=[("wm", s)])
                for s in range(6):
                    wt = wm[s]
                    pst = PS(s % 2)
                    for m in range(8):
                        for k in range(8):
                            S.op("pe", "matmul", pst[:, m * 2:m * 2 + 2], lhsT=wt[:, k, m * 128:(m + 1) * 128],
                                 rhs=scb[:, k, :], start=(k == 0), stop=(k == 7),
                                 r=[("wm", s), "scb"], w=[("ps", s % 2)])
                    ob, _ = PAR_OFF["bmod"]
                    bcol = par[:, ob + l * 48 + s * 8: ob + l * 48 + s * 8 + 8]
                    S.op("dve", "tensor_tensor", out=MODL[:, s, :, :],
                         in0=pst[:, 0:16].rearrange("p (m w) -> p m w", w=2),
                         in1=bcol.unsqueeze(2).to_broadcast([128, 8, 2]), op=ALU.add,
                         r=[("ps", s % 2), "par"], w=["mod"])
                for s, gname in ((1, "gmix"), (4, "gffn")):
                    og, _ = PAR_OFF[gname]
                    gcol = par[:, og + l * 8: og + l * 8 + 8]
                    S.op("dve", "tensor_scalar", out=t1[:], in0=MODL[:, s, :, :], scalar1=1.0, scalar2=None,
                         op0=ALU.add, r=["mod"], w=["modt1"])
                    S.op("dve", "tensor_tensor", out=MODL[:, s, :, :], in0=t1[:],
                         in1=gcol.unsqueeze(2).to_broadcast([128, 8, 2]), op=ALU.mult,
                         r=["modt1", "par"], w=["mod"])
                S.flush()

        def phase_B(l, src, dst, do_ctx):
            j = l // 2
            with ExitStack() as es:
                xw = sb(es, "xw", [128, 8, 512], F32)
                xo = sb(es, "xo", [128, 8, 512], F32)
                hx = [sb(es, "hx%d" % k, [128, 512], BF16) for k in range(8)]
                tmp = mk_tmp(es, 512)
                win = sb(es, "win", [128, 8, 3 * D], BF16)
                wout = sb(es, "wout", [128, 8, D], BF16)
                xv_sb = [sb(es, "xv%d" % i, [128, 512], F32) for i in range(2)]
                m_sb = [sb(es, "m%d" % i, [128, 512], F32) for i in range(2)]
                acc = [sb(es, "acc%d" % i, [128, 512], F32) for i in range(2)]
                z = [sb(es, "z%d" % k, [128, 512], BF16) for k in range(8)]
                for pc in range(3):
                    S.dma("pool", win[:, :, pc * D:(pc + 1) * D],
                          Wi[("b_w_in", j)][:, pc * D:(pc + 1) * D].rearrange("(k p) n -> p k n", p=128), w=[("win", pc)])
                S.dma("pool", wout[:], Wi[("b_w_out", j)].rearrange("(k p) n -> p k n", p=128), w=["wout"])
                def tile(xs, xregs, n, rowlen, which, outs, oregs):
                    ow, _ = PAR_OFF["bwc_c" if which == 1 else "bwc_l"]
                    norm_mod(xs, xregs, n, which, 1, 0, hx, tmp)
                    for c in range(8):
                        pr = c % 2
                        pb, pc_, pv = PS(3 * pr), PS(3 * pr + 1), PS(3 * pr + 2)
                        for gi, pt in enumerate((pb, pc_, pv)):
                            for k in range(8):
                                S.op("pe", "matmul", pt[:, 0:n],
                                     lhsT=win[:, k, gi * D + c * 128: gi * D + (c + 1) * 128], rhs=hx[k][:, 0:n],
                                     start=(k == 0), stop=(k == 7),
                                     r=[("win", gi), ("hx", k)], w=[("ps", 3 * pr + gi)])
                        S.op("act", "activation", out=xv_sb[pr][:, 0:n], in_=pv[:, 0:n], func=AF.Identity,
                             r=[("ps", 3 * pr + 2)], w=[("xv", pr)])
                        S.op("dve", "tensor_tensor", out=m_sb[pr][:, 0:n], in0=pc_[:, 0:n], in1=xv_sb[pr][:, 0:n],
                             op=ALU.mult, r=[("ps", 3 * pr + 1), ("xv", pr)], w=[("m", pr)])
                        w0 = par[:, ow + (j * 3 + 0) * 8 + c: ow + (j * 3 + 0) * 8 + c + 1]
                        w1 = par[:, ow + (j * 3 + 1) * 8 + c: ow + (j * 3 + 1) * 8 + c + 1]
                        w2 = par[:, ow + (j * 3 + 2) * 8 + c: ow + (j * 3 + 2) * 8 + c + 1]
                        S.op("act", "activation", out=acc[pr][:, 0:n], in_=m_sb[pr][:, 0:n], func=AF.Identity,
                             scale=w1, r=[("m", pr), "par"], w=[("acc", pr)])
                        a3 = v3(acc[pr][:, 0:n], rowlen)
                        m3 = v3(m_sb[pr][:, 0:n], rowlen)
                        S.op("dve", "scalar_tensor_tensor", out=a3[:, :, 1:rowlen], in0=m3[:, :, 0:rowlen - 1],
                             scalar=w0, op0=ALU.mult, in1=a3[:, :, 1:rowlen], op1=ALU.add,
                             r=[("m", pr), ("acc", pr), "par"], w=[("acc", pr)])
                        S.op("dve", "scalar_tensor_tensor", out=a3[:, :, 0:rowlen - 1], in0=m3[:, :, 1:rowlen],
                             scalar=w2, op0=ALU.mult, in1=a3[:, :, 0:rowlen - 1], op1=ALU.add,
                             r=[("m", pr), ("acc", pr), "par"], w=[("acc", pr)])
                        S.op("dve", "tensor_tensor", out=z[c][:, 0:n], in0=pb[:, 0:n], in1=acc[pr][:, 0:n],
                             op=ALU.mult, r=[("ps", 3 * pr), ("acc", pr)], w=[("z", c)])
                    for mo in range(8):
                        pt = PS(mo % 6)
                        for c in range(8):
                            S.op("pe", "matmul", pt[:, 0:n], lhsT=wout[:, c, mo * 128:(mo + 1) * 128],
                                 rhs=z[c][:, 0:n], start=(c == 0), stop=(c == 7),
                                 r=["wout", ("z", c)], w=[("ps", mo % 6)])
                        resid(outs[mo], pt[:, 0:n], MODL[:, 2, mo, which:which + 1], xs[mo],
                              r=[("ps", mo % 6), "mod", xregs[mo]], w=[oregs[mo]])

                if do_ctx:
                    tile([ctx[k][:, :] for k in range(8)], [("ctx", k) for k in range(8)], TC, TC, 1,
                         [ctx[k][:, :] for k in range(8)], [("ctx", k) for k in range(8)])
                for i in range(T // 512):
                    t0 = i * 512
                    S.dma("sp", xw[:], src[:, :, t0:t0 + 512].rearrange("k p t -> p k t"), w=["xw"])
                    tile([xw[:, k, :] for k in range(8)], ["xw"] * 8, 512, GW, 0,
                         [xo[:, k, :] for k in range(8)], ["xo"] * 8)
                    S.dma("sp", dst[:, :, t0:t0 + 512].rearrange("k p t -> p k t"), xo[:], r=["xo"])
                S.flush()

        def phase_F(l, src, dst, do_ctx):
            with ExitStack() as es:
                NW = 640
                xw = sb(es, "xw", [128, 8, NW], F32)
                xo = sb(es, "xo", [128, 8, 512], F32)
                hx = [sb(es, "hx%d" % k, [128, NW], BF16) for k in range(8)]
                tmp = mk_tmp(es, NW)
                wg = [sb(es, "wg%d" % i, [128, 8, 256], BF16) for i in range(2)]
                wv = [sb(es, "wv%d" % i, [128, 8, 256], BF16) for i in range(2)]
                wdn = sb(es, "wdn", [128, NJ, D], BF16)
                acc = [sb(es, "acc%d" % i, [128, 512], F32) for i in range(2)]
                sil = [sb(es, "sil%d" % i, [128, 512], F32) for i in range(2)]
                act = [sb(es, "act%d" % jj, [128, 512], BF16) for jj in range(NJ)]
                for jb in range(0, NJ, 11):
                    S.dma("pool", wdn[:, jb:jb + 11, :],
                          Wi[("f_w_down", l)][jb * 128:(jb + 11) * 128, :].rearrange("(j p) n -> p j n", p=128),
                          w=[("wdn", jb)])
                obc, _ = PAR_OFF["fbc"]

                def tile(xs, xregs, nw, co, n, shift, which, outs, oregs):
                    owc, _ = PAR_OFF["fwc_c" if which == 1 else "fwc_l"]
                    lo_ok = co >= shift
                    hi_ok = nw >= co + n + shift
                    norm_mod(xs, xregs, nw, which, 4, 3, hx, tmp)
                    gp = [(c0, min(512, nw - c0)) for c0 in range(0, nw, 512)]
                    for jj in range(NJ):
                        pr = jj % 2
                        if jj % 2 == 0:
                            wb = (jj // 2) % 2
                            S.dma("sp", wg[wb][:], Wb[("f_w_up", l)][:, jj * 128: jj * 128 + 256].rearrange(
                                "(k p) n -> p k n", p=128), w=[("wg", wb)])
                            S.dma("sp", wv[wb][:], Wb[("f_w_up", l)][:, DFF + jj * 128: DFF + jj * 128 + 256].rearrange(
                                "(k p) n -> p k n", p=128), w=[("wv", wb)])
                        wb = (jj // 2) % 2
                        wo_ = (jj % 2) * 128
                        gflat = psf[:, 2 * pr:2 * pr + 2, :].rearrange("p b c -> p (b c)")
                        pv = PS(4 + pr)
                        for pi, (c0, cn) in enumerate(gp):
                            for k in range(8):
                                S.op("pe", "matmul", PS(2 * pr + pi)[:, 0:cn], lhsT=wg[wb][:, k, wo_:wo_ + 128],
                                     rhs=hx[k][:, c0:c0 + cn], start=(k == 0), stop=(k == 7),
                                     r=[("wg", wb), ("hx", k)], w=[("ps", 2 * pr + pi)])
                        for k in range(8):
                            S.op("pe", "matmul", pv[:, 0:n], lhsT=wv[wb][:, k, wo_:wo_ + 128],
                                 rhs=hx[k][:, co:co + n], start=(k == 0), stop=(k == 7),
                                 r=[("wv", wb), ("hx", k)], w=[("ps", 4 + pr)])
                        greg = [("ps", 2 * pr), ("ps", 2 * pr + 1)]
                        w0 = par[:, owc + (l * 3 + 0) * NJ + jj: owc + (l * 3 + 0) * NJ + jj + 1]
                        w1 = par[:, owc + (l * 3 + 1) * NJ + jj: owc + (l * 3 + 1) * NJ + jj + 1]
                        w2 = par[:, owc + (l * 3 + 2) * NJ + jj: owc + (l * 3 + 2) * NJ + jj + 1]
                        bc = par[:, obc + l * NJ + jj: obc + l * NJ + jj + 1]
                        a = acc[pr]
                        S.op("dve", "tensor_scalar", out=a[:, 0:n], in0=gflat[:, co:co + n], scalar1=w1, scalar2=None,
                             op0=ALU.mult, r=greg + ["par"], w=[("acc", pr)])
                        if lo_ok:
                            o0, o1, s0 = 0, n, co - shift
                        else:
                            o0, o1, s0 = shift, n, co
                        S.op("dve", "scalar_tensor_tensor", out=a[:, o0:o1], in0=gflat[:, s0:s0 + (o1 - o0)],
                             scalar=w0, op0=ALU.mult, in1=a[:, o0:o1], op1=ALU.add,
                             r=greg + ["par", ("acc", pr)], w=[("acc", pr)])
                        if hi_ok:
                            o0, o1 = 0, n
                        else:
                            o0, o1 = 0, n - shift
                        S.op("dve", "scalar_tensor_tensor", out=a[:, o0:o1],
                             in0=gflat[:, co + shift:co + shift + (o1 - o0)],
                             scalar=w2, op0=ALU.mult, in1=a[:, o0:o1], op1=ALU.add,
                             r=greg + ["par", ("acc", pr)], w=[("acc", pr)])
                        S.op("act", "activation", out=sil[pr][:, 0:n], in_=a[:, 0:n], func=AF.Silu, bias=bc,
                             r=[("acc", pr), "par"], w=[("sil", pr)])
                        S.op("dve", "tensor_tensor", out=act[jj][:, 0:n], in0=pv[:, 0:n], in1=sil[pr][:, 0:n],
                             op=ALU.mult, r=[("ps", 4 + pr), ("sil", pr)], w=[("act", jj)])
                    for mo in range(8):
                        pt = PS(mo % 4)
                        for jj in range(NJ):
                            S.op("pe", "matmul", pt[:, 0:n], lhsT=wdn[:, jj, mo * 128:(mo + 1) * 128],
                                 rhs=act[jj][:, 0:n], start=(jj == 0), stop=(jj == NJ - 1),
                                 r=[("wdn", 0), ("wdn", 11), ("act", jj)], w=[("ps", mo % 4)])
                        resid(outs[mo], pt[:, 0:n], MODL[:, 5, mo, which:which + 1], xs[mo][:, co:co + n],
                              r=[("ps", mo % 4), "mod", xregs[mo]], w=[oregs[mo]])

                if do_ctx:
                    tile([ctx[k][:, :] for k in range(8)], [("ctx", k) for k in range(8)], TC, 0, TC, 1, 1,
                         [ctx[k][:, :] for k in range(8)], [("ctx", k) for k in range(8)])
                for i in range(NROW // 8):
                    r0 = i * 8
                    wlo, whi = max(0, r0 - 1), min(NROW, r0 + 9)
                    nw = (whi - wlo) * GW
                    co = (r0 - wlo) * GW
                    S.dma("sp", xw[:, :, 0:nw], src[:, :, wlo * GW:whi * GW].rearrange("k p t -> p k t"), w=["xw"])
                    if r0 + 9 > NROW:
                        S.op("act", "activation", out=xw[:, :, nw:nw + GW], in_=halo[:], func=AF.Identity,
                             r=["hrecv"], w=["xw"])
                        nw += GW
                    tile([xw[:, k, 0:nw] for k in range(8)], ["xw"] * 8, nw, co, 512, GW, 0,
                         [xo[:, k, :] for k in range(8)], ["xo"] * 8)
                    S.dma("sp", dst[:, :, r0 * GW:r0 * GW + 512].rearrange("k p t -> p k t"), xo[:], r=["xo"])
                S.flush()

        def phase_A1(l, src, emit_ctx):
            j = l // 2
            with ExitStack() as es:
                xw = sb(es, "xw", [128, 8, 512], F32)
                hx = [sb(es, "hx%d" % k, [128, 512], BF16) for k in range(8)]
                tmp = mk_tmp(es, 512)
                win = sb(es, "win", [128, 8, APROJ], BF16)
                qkb = sb(es, "qkb", [128, 8, 512], BF16)
                kt_sb = [sb(es, "kt%d" % i, [128, NH * DK], F32) for i in range(2)]
                va_sb = [sb(es, "va%d" % i, [128, NH * DV], BF16) for i in range(2)]
                so_sb = [sb(es, "so%d" % i, [128, D], F32) for i in range(2)]
                g_sb = [sb(es, "g%d" % i, [128, 16], F32) for i in range(2)]
                sp_sb = [sb(es, "sp%d" % i, [128, 8], F32) for i in range(2)]
                u_sb = [sb(es, "u%d" % i, [128, 8], F32) for i in range(2)]
                sc_sb = [sb(es, "sc%d" % i, [128, 16], F32) for i in range(2)]
                for pc, (c0, c1) in enumerate(((0, 1024), (1024, 2048), (2048, APROJ))):
                    S.dma("pool", win[:, :, c0:c1], Wi[("a_w_in", j)][:, c0:c1].rearrange("(k p) n -> p k n", p=128),
                          w=[("win", pc)])
                wreg = [("win", 0), ("win", 1), ("win", 2)]
                obg, _ = PAR_OFF["bgate"]
                bg = par[:, obg + j * 16: obg + j * 16 + 16]
                stc = [0]
                pendB = []

                def tile(xs, xregs, n, which, ta0, do_o):
                    tr0, tr1 = (C_TRIF, C_TRIB) if which == 1 else (C_TRL0, C_TRL1)
                    norm_mod(xs, xregs, n, which, 1, 0, hx, tmp)
                    for m in range(8):
                        pt = PS(m % 2)
                        for k in range(8):
                            S.op("pe", "matmul", pt[:, 0:n], lhsT=win[:, k, m * 128:(m + 1) * 128], rhs=hx[k][:, 0:n],
                                 start=(k == 0), stop=(k == 7), r=wreg + [("hx", k)], w=[("ps", m % 2)])
                        S.op("act", "activation", out=qkb[:, m, 0:n], in_=pt[:, 0:n], func=AF.Identity,
                             scale=(1.0 if m < 4 else DK ** -0.5), r=[("ps", m % 2)], w=["qkb"])
                    S.dma("sp", QK[:, :, ta0:ta0 + n].rearrange("k p t -> p k t"), qkb[:, :, 0:n], r=["qkb"])
                    for s in range(n // 128):
                        st = stc[0] % 2
                        stc[0] += 1
                        tsl = slice(s * 128, (s + 1) * 128)
                        row0 = ta0 + s * 128

                        def proj(bank, c0, cn):
                            for k in range(8):
                                S.op("pe", "matmul", PS(bank)[:, 0:cn], lhsT=hx[k][:, tsl], rhs=win[:, k, c0:c0 + cn],
                                     start=(k == 0), stop=(k == 7), r=wreg + [("hx", k)], w=[("ps", bank)])
                        proj(2, 512, 512)
                        S.op("act", "activation", out=kt_sb[st][:], in_=PS(2)[:, :], func=AF.Identity,
                             scale=DK ** -0.5, r=[("ps", 2)], w=[("kt", st)])
                        S.dma("sp", KT[row0:row0 + 128, :], kt_sb[st][:], r=[("kt", st)])
                        for pi in range(2):
                            proj(3 + pi, 1024 + pi * 512, 512)
                            S.op("act", "activation", out=va_sb[st][:, pi * 512:(pi + 1) * 512], in_=PS(3 + pi)[:, :],
                                 func=AF.Identity, r=[("ps", 3 + pi)], w=[("va", st)])
                        S.dma("sp", VA[row0:row0 + 128, :], va_sb[st][:], r=[("va", st)])
                        if do_o:
                            for pi in range(2):
                                proj(2 + 2 * pi, 2048 + pi * 512, 512)
                                S.op("act", "activation", out=so_sb[st][:, pi * 512:(pi + 1) * 512],
                                     in_=PS(2 + 2 * pi)[:, :], func=AF.Sigmoid, r=[("ps", 2 + 2 * pi)],
                                     w=[("so", st)])
                            S.dma("sp", SO[row0:row0 + 128, :], so_sb[st][:], r=[("so", st)])
                        proj(5, 3072, 16)
                        while pendB:
                            pendB.pop(0)()
                        S.op("dve", "tensor_tensor", out=g_sb[st][:], in0=PS(5)[:, 0:16], in1=bg, op=ALU.add,
                             r=[("ps", 5), "par"], w=[("g", st)])
                        S.op("act", "activation", out=sp_sb[st][:], in_=g_sb[st][:, 8:16], func=AF.Exp, scale=-1.0,
                             r=[("g", st)], w=[("sp", st)])
                        S.op("act", "activation", out=sp_sb[st][:], in_=sp_sb[st][:], func=AF.Ln, bias=1.0, scale=1.0,
                             r=[("sp", st)], w=[("sp", st)])
                        def partB(st=st, row0=row0, tr0=tr0, tr1=tr1):
                            S.op("pe", "matmul", PS(6)[:, 32:36], lhsT=cst[:, tr0:tr0 + 128], rhs=sp_sb[st][:, 0:4],
                                 start=True, stop=True, r=[("sp", st), "cst"], w=[("ps", 6)])
                            S.op("pe", "matmul", PS(6)[:, 36:40], lhsT=cst[:, tr1:tr1 + 128], rhs=sp_sb[st][:, 4:8],
                                 start=True, stop=True, r=[("sp", st), "cst"], w=[("ps", 6)])
                            S.op("pe", "matmul", PS(6)[:, 64:72], lhsT=cst[:, C_BLK0:C_BLK0 + 128], rhs=sp_sb[st][:, 0:8],
                                 start=True, stop=True, r=[("sp", st), "cst"], w=[("ps", 6)])
                            S.op("pe", "matmul", PS(6)[:, 72:80], lhsT=cst[:, C_BLK1:C_BLK1 + 128], rhs=sp_sb[st][:, 0:8],
                                 start=True, stop=True, r=[("sp", st), "cst"], w=[("ps", 6)])
                            S.op("dve", "tensor_tensor", out=u_sb[st][:], in0=PS(6)[:, 32:40], in1=g_sb[st][:, 0:8],
                                 op=ALU.add, r=[("ps", 6), ("g", st)], w=[("u", st)])
                            S.op("act", "activation", out=sc_sb[st][:, 0:8], in_=u_sb[st][:], func=AF.Exp,
                                 r=[("u", st)], w=[("sc", st)])
                            S.op("act", "activation", out=sc_sb[st][:, 8:16], in_=PS(6)[:, 32:40], func=AF.Exp,
                                 r=[("ps", 6)], w=[("sc", st)])
                            ch0 = row0 // CH
                            S.op("act", "activation", out=EG[:, ch0:ch0 + 2, :].rearrange("p c g -> p (c g)"),
                                 in_=PS(6)[:, 64:80], func=AF.Exp, scale=-1.0, r=[("ps", 6)], w=["eg"])
                            S.dma("sp", SC[row0:row0 + 128, :], sc_sb[st][:], r=[("sc", st)])
                        pendB.append(partB)
                    while pendB:
                        pendB.pop(0)()

                tile([ctx[k][:, :] for k in range(8)], [("ctx", k) for k in range(8)], TC, 1, 0, emit_ctx)
                for i in range(T // 512):
                    t0 = i * 512
                    S.dma("sp", xw[:], src[:, :, t0:t0 + 512].rearrange("k p t -> p k t"), w=["xw"])
                    tile([xw[:, k, :] for k in range(8)], ["xw"] * 8, 512, 0, TC + t0, True)
                S.flush()

        def phase_scan(d, emit_ctx):
            with ExitStack() as es:
                qk_t = [sb(es, "qkt%d" % i, [128, 8, 512], BF16) for i in range(2)]
                kt_t = [sb(es, "ktt%d" % i, [64, 8, NH * DK], F32) for i in range(2)]
                va_t = [sb(es, "vat%d" % i, [64, 8, NH * DV], BF16) for i in range(2)]
                sc_t = [sb(es, "sct%d" % i, [64, 8, 16], F32) for i in range(2)]
                hout = [sb(es, "hout%d" % i, [64, 8, D], F32) for i in range(2)]
                Ct = sb(es, "Ct", [128, NH, DV], F32)
                nst = sb(es, "nst", [128, NH], F32)
                Cb = [sb(es, "Cb%d" % i, [128, NH, DV], BF16) for i in range(2)]
                nb_ = [sb(es, "nb%d" % i, [128, NH], BF16) for i in range(2)]
                ntmp = sb(es, "ntmp", [128, NH], F32)
                pT = [sb(es, "pT%d" % i, [64, NH, 64], BF16) for i in range(2)]
                ks = [sb(es, "ks%d" % i, [64, NH * DK], BF16) for i in range(2)]
                absb = sb(es, "absb", [64, NH], F32)
                den = sb(es, "den", [64, NH], F32)
                onesb = sb(es, "onesb", [128, 2], BF16)
                S.op("dve", "memset", Ct[:], 0.0, w=[("Ct", h) for h in range(NH)])
                S.op("dve", "memset", nst[:], 0.0, w=["nst"])
                S.op("dve", "memset", Cb[0][:], 0.0, w=[("Cb", 0, h) for h in range(NH)])
                S.op("dve", "memset", nb_[0][:], 0.0, w=[("nb", 0)])
                S.op("dve", "memset", onesb[:], 1.0, w=["onesb"])
                issue_bg_casts()
                mc_ctx = C_MF if d == 0 else C_MB
                mc_lat = C_ML0 if d == 0 else C_ML1
                blocks = [(0, 4, True)] + [(TC + i * 512, 8, False) for i in range(T // 512)]
                if d == 1:
                    blocks = ([blocks[0]] if emit_ctx else []) + blocks[1:][::-1]
                chunks = []
                for bi, (ta0, nb, is_ctx) in enumerate(blocks):
                    order = list(range(nb)) if d == 0 else list(range(nb))[::-1]
                    for ci in order:
                        chunks.append(dict(bi=bi, bf=bi % 2, ta0=ta0, nb=nb, is_ctx=is_ctx, ci=ci,
                                           chg=ta0 // CH + ci, emit=((not is_ctx) or emit_ctx),
                                           first=(ci == order[0]), last=(ci == order[-1])))
                ones4 = cst[:, C_ONES:C_ONES + NH]
                SB = (0, 6)
                st = dict(prev=None, injected=(d == 0))

                def load_block(bi):
                    if bi >= len(blocks):
                        return
                    ta0, nb, _ = blocks[bi]
                    bf, ntok = bi % 2, nb * 64
                    if True:
                        S.dma("sp", qk_t[bf][:, :, 0:ntok], QK[:, :, ta0:ta0 + ntok].rearrange("k p t -> p k t"),
                              w=[("qkt", bf)])
                        S.dma("sp", kt_t[bf][:, 0:nb, :], KT[ta0:ta0 + ntok, :].rearrange("(c s) d -> s c d", s=64),
                              w=[("ktt", bf)])
                        S.dma("sp", va_t[bf][:, 0:nb, :], VA[ta0:ta0 + ntok, :].rearrange("(c s) d -> s c d", s=64),
                              w=[("vat", bf)])
                        S.dma("sp", sc_t[bf][:, 0:nb, :], SC[ta0:ta0 + ntok, :].rearrange("(c s) d -> s c d", s=64),
                              w=[("sct", bf)])

                def P1(ch, q):
                    bf, ci = ch["bf"], ch["ci"]
                    tsl = slice(ci * 64, (ci + 1) * 64)
                    if ch["emit"]:
                        for h in range(NH):
                            S.op("pe", "matmul", PS(SB[q % 2])[0:64, h * 64:(h + 1) * 64], lhsT=qk_t[bf][:, 4 + h, tsl],
                                 rhs=qk_t[bf][:, h, tsl], start=True, stop=True,
                                 r=[("qkt", bf)], w=[("ps", SB[q % 2])])
                    S.op("dve", "tensor_tensor", out=ks[q % 2][:].rearrange("p (h d) -> p h d", h=NH),
                         in0=kt_t[bf][:, ci, :].rearrange("p (h d) -> p h d", h=NH),
                         in1=sc_t[bf][:, ci, d * 4:d * 4 + 4].unsqueeze(2).to_broadcast([64, NH, DK]), op=ALU.mult,
                         r=[("ktt", bf), ("sct", bf)], w=[("ks", q % 2)])

                def P2(ch, q):
                    bf, ci = ch["bf"], ch["ci"]
                    mcol = mc_ctx if ch["is_ctx"] else mc_lat
                    mask = cst[0:64, mcol:mcol + 64]
                    if ch["emit"]:
                        for h in range(NH):
                            S.op("dve", "scalar_tensor_tensor", out=pT[q % 2][:, h, :],
                                 in0=PS(SB[q % 2])[0:64, h * 64:(h + 1) * 64],
                                 scalar=sc_t[bf][:, ci, d * 4 + h:d * 4 + h + 1],
                                 op0=ALU.mult, in1=mask, op1=ALU.mult,
                                 r=[("ps", SB[q % 2]), ("sct", bf), "cst"], w=[("pT", q % 2, h)])
                    for h in range(NH):
                        pu = PS(4 + h // 2)[:, (h % 2) * 256:(h % 2) * 256 + 256]
                        S.op("pe", "matmul", pu, lhsT=ks[q % 2][:, h * DK:(h + 1) * DK],
                             rhs=va_t[bf][:, ci, h * DV:(h + 1) * DV], start=True, stop=True,
                             r=[("ks", q % 2), ("vat", bf)], w=[("ps", 4 + h // 2)])
                    for h in range(NH):
                        S.op("pe", "matmul", PS(3)[:, 8 + h:9 + h], lhsT=ks[q % 2][:, h * DK:(h + 1) * DK],
                             rhs=onesb[0:64, 0:1], start=True, stop=True, r=[("ks", q % 2), "onesb"], w=[("psn",)])

                def P3(ch, q):
                    bf, ci, chg = ch["bf"], ch["ci"], ch["chg"]
                    tsl = slice(ci * 64, (ci + 1) * 64)
                    cur, nxt = q % 2, (q + 1) % 2
                    if not ch["is_ctx"] and not st["injected"]:
                        st["injected"] = True
                        st["prev"] = "ones"
                        S.op("dve", "tensor_copy", out=Ct[:].rearrange("p h v -> p (h v)"), in_=crecv[:, 0:NH * DV],
                             r=["crecv"], w=[("Ct", h) for h in range(NH)])
                        S.op("dve", "tensor_copy", out=nst[:], in_=crecv[:, NH * DV:NST], r=["crecv"], w=["nst"])
                        S.op("act", "activation", out=Cb[cur][:].rearrange("p h v -> p (h v)"),
                             in_=crecv[:, 0:NH * DV], func=AF.Identity, r=["crecv"], w=[("Cb", cur, h) for h in range(NH)])
                        S.op("act", "activation", out=nb_[cur][:], in_=crecv[:, NH * DV:NST], func=AF.Identity,
                             r=["crecv"], w=[("nb", cur)])
                    use_ones = (st["prev"] == "ones")
                    pgl = chg if (st["prev"] is None or use_ones) else st["prev"]
                    st["prev"] = chg
                    if ch["emit"]:
                        for h in range(NH):
                            pa = PS(1 + h // 2)[0:64, (h % 2) * 256:(h % 2) * 256 + 256]
                            S.op("pe", "matmul", pa, lhsT=pT[cur][:, h, :], rhs=va_t[bf][:, ci, h * DV:(h + 1) * DV],
                                 start=True, stop=False, r=[("pT", cur, h), ("vat", bf)], w=[("ps", 1 + h // 2)])
                            S.op("pe", "matmul", pa, lhsT=qk_t[bf][:, h, tsl], rhs=Cb[cur][:, h, :],
                                 start=False, stop=True, r=[("qkt", bf), ("Cb", cur, h)], w=[("ps", 1 + h // 2)])
                        for h in range(NH):
                            pbn = PS(3)[0:64, h:h + 1]
                            S.op("pe", "matmul", pbn, lhsT=pT[cur][:, h, :], rhs=onesb[0:64, 0:1],
                                 start=True, stop=False, r=[("pT", cur, h), "onesb"], w=[("psb4",)])
                            S.op("pe", "matmul", pbn, lhsT=qk_t[bf][:, h, tsl], rhs=nb_[cur][:, h:h + 1],
                                 start=False, stop=True, r=[("qkt", bf), ("nb", cur)], w=[("psb4",)])
                    egp = ones4 if use_ones else EG[:, pgl, d * 4:d * 4 + 4]
                    egc = EG[:, chg, d * 4:d * 4 + 4]
                    for h in range(NH):
                        pu = PS(4 + h // 2)[:, (h % 2) * 256:(h % 2) * 256 + 256]
                        S.op("dve", "scalar_tensor_tensor", out=Ct[:, h, :], in0=Ct[:, h, :],
                             scalar=(cst[:, C_ONES:C_ONES + 1] if use_ones else EG[:, pgl, d * 4 + h:d * 4 + h + 1]),
                             op0=ALU.mult, in1=pu, op1=ALU.add,
                             r=[("Ct", h), "eg", ("ps", 4 + h // 2)], w=[("Ct", h)])
                        S.op("act", "activation", out=Cb[nxt][:, h, :], in_=Ct[:, h, :], func=AF.Identity,
                             scale=EG[:, chg, d * 4 + h:d * 4 + h + 1], r=[("Ct", h), "eg"], w=[("Cb", nxt, h)])
                    S.op("dve", "tensor_tensor", out=ntmp[:], in0=nst[:], in1=egp, op=ALU.mult,
                         r=["nst", "eg"], w=["ntmp"])
                    S.op("dve", "tensor_tensor", out=nst[:], in0=PS(3)[:, 8:8 + NH], in1=ntmp[:], op=ALU.add,
                         r=[("psn",), "ntmp"], w=["nst"])
                    S.op("dve", "tensor_tensor", out=nb_[nxt][:], in0=nst[:], in1=egc, op=ALU.mult,
                         r=["nst", "eg"], w=[("nb", nxt)])

                def P4(ch, q):
                    bf, ci = ch["bf"], ch["ci"]
                    if not ch["emit"]:
                        return
                    ho = hout[ch["bi"] % 2]
                    S.op("act", "activation", out=absb[:], in_=PS(3)[0:64, 0:NH], func=AF.Abs,
                         r=[("psb4",)], w=["absb"])
                    S.op("dve", "tensor_tensor", out=den[:], in0=absb[:], in1=sc_t[bf][:, ci, 8 + d * 4:12 + d * 4],
                         op=ALU.max, r=["absb", ("sct", bf)], w=["den"])
                    S.op("dve", "reciprocal", out=den[:], in_=den[:], r=["den"], w=["den"])
                    for h in range(NH):
                        pa = PS(1 + h // 2)[0:64, (h % 2) * 256:(h % 2) * 256 + 256]
                        S.op("act", "activation", out=ho[:, ci, h * DV:(h + 1) * DV], in_=pa, func=AF.Identity,
                             scale=den[:, h:h + 1], r=[("ps", 1 + h // 2), "den"], w=[("hout", ch["bi"] % 2, h)])
                    if ch["last"]:
                        ta0, ntok, nb = ch["ta0"], ch["nb"] * 64, ch["nb"]
                        S.dma("sp", HD[d][ta0:ta0 + ntok, :].rearrange("(c s) v -> s c v", s=64), ho[:, 0:nb, :],
                              r=[("hout", ch["bi"] % 2, h) for h in range(NH)])

                load_block(0)
                load_block(1)
                P1(chunks[0], 0)
                P2(chunks[0], 0)
                for q, ch in enumerate(chunks):
                    P3(ch, q)
                    if q + 1 < len(chunks):
                        P1(chunks[q + 1], q + 1)
                    P4(ch, q)
                    if ch["last"]:
                        load_block(ch["bi"] + 2)
                    if q + 1 < len(chunks):
                        P2(chunks[q + 1], q + 1)
                if d == 0:
                    csend = sb(es, "csend", [128, NST], F32)
                    lastc = st["prev"]
                    for h in range(NH):
                        S.op("act", "activation", out=csend[:, h * DV:(h + 1) * DV], in_=Ct[:, h, :], func=AF.Identity,
                             scale=EG[:, lastc, d * 4 + h:d * 4 + h + 1], r=[("Ct", h), "eg"], w=["csend"])
                    S.op("dve", "tensor_tensor", out=csend[:, NH * DV:NST], in0=nst[:],
                         in1=EG[:, lastc, d * 4:d * 4 + 4], op=ALU.mult, r=["nst", "eg"], w=["csend"])
                    S.dma("sp", csrc.ap(), csend[:], r=["csend"], w=["csrc"])
                S.flush()
            if d == 0:
                exchange(csrc, cdst, NST, lambda a: None, crecv[:], "c")

        def phase_A5(l, src, dst, do_ctx):
            j = l // 2
            with ExitStack() as es:
                xw = sb(es, "xw", [128, 8, 512], F32)
                xo = sb(es, "xo", [128, 8, 512], F32)
                hf = [sb(es, "hf%d" % i, [128, D], F32) for i in range(4)]
                hb = [sb(es, "hb%d" % i, [128, D], F32) for i in range(4)]
                so = [sb(es, "so%d" % i, [128, D], F32) for i in range(4)]
                hs = sb(es, "hs", [128, D], F32)
                junk = sb(es, "junk", [128, DV], BF16)
                ss = sb(es, "ss", [128, NH], F32)
                yb = [sb(es, "yb%d" % i, [128, D], BF16) for i in range(2)]
                gain = sb(es, "gaint", [128, D], F32)
                yT = sb(es, "yT", [128, 8, 512], BF16)
                wout = sb(es, "wout", [128, 8, D], BF16)
                S.dma("sp", gain[:], gain_in[:, j, :], w=["gain"])
                S.dma("pool", wout[:], Wi[("a_w_out", j)].rearrange("(k p) n -> p k n", p=128), w=["wout"])
                stc = [0]

                def tile(xs, xregs, n, which, ta0, outs, oregs):
                    for s in range(n // 128):
                        st = stc[0] % 4
                        sy = stc[0] % 2
                        stc[0] += 1
                        row0 = ta0 + s * 128
                        S.dma("sp", hf[st][:], HD[0][row0:row0 + 128, :], w=[("hf", st)])
                        S.dma("sp", hb[st][:], HD[1][row0:row0 + 128, :], w=[("hb", st)])
                        S.dma("sp", so[st][:], SO[row0:row0 + 128, :], w=[("so", st)])
                        S.op("pool", "tensor_tensor", out=hs[:], in0=hf[st][:], in1=hb[st][:], op=ALU.add,
                             r=[("hf", st), ("hb", st)], w=["hs"])
                        for h in range(NH):
                            S.op("act", "activation", out=junk[:], in_=hs[:, h * DV:(h + 1) * DV], func=AF.Square,
                                 accum_out=ss[:, h:h + 1], r=["hs"], w=["junk", "ss"])
                        S.op("act", "activation", out=ss[:], in_=ss[:], func=AF.Sqrt, scale=1.0 / DV, bias=EPS,
                             r=["ss"], w=["ss"])
                        S.op("dve", "reciprocal", out=ss[:], in_=ss[:], r=["ss"], w=["ss"])
                        S.op("pool", "tensor_tensor", out=so[st][:], in0=so[st][:], in1=gain[:], op=ALU.mult,
                             r=[("so", st), "gain"], w=[("so", st)])
                        for h in range(NH):
                            S.op("dve", "scalar_tensor_tensor", out=yb[sy][:, h * DV:(h + 1) * DV],
                                 in0=hs[:, h * DV:(h + 1) * DV], scalar=ss[:, h:h + 1], op0=ALU.mult,
                                 in1=so[st][:, h * DV:(h + 1) * DV], op1=ALU.mult,
                                 r=["hs", "ss", ("so", st)], w=[("yb", sy)])
                        for c in range(8):
                            S.op("pe", "transpose", psb[:, c * 128:(c + 1) * 128], yb[sy][:, c * 128:(c + 1) * 128],
                                 identb[:], r=[("yb", sy), "identb"], w=["psb"])
                        S.op("act", "activation", out=yT[:, :, s * 128:(s + 1) * 128],
                             in_=psb[:].rearrange("p (c t) -> p c t", c=8), func=AF.Identity, r=["psb"], w=["yT"])
                    for mo in range(8):
                        pt = PS(mo % 4)
                        for c in range(8):
                            S.op("pe", "matmul", pt[:, 0:n], lhsT=wout[:, c, mo * 128:(mo + 1) * 128], rhs=yT[:, c, 0:n],
                                 start=(c == 0), stop=(c == 7), r=["wout", "yT"], w=[("ps", mo % 4)])
                        resid(outs[mo], pt[:, 0:n], MODL[:, 2, mo, which:which + 1], xs[mo],
                              r=[("ps", mo % 4), "mod", xregs[mo]], w=[oregs[mo]])

                if do_ctx:
                    tile([ctx[k][:, :] for k in range(8)], [("ctx", k) for k in range(8)], TC, 1, 0,
                         [ctx[k][:, :] for k in range(8)], [("ctx", k) for k in range(8)])
                for i in range(T // 512):
                    t0 = i * 512
                    S.dma("sp", xw[:], src[:, :, t0:t0 + 512].rearrange("k p t -> p k t"), w=["xw"])
                    tile([xw[:, k, :] for k in range(8)], ["xw"] * 8, 512, 0, TC + t0,
                         [xo[:, k, :] for k in range(8)], ["xo"] * 8)
                    S.dma("sp", dst[:, :, t0:t0 + 512].rearrange("k p t -> p k t"), xo[:], r=["xo"])
                S.flush()

        def phase_final(src):
            with ExitStack() as es:
                xw = sb(es, "xw", [128, 8, 512], F32)
                xo = sb(es, "xo", [128, 8, 512], F32)
                tmp = mk_tmp(es, 512)
                og, _ = PAR_OFF["gfin"]
                for i in range(T // 512):
                    t0 = i * 512
                    S.dma("sp", xw[:], src[:, :, t0:t0 + 512].rearrange("k p t -> p k t"), w=["xw"])
                    for k in range(8):
                        sq = tmp["sq%d" % (k % 2)]
                        S.op("act", "activation", out=sq[:], in_=xw[:, k, :], func=AF.Square, r=["xw"], w=[("sq", k % 2)])
                        S.op("pe", "matmul", PS(6)[:, :], lhsT=ones_f, rhs=sq[:], start=(k == 0), stop=(k == 7),
                             r=[("sq", k % 2), "cst"], w=[("ps", 6)])
                    std = tmp["std"]
                    S.op("act", "activation", out=std[:], in_=PS(6)[:, :], func=AF.Sqrt, scale=1.0 / D, bias=EPS,
                         r=[("ps", 6)], w=["std"])
                    S.op("dve", "reciprocal", out=std[:], in_=std[:], r=["std"], w=["std"])
                    for k in range(8):
                        S.op("dve", "scalar_tensor_tensor", out=xo[:, k, :], in0=xw[:, k, :],
                             scalar=par[:, og + k:og + k + 1], op0=ALU.mult, in1=std[:], op1=ALU.mult,
                             r=["xw", "par", "std"], w=["xo"])
                    S.dma("sp", outT[:, :, t0:t0 + 512].rearrange("k p t -> p k t"), xo[:], r=["xo"])
                S.flush()

        def halo_exchange(xsrc):
            def load(a):
                S.dma("sp", a.rearrange("p (k t) -> p k t", k=8),
                      xsrc[:, :, T - GW:T].rearrange("k p t -> p k t"), w=["hsrc"])
            exchange(hsrc, hdst, 8 * GW, load, halo[:].rearrange("p k t -> p (k t)"), "h")

        cur = xT_in
        last_mod = None
        for kind, l in plan:
            if last_mod != l:
                phase_mod(l)
                last_mod = l
            ctx_live = l < 2
            if kind == "A":
                phase_A1(l, cur, ctx_live)
                phase_scan(0, ctx_live)
                phase_scan(1, ctx_live)
                phase_A5(l, cur, XB, ctx_live)
                cur = XB
            elif kind == "B":
                phase_B(l, cur, XB, ctx_live)
                cur = XB
            elif kind == "F":
                need_weights(("f", l))
                halo_exchange(cur)
                phase_F(l, cur, XA, ctx_live)
                cur = XA
            elif kind == "M":
                pass
        if final:
            phase_final(cur)
        else:
            for k in range(8):
                S.dma("sp", outT[k], cur[k])
        for k in range(8):
            S.dma("sp", ctx_out[k], ctx[k][:], r=[("ctx", k)])
        S.flush()
        S.finish("sp")
        print("sched: ins=%d waits=%d cnt=%s" % (S.n_ins, S.n_wait, S.cnt))
    return nc


FULL_PLAN = [("A", 0), ("F", 0), ("B", 1), ("F", 1), ("A", 2), ("F", 2), ("B", 3), ("F", 3)]
_NC_CACHE = {}


def pack_par(b, odd, c, c_ctx, b_mod, g_mix, g_ffn, g_final, b_w_conv, f_w_conv, f_b_conv, a_b_gate):
    par = np.zeros((128, NPAR), np.float32)

    def put(name, arr):
        o, n = PAR_OFF[name]
        a = np.asarray(arr, np.float32).reshape(128, -1)
        assert a.shape[1] == n, (name, a.shape, n)
        par[:, o:o + n] = a
    bwc = np.asarray(b_w_conv, np.float32)
    fwc = np.asarray(f_w_conv, np.float32)
    put("cc", np.stack([fm(c[b]), fm(c_ctx)], axis=-1))
    put("bmod", fm(b_mod))
    put("gmix", fm(g_mix))
    put("gffn", fm(g_ffn))
    put("gfin", fm(g_final))
    put("bwc_l", fm(bwc))
    put("bwc_c", fm(bwc[:, ::-1] if odd else bwc))
    put("fwc_l", fm(fwc[:, ::-1] if odd else fwc))
    put("fwc_c", fm(fwc[:, ::-1] if odd else fwc))
    put("fbc", fm(f_b_conv))
    put("bgate", np.broadcast_to(gate_perm(np.asarray(a_b_gate, np.float32), odd).reshape(1, 32), (128, 32)))
    put("sel", np.broadcast_to(np.array([[1.0, 0.0]] if odd else [[0.0, 1.0]], np.float32), (128, 2)))
    return par


def gate_perm(g, odd):
    if not odd:
        return g
    sh = g.shape
    return np.ascontiguousarray(g.reshape(sh[:-1] + (2, 2, NH))[..., ::-1, :].reshape(sh))


def core_tokens(x_b, odd):
    r = np.asarray(x_b, np.float32).reshape(TFULL // GW, GW, D)
    r = r[NROW:][::-1] if odd else r[:NROW]
    return np.ascontiguousarray(r.reshape(T, D).T.reshape(8, 128, T))


def make_in_maps(inputs, cores, plan=None):
    maps = []
    gain = np.ascontiguousarray(np.broadcast_to(
        np.asarray(inputs["a_head_gain"], np.float32)[None], (128, 2, D)))
    wcache = {}
    for b, odd in cores:
        cx = np.asarray(inputs["ctx"][b], np.float32)
        if odd:
            cx = cx[::-1]
        m = {
            "xT": core_tokens(inputs["x"][b], odd),
            "ctxT": np.ascontiguousarray(cx.T.reshape(8, 128, TC)),
            "par": pack_par(b, odd, inputs["c"], inputs["c_ctx"], inputs["b_mod"], inputs["g_mix"], inputs["g_ffn"],
                            inputs["g_final"], inputs["b_w_conv"], inputs["f_w_conv"], inputs["f_b_conv"],
                            inputs["a_b_gate"]),
            "consts": make_consts(odd),
            "gain": gain,
        }
        for kind, l in (plan if plan is not None else FULL_PLAN):
            m["w_mod_%d" % l] = np.ascontiguousarray(inputs["w_mod"][l], np.float32)
            if kind == "A":
                j = l // 2
                key = ("awin", j, odd)
                if key not in wcache:
                    w = np.array(inputs["a_w_in"][j], np.float32)
                    w[:, 3072:] = gate_perm(w[:, 3072:], odd)
                    wcache[key] = w
                m["a_w_in_%d" % j] = wcache[key]
                m["a_w_out_%d" % j] = np.ascontiguousarray(inputs["a_w_out"][j], np.float32)
            elif kind == "B":
                m["b_w_in_%d" % (l // 2)] = np.ascontiguousarray(inputs["b_w_in"][l // 2], np.float32)
                m["b_w_out_%d" % (l // 2)] = np.ascontiguousarray(inputs["b_w_out"][l // 2], np.float32)
            elif kind == "F":
                m["f_w_up_%d" % l] = np.ascontiguousarray(inputs["f_w_up"][l], np.float32)
                m["f_w_down_%d" % l] = np.ascontiguousarray(inputs["f_w_down"][l], np.float32)
        maps.append(m)
    return maps


def assemble(results, cores, nb):
    out = np.empty((nb, TFULL, D), np.float32)
    for res, (b, odd) in zip(results, cores):
        o = res["outT"].reshape(D, T).T.reshape(NROW, GW, D)
        if odd:
            out[b, T:] = o[::-1].reshape(T, D)
        else:
            out[b, :T] = o.reshape(T, D)
    return out


def kernel(**inputs):
    key = "full"
    if key not in _NC_CACHE:
        _NC_CACHE[key] = build(FULL_PLAN, final=True, ncores=8)
    nc = _NC_CACHE[key]
    cores = [(b, odd) for b in range(4) for odd in (0, 1)]
    in_maps = make_in_maps(inputs, cores)
    res = run_bass_kernel_spmd(nc, in_maps, core_ids=list(range(8)))
    return assemble(res.results, cores, 4)
```

```python
import numpy as np
from contextlib import ExitStack
import ml_dtypes
import concourse.bass as bass
import concourse.mybir as mybir
from concourse.bass_utils import run_bass_kernel_spmd

F32 = mybir.dt.float32
BF16 = mybir.dt.bfloat16
AF = mybir.ActivationFunctionType
ALU = mybir.AluOpType

D = 1024
TFULL = 8192
T = 4096
TC = 256
TA = TC + T
DEPTH = 4
NH = 4
DK = 128
DV = 256
DVA = DV + 1
DFF = 2816
NJ = DFF // 128
GW = 64
CH = 64
NROW = T // GW
APROJ = 3088
EPS = 1e-6
NCH = TA // CH
SAME_ENG_SYNC = True


class Sched:
    ENGS = ("pe", "act", "dve", "pool", "sp")

    def __init__(self, nc, es, n_dma_sems=48, n_bg_sems=8):
        self.nc = nc
        self.eng = {"pe": nc.tensor, "act": nc.scalar, "dve": nc.vector,
                    "pool": nc.gpsimd, "sp": nc.sync}
        self.sems = []
        self.esem = {}
        for e in self.ENGS:
            self.esem[e] = len(self.sems)
            self.sems.append(es.enter_context(nc.semaphore("s_" + e)))
        self.dsem = []
        for i in range(n_dma_sems):
            self.dsem.append(len(self.sems))
            self.sems.append(es.enter_context(nc.semaphore("d%d" % i)))
        self.bsem = []
        for i in range(n_bg_sems):
            self.bsem.append(len(self.sems))
            self.sems.append(es.enter_context(nc.semaphore("b%d" % i)))
        self.brr = 0
        self.btarget = [0] * n_bg_sems
        self.bgev = {}
        self.ops = []
        self.cnt = {e: 0 for e in self.ENGS}
        self.waited = {e: {} for e in self.ENGS}
        self.pending = {e: {} for e in self.ENGS}
        self.rr = 0
        self.target = [0] * n_dma_sems
        self.n_ins = 0
        self.n_wait = 0

    def op(self, eng, meth, *args, r=(), w=(), **kw):
        self.ops.append(dict(eng=eng, meth=meth, args=args, kw=kw, r=tuple(r), w=tuple(w),
                             dma=False, signal=False, ev=None))

    def dma(self, eng, out, in_, r=(), w=(), bg=None):
        self.ops.append(dict(eng=eng, meth="dma_start", args=(), kw=dict(out=out, in_=in_),
                             r=tuple(r), w=tuple(w), dma=True, signal=True, ev=None, inc=16, bg=bg))

    def join_bg(self, group):
        for s, v in self.bgev.pop(group, []):
            for e in self.ENGS:
                if self.pending[e].get(s, 0) < v:
                    self.pending[e][s] = v

    def coll(self, ins, outs, groups, r=(), w=()):
        self.ops.append(dict(eng="pool", meth="collective_compute", args=("AllGather", ALU.bypass),
                             kw=dict(replica_groups=groups, ins=ins, outs=outs),
                             r=tuple(r), w=tuple(w), dma=True, signal=True, ev=None, inc=1))

    def flush(self):
        ops = self.ops
        self.ops = []
        state = {}
        deps = []
        last = {}
        for i, o in enumerate(ops):
            d = {}
            for r in o["r"]:
                st = state.get(r)
                if st is not None and st[0] is not None:
                    d[id(st[0])] = st[0]
            for r in o["w"]:
                st = state.get(r)
                if st is not None:
                    if st[0] is not None:
                        d[id(st[0])] = st[0]
                    for x in st[1].values():
                        d[id(x)] = x
                    for x in st[2]:
                        d[id(x)] = x
            for r in o["r"]:
                st = state.setdefault(r, [None, {}, []])
                if o["dma"]:
                    st[2].append(o)
                else:
                    st[1][o["eng"]] = o
            for r in o["w"]:
                st = state.setdefault(r, [None, {}, []])
                st[0] = o
                st[1] = {}
                st[2] = []
            d.pop(id(o), None)
            dd = []
            for oj in d.values():
                if (not oj["dma"]) and (not o["dma"]) and oj["eng"] == o["eng"] and \
                        (o["eng"] == "pe" or not SAME_ENG_SYNC):
                    continue
                dd.append(oj)
                oj["signal"] = True
            deps.append(dd)
            if not o["dma"]:
                last[o["eng"]] = o
        for o in last.values():
            o["signal"] = True
        nd = len(self.dsem)
        bar = {}
        for i, o in enumerate(ops):
            en = o["eng"]
            E = self.eng[en]
            need = dict(self.pending[en])
            self.pending[en] = {}
            for oj in deps[i]:
                s, v = oj["ev"]
                if need.get(s, 0) < v:
                    need[s] = v
            bgd = o["dma"] and o.get("bg") is not None
            if bgd:
                k = self.brr
                self.brr = (self.brr + 1) % len(self.bsem)
                s = self.bsem[k]
                if self.btarget[k] > 0 and need.get(s, 0) < self.btarget[k]:
                    need[s] = self.btarget[k]
            elif o["dma"]:
                k = self.rr
                self.rr = (self.rr + 1) % nd
                s = self.dsem[k]
                if self.target[k] > 0 and need.get(s, 0) < self.target[k]:
                    need[s] = self.target[k]
            for s, v in need.items():
                if self.waited[en].get(s, 0) < v:
                    E.wait_ge(self.sems[s], v)
                    self.waited[en][s] = v
                    self.n_wait += 1
            ins = getattr(E, o["meth"])(*o["args"], **o["kw"])
            self.n_ins += 1
            if bgd:
                self.btarget[k] += 16
                ins.then_inc(self.sems[self.bsem[k]], 16)
                o["ev"] = (self.bsem[k], self.btarget[k])
                self.bgev.setdefault(o["bg"], []).append(o["ev"])
            elif o["dma"]:
                self.target[k] += o["inc"]
                if o["inc"] == 16:
                    ins.then_inc(self.sems[self.dsem[k]], 16)
                else:
                    ins.then_inc(self.sems[self.dsem[k]])
                o["ev"] = (self.dsem[k], self.target[k])
                bar[self.dsem[k]] = self.target[k]
            elif o["signal"]:
                self.cnt[en] += 1
                ins.then_inc(self.sems[self.esem[en]], 1)
                o["ev"] = (self.esem[en], self.cnt[en])
                bar[self.esem[en]] = self.cnt[en]
        for e in self.ENGS:
            p = self.pending[e]
            for s, v in bar.items():
                if p.get(s, 0) < v:
                    p[s] = v

    def finish(self, eng="sp"):
        E = self.eng[eng]
        for s, v in self.pending[eng].items():
            if self.waited[eng].get(s, 0) < v:
                E.wait_ge(self.sems[s], v)
                self.waited[eng][s] = v


def _par_layout():
    off = {}
    o = 0
    for name, n in (("cc", 16), ("bmod", DEPTH * 48), ("gmix", DEPTH * 8), ("gffn", DEPTH * 8),
                    ("gfin", 8), ("bwc_l", 2 * 3 * 8), ("bwc_c", 2 * 3 * 8), ("fwc_l", DEPTH * 3 * NJ),
                    ("fwc_c", DEPTH * 3 * NJ), ("fbc", DEPTH * NJ), ("bgate", 2 * 16), ("sel", 2)):
        off[name] = (o, n)
        o += n
    return off, o


PAR_OFF, NPAR = _par_layout()
C_ONES, C_TRIF, C_TRIB, C_BLK0, C_BLK1, C_ID, C_MF, C_MB, C_TRL0, C_TRL1, C_ML0, C_ML1, NCONST = \
    0, 128, 256, 384, 512, 640, 768, 832, 896, 1024, 1152, 1216, 1280


def fm(v):
    v = np.asarray(v, np.float32)
    lead = v.shape[:-1]
    k = v.shape[-1] // 128
    a = v.reshape(lead + (k, 128))
    a = np.moveaxis(a, -1, 0)
    return np.ascontiguousarray(a)


def make_consts(odd):
    c = np.zeros((128, NCONST), np.float32)
    c[:, C_ONES:C_ONES + 128] = 1.0
    s = np.arange(128)[:, None]
    t = np.arange(128)[None, :]
    same = (s // 64) == (t // 64)
    c[:, C_TRIF:C_TRIF + 128] = (same & (s <= t))
    c[:, C_TRIB:C_TRIB + 128] = (same & (s >= t))
    c[:, C_BLK0:C_BLK0 + 128] = (s < 64)
    c[:, C_BLK1:C_BLK1 + 128] = (s >= 64)
    c[:, C_ID:C_ID + 128] = (s == t)
    s6 = np.arange(128)[:, None] % 64
    t6 = np.arange(64)[None, :]
    c[:, C_MF:C_MF + 64] = (s6 <= t6)
    c[:, C_MB:C_MB + 64] = (s6 >= t6)
    f0, f1 = (C_TRIB, C_TRIF) if odd else (C_TRIF, C_TRIB)
    c[:, C_TRL0:C_TRL0 + 128] = c[:, f0:f0 + 128]
    c[:, C_TRL1:C_TRL1 + 128] = c[:, f1:f1 + 128]
    m0, m1 = (C_MB, C_MF) if odd else (C_MF, C_MB)
    c[:, C_ML0:C_ML0 + 64] = c[:, m0:m0 + 64]
    c[:, C_ML1:C_ML1 + 64] = c[:, m1:m1 + 64]
    return c


def build(plan, final=True, ncores=8):
    nc = bass.Bass("TRN2", target_bir_lowering=False)

    def dram(name, shape, dtype, kind):
        return nc.dram_tensor(name, list(shape), dtype, kind=kind).ap()

    xT_in = dram("xT", [8, 128, T], F32, "ExternalInput")
    ctxT_in = dram("ctxT", [8, 128, TC], F32, "ExternalInput")
    par_in = dram("par", [128, NPAR], F32, "ExternalInput")
    const_in = dram("consts", [128, NCONST], F32, "ExternalInput")
    gain_in = dram("gain", [128, 2, D], F32, "ExternalInput")
    need_w = set()
    for kind, l in plan:
        need_w.add(("mod", l))
        if kind == "A":
            need_w.add(("a", l // 2))
        elif kind == "B":
            need_w.add(("b", l // 2))
        elif kind == "F":
            need_w.add(("f", l))
    Wi, Wb = {}, {}
    for kind, l in sorted(need_w):
        if kind == "mod":
            specs = [("w_mod", [D, 6 * D])]
        elif kind == "a":
            specs = [("a_w_in", [D, APROJ]), ("a_w_out", [D, D])]
        elif kind == "b":
            specs = [("b_w_in", [D, 3 * D]), ("b_w_out", [D, D])]
        else:
            specs = [("f_w_up", [D, 2 * DFF]), ("f_w_down", [DFF, D])]
        for nm, shp in specs:
            Wi[(nm, l)] = dram("%s_%d" % (nm, l), shp, F32, "ExternalInput")
            if nm == "f_w_up":
                Wb[(nm, l)] = dram("%s_%d_b" % (nm, l), shp, BF16, "Internal")
    outT = dram("outT", [8, 128, T], F32, "ExternalOutput")
    ctx_out = dram("ctx_out", [8, 128, TC], F32, "ExternalOutput")
    XA = dram("XA", [8, 128, T], F32, "Internal")
    XB = dram("XB", [8, 128, T], F32, "Internal")
    QK = dram("QK", [8, 128, TA], BF16, "Internal")
    KT = dram("KT", [TA, NH * DK], F32, "Internal")
    VA = dram("VA", [TA, NH * DV], BF16, "Internal")
    SO = dram("SO", [TA, D], F32, "Internal")
    SC = dram("SC", [TA, 16], F32, "Internal")
    HD = [dram("HF", [TA, D], F32, "Internal"), dram("HB", [TA, D], F32, "Internal")]
    NST = NH * DV + NH
    csrc = nc.dram_tensor("csrc", [128, NST], F32)
    cdst = nc.dram_tensor("cdst", [256, NST], F32)
    hsrc = nc.dram_tensor("hsrc", [128, 8 * GW], F32)
    hdst = nc.dram_tensor("hdst", [256, 8 * GW], F32)
    PAIRS = [[2 * i, 2 * i + 1] for i in range(ncores // 2)]

    ges = ExitStack()
    with ges:
        S = Sched(nc, ges)

        uid = [0]

        def sb(es, name, shape, dtype):
            uid[0] += 1
            return es.enter_context(nc.sbuf_tensor("%s_u%d" % (name, uid[0]), list(shape), dtype))

        par = sb(ges, "par", [128, NPAR], F32)
        cst = sb(ges, "cst", [128, NCONST], F32)
        identb = sb(ges, "identb", [128, 128], BF16)
        MODL = sb(ges, "modl", [128, 6, 8, 2], F32)
        ctx = [sb(ges, "ctx%d" % k, [128, TC], F32) for k in range(8)]
        EG = sb(ges, "eg", [128, NCH, 8], F32)
        halo = sb(ges, "halo", [128, 8, GW], F32)
        crecv = sb(ges, "crecv", [128, NST], F32)
        osel, _ = PAR_OFF["sel"]
        sel0 = par[:, osel:osel + 1]
        sel1 = par[:, osel + 1:osel + 2]

        def exchange(src_d, dst_d, n, load_src, out_ap, tag):
            with ExitStack() as es:
                two = sb(es, "xch2", [128, 2, n], F32)
                tmpx = sb(es, "xcht", [128, n], F32)
                load_src(src_d.ap())
                S.coll([src_d.ap().opt()], [dst_d.ap().opt()], PAIRS, r=[tag + "src"], w=[tag + "dst"])
                S.dma("sp", two[:], dst_d.ap().rearrange("(r p) n -> p r n", p=128), r=[tag + "dst"], w=["xch2"])
                S.op("dve", "tensor_scalar", out=tmpx[:], in0=two[:, 0, :], scalar1=sel0, scalar2=None,
                     op0=ALU.mult, r=["xch2", "par"], w=["xcht"])
                S.op("dve", "scalar_tensor_tensor", out=out_ap, in0=two[:, 1, :], scalar=sel1, op0=ALU.mult,
                     in1=tmpx[:], op1=ALU.add, r=["xch2", "xcht", "par"], w=[tag + "recv"])
                S.flush()
        psf = ges.enter_context(nc.psum_tensor("psf", [128, 7, 512], F32))
        psb = ges.enter_context(nc.psum_tensor("psb", [128, 1024], BF16))

        def PS(b):
            return psf[:, b, :]

        def pcol(name, idx=0):
            o, n = PAR_OFF[name]
            return o + idx

        onesb128 = sb(ges, "onesb128", [128, 128], BF16)
        ones_f = onesb128[:]

        S.dma("sp", par[:], par_in[:, :], w=["par"])
        S.dma("sp", cst[:], const_in[:, :], w=["cst"])
        for k in range(8):
            S.dma("sp", ctx[k][:], ctxT_in[k], w=[("ctx", k)])
        S.op("dve", "tensor_copy", out=identb[:], in_=cst[:, C_ID:C_ID + 128], r=["cst"], w=["identb"])
        S.op("dve", "tensor_copy", out=onesb128[:], in_=cst[:, C_ONES:C_ONES + 128], r=["cst"], w=["onesb128"])
        def cast_rows(dst, src, nrows, bg):
            for r0 in range(0, nrows, 128):
                S.dma("pool", dst[r0:r0 + 128, :], src[r0:r0 + 128, :], bg=bg)

        fgroups = []
        for kind, l in plan:
            if kind == "F" and ("f", l) not in fgroups:
                fgroups.append(("f", l))
        joined = set()

        def need_weights(g):
            if g[0] == "f" and g not in joined:
                issue_bg_casts()
                joined.add(g)
                S.join_bg(g)

        S.flush()
        bg_done = [False]

        def issue_bg_casts():
            if bg_done[0]:
                return
            bg_done[0] = True
            for g in fgroups:
                key = ("f_w_up", g[1])
                cast_rows(Wb[key], Wi[key], Wi[key].shape[0], g)

        def mk_tmp(es, n):
            t = {nm: sb(es, nm, [128, n], F32) for nm in ("std", "nt0", "nt1")}
            t["sq0"] = sb(es, "sq0", [128, n], BF16)
            t["sq1"] = sb(es, "sq1", [128, n], BF16)
            return t

        def norm_mod(xs, xregs, n, which, s_gs, s_sh, hx, tmp):
            pieces = [(c0, min(512, n - c0)) for c0 in range(0, n, 512)]
            for k in range(8):
                sq = tmp["sq%d" % (k % 2)]
                S.op("act", "activation", out=sq[:, 0:n], in_=xs[k], func=AF.Square,
                     r=[xregs[k]], w=[("sq", k % 2)])
                for pi, (c0, cn) in enumerate(pieces):
                    S.op("pe", "matmul", PS(6 - pi)[:, 0:cn], lhsT=ones_f, rhs=sq[:, c0:c0 + cn],
                         start=(k == 0), stop=(k == 7), r=[("sq", k % 2), "cst"], w=[("ps", 6 - pi)])
            std = tmp["std"]
            for pi, (c0, cn) in enumerate(pieces):
                S.op("act", "activation", out=std[:, c0:c0 + cn], in_=PS(6 - pi)[:, 0:cn], func=AF.Sqrt,
                     scale=1.0 / D, bias=EPS, r=[("ps", 6 - pi)], w=["std"])
            S.op("dve", "reciprocal", out=std[:, 0:n], in_=std[:, 0:n], r=["std"], w=["std"])
            for k in range(8):
                tt = tmp["nt%d" % (k % 2)]
                S.op("dve", "tensor_tensor", out=tt[:, 0:n], in0=xs[k], in1=std[:, 0:n], op=ALU.mult,
                     r=[xregs[k], "std"], w=[("nt", k % 2)])
                S.op("act", "activation", out=hx[k][:, 0:n], in_=tt[:, 0:n], func=AF.Identity,
                     scale=MODL[:, s_gs, k, which:which + 1], bias=MODL[:, s_sh, k, which:which + 1],
                     r=[("nt", k % 2), "mod"], w=[("hx", k)])

        def resid(out_ap, ps_ap, gcol, x_ap, r, w):
            S.op("dve", "scalar_tensor_tensor", out=out_ap, in0=ps_ap, scalar=gcol, op0=ALU.mult,
                 in1=x_ap, op1=ALU.add, r=r, w=w)

        def v3(ap, rowlen):
            return ap.rearrange("p (a b) -> p a b", b=rowlen)

        def phase_mod(l):
            with ExitStack() as es:
                scb = sb(es, "scb", [128, 8, 2], BF16)
                wm = [sb(es, "wm%d" % i, [128, 8, D], BF16) for i in range(6)]
                t1 = sb(es, "modt1", [128, 8, 2], F32)
                o, _ = PAR_OFF["cc"]
                S.op("act", "activation", out=scb[:].rearrange("p k w -> p (k w)"), in_=par[:, o:o + 16],
                     func=AF.Silu, r=["par"], w=["scb"])
                for s in range(6):
                    S.dma("pool", wm[s][:], Wi[("w_mod", l)][:, s * D:(s + 1) * D].rearrange("(k p) n -> p k n", p=128),
                          w=[("wm", s)])
                for s in range(6):
                    wt = wm[s]
                    pst = PS(s % 2)
                    for m in range(8):
                        for k in range(8):
                            S.op("pe", "matmul", pst[:, m * 2:m * 2 + 2], lhsT=wt[:, k, m * 128:(m + 1) * 128],
                                 rhs=scb[:, k, :], start=(k == 0), stop=(k == 7),
                                 r=[("wm", s), "scb"], w=[("ps", s % 2)])
                    ob, _ = PAR_OFF["bmod"]
                    bcol = par[:, ob + l * 48 + s * 8: ob + l * 48 + s * 8 + 8]
                    S.op("dve", "tensor_tensor", out=MODL[:, s, :, :],
                         in0=pst[:, 0:16].rearrange("p (m w) -> p m w", w=2),
                         in1=bcol.unsqueeze(2).to_broadcast([128, 8, 2]), op=ALU.add,
                         r=[("ps", s % 2), "par"], w=["mod"])
                for s, gname in ((1, "gmix"), (4, "gffn")):
                    og, _ = PAR_OFF[gname]
                    gcol = par[:, og + l * 8: og + l * 8 + 8]
                    S.op("dve", "tensor_scalar", out=t1[:], in0=MODL[:, s, :, :], scalar1=1.0, scalar2=None,
                         op0=ALU.add, r=["mod"], w=["modt1"])
                    S.op("dve", "tensor_tensor", out=MODL[:, s, :, :], in0=t1[:],
                         in1=gcol.unsqueeze(2).to_broadcast([128, 8, 2]), op=ALU.mult,
                         r=["modt1", "par"], w=["mod"])
                S.flush()

        def phase_B(l, src, dst, do_ctx):
            j = l // 2
            with ExitStack() as es:
                xw = sb(es, "xw", [128, 8, 512], F32)
                xo = sb(es, "xo", [128, 8, 512], F32)
                hx = [sb(es, "hx%d" % k, [128, 512], BF16) for k in range(8)]
                tmp = mk_tmp(es, 512)
                win = sb(es, "win", [128, 8, 3 * D], BF16)
                wout = sb(es, "wout", [128, 8, D], BF16)
                xv_sb = [sb(es, "xv%d" % i, [128, 512], F32) for i in range(2)]
                m_sb = [sb(es, "m%d" % i, [128, 512], F32) for i in range(2)]
                acc = [sb(es, "acc%d" % i, [128, 512], F32) for i in range(2)]
                z = [sb(es, "z%d" % k, [128, 512], BF16) for k in range(8)]
                for pc in range(3):
                    S.dma("pool", win[:, :, pc * D:(pc + 1) * D],
                          Wi[("b_w_in", j)][:, pc * D:(pc + 1) * D].rearrange("(k p) n -> p k n", p=128), w=[("win", pc)])
                S.dma("pool", wout[:], Wi[("b_w_out", j)].rearrange("(k p) n -> p k n", p=128), w=["wout"])
                def tile(xs, xregs, n, rowlen, which, outs, oregs):
                    ow, _ = PAR_OFF["bwc_c" if which == 1 else "bwc_l"]
                    norm_mod(xs, xregs, n, which, 1, 0, hx, tmp)
                    for c in range(8):
                        pr = c % 2
                        pb, pc_, pv = PS(3 * pr), PS(3 * pr + 1), PS(3 * pr + 2)
                        for gi, pt in enumerate((pb, pc_, pv)):
                            for k in range(8):
                                S.op("pe", "matmul", pt[:, 0:n],
                                     lhsT=win[:, k, gi * D + c * 128: gi * D + (c + 1) * 128], rhs=hx[k][:, 0:n],
                                     start=(k == 0), stop=(k == 7),
                                     r=[("win", gi), ("hx", k)], w=[("ps", 3 * pr + gi)])
                        S.op("act", "activation", out=xv_sb[pr][:, 0:n], in_=pv[:, 0:n], func=AF.Identity,
                             r=[("ps", 3 * pr + 2)], w=[("xv", pr)])
                        S.op("dve", "tensor_tensor", out=m_sb[pr][:, 0:n], in0=pc_[:, 0:n], in1=xv_sb[pr][:, 0:n],
                             op=ALU.mult, r=[("ps", 3 * pr + 1), ("xv", pr)], w=[("m", pr)])
                        w0 = par[:, ow + (j * 3 + 0) * 8 + c: ow + (j * 3 + 0) * 8 + c + 1]
                        w1 = par[:, ow + (j * 3 + 1) * 8 + c: ow + (j * 3 + 1) * 8 + c + 1]
                        w2 = par[:, ow + (j * 3 + 2) * 8 + c: ow + (j * 3 + 2) * 8 + c + 1]
                        S.op("act", "activation", out=acc[pr][:, 0:n], in_=m_sb[pr][:, 0:n], func=AF.Identity,
                             scale=w1, r=[("m", pr), "par"], w=[("acc", pr)])
                        a3 = v3(acc[pr][:, 0:n], rowlen)
                        m3 = v3(m_sb[pr][:, 0:n], rowlen)
                        S.op("dve", "scalar_tensor_tensor", out=a3[:, :, 1:rowlen], in0=m3[:, :, 0:rowlen - 1],
                             scalar=w0, op0=ALU.mult, in1=a3[:, :, 1:rowlen], op1=ALU.add,
                             r=[("m", pr), ("acc", pr), "par"], w=[("acc", pr)])
                        S.op("dve", "scalar_tensor_tensor", out=a3[:, :, 0:rowlen - 1], in0=m3[:, :, 1:rowlen],
                             scalar=w2, op0=ALU.mult, in1=a3[:, :, 0:rowlen - 1], op1=ALU.add,
                             r=[("m", pr), ("acc", pr), "par"], w=[("acc", pr)])
                        S.op("dve", "tensor_tensor", out=z[c][:, 0:n], in0=pb[:, 0:n], in1=acc[pr][:, 0:n],
                             op=ALU.mult, r=[("ps", 3 * pr), ("acc", pr)], w=[("z", c)])
                    for mo in range(8):
                        pt = PS(mo % 6)
                        for c in range(8):
                            S.op("pe", "matmul", pt[:, 0:n], lhsT=wout[:, c, mo * 128:(mo + 1) * 128],
                                 rhs=z[c][:, 0:n], start=(c == 0), stop=(c == 7),
                                 r=["wout", ("z", c)], w=[("ps", mo % 6)])
                        resid(outs[mo], pt[:, 0:n], MODL[:, 2, mo, which:which + 1], xs[mo],
                              r=[("ps", mo % 6), "mod", xregs[mo]], w=[oregs[mo]])

                if do_ctx:
                    tile([ctx[k][:, :] for k in range(8)], [("ctx", k) for k in range(8)], TC, TC, 1,
                         [ctx[k][:, :] for k in range(8)], [("ctx", k) for k in range(8)])
                xw2 = sb(es, "xw2", [128, 8, 512], F32)
                xws = [xw, xw2]

                def load(i):
                    S.dma("sp", xws[i % 2][:], src[:, :, i * 512:(i + 1) * 512].rearrange("k p t -> p k t"),
                          w=[("xw", i % 2)])
                load(0)
                for i in range(T // 512):
                    t0 = i * 512
                    if i + 1 < T // 512:
                        load(i + 1)
                    xb = xws[i % 2]
                    tile([xb[:, k, :] for k in range(8)], [("xw", i % 2)] * 8, 512, GW, 0,
                         [xo[:, k, :] for k in range(8)], ["xo"] * 8)
                    S.dma("sp", dst[:, :, t0:t0 + 512].rearrange("k p t -> p k t"), xo[:], r=["xo"])
                S.flush()

        def phase_F(l, src, dst, do_ctx):
            with ExitStack() as es:
                NW = 640
                xw = sb(es, "xw", [128, 8, NW], F32)
                xo = sb(es, "xo", [128, 8, 512], F32)
                hx = [sb(es, "hx%d" % k, [128, NW], BF16) for k in range(8)]
                tmp = mk_tmp(es, NW)
                wg = [sb(es, "wg%d" % i, [128, 8, 256], BF16) for i in range(2)]
                wv = [sb(es, "wv%d" % i, [128, 8, 256], BF16) for i in range(2)]
                wdn = sb(es, "wdn", [128, NJ, D], BF16)
                acc = [sb(es, "acc%d" % i, [128, 512], F32) for i in range(2)]
                sil = [sb(es, "sil%d" % i, [128, 512], F32) for i in range(2)]
                act = [sb(es, "act%d" % jj, [128, 512], BF16) for jj in range(NJ)]
                for jb in range(0, NJ, 11):
                    S.dma("pool", wdn[:, jb:jb + 11, :],
                          Wi[("f_w_down", l)][jb * 128:(jb + 11) * 128, :].rearrange("(j p) n -> p j n", p=128),
                          w=[("wdn", jb)])
                obc, _ = PAR_OFF["fbc"]

                def tile(xs, xregs, nw, co, n, shift, which, outs, oregs):
                    owc, _ = PAR_OFF["fwc_c" if which == 1 else "fwc_l"]
                    lo_ok = co >= shift
                    hi_ok = nw >= co + n + shift
                    norm_mod(xs, xregs, nw, which, 4, 3, hx, tmp)
                    gp = [(c0, min(512, nw - c0)) for c0 in range(0, nw, 512)]
                    for jj in range(NJ):
                        pr = jj % 2
                        if jj % 2 == 0:
                            wb = (jj // 2) % 2
                            S.dma("sp", wg[wb][:], Wb[("f_w_up", l)][:, jj * 128: jj * 128 + 256].rearrange(
                                "(k p) n -> p k n", p=128), w=[("wg", wb)])
                            S.dma("sp", wv[wb][:], Wb[("f_w_up", l)][:, DFF + jj * 128: DFF + jj * 128 + 256].rearrange(
                                "(k p) n -> p k n", p=128), w=[("wv", wb)])
                        wb = (jj // 2) % 2
                        wo_ = (jj % 2) * 128
                        gflat = psf[:, 2 * pr:2 * pr + 2, :].rearrange("p b c -> p (b c)")
                        pv = PS(4 + pr)
                        for pi, (c0, cn) in enumerate(gp):
                            for k in range(8):
                                S.op("pe", "matmul", PS(2 * pr + pi)[:, 0:cn], lhsT=wg[wb][:, k, wo_:wo_ + 128],
                                     rhs=hx[k][:, c0:c0 + cn], start=(k == 0), stop=(k == 7),
                                     r=[("wg", wb), ("hx", k)], w=[("ps", 2 * pr + pi)])
                        for k in range(8):
                            S.op("pe", "matmul", pv[:, 0:n], lhsT=wv[wb][:, k, wo_:wo_ + 128],
                                 rhs=hx[k][:, co:co + n], start=(k == 0), stop=(k == 7),
                                 r=[("wv", wb), ("hx", k)], w=[("ps", 4 + pr)])
                        greg = [("ps", 2 * pr), ("ps", 2 * pr + 1)]
                        w0 = par[:, owc + (l * 3 + 0) * NJ + jj: owc + (l * 3 + 0) * NJ + jj + 1]
                        w1 = par[:, owc + (l * 3 + 1) * NJ + jj: owc + (l * 3 + 1) * NJ + jj + 1]
                        w2 = par[:, owc + (l * 3 + 2) * NJ + jj: owc + (l * 3 + 2) * NJ + jj + 1]
                        bc = par[:, obc + l * NJ + jj: obc + l * NJ + jj + 1]
                        a = acc[pr]
                        S.op("dve", "tensor_scalar", out=a[:, 0:n], in0=gflat[:, co:co + n], scalar1=w1, scalar2=None,
                             op0=ALU.mult, r=greg + ["par"], w=[("acc", pr)])
                        if lo_ok:
                            o0, o1, s0 = 0, n, co - shift
                        else:
                            o0, o1, s0 = shift, n, co
                        S.op("dve", "scalar_tensor_tensor", out=a[:, o0:o1], in0=gflat[:, s0:s0 + (o1 - o0)],
                             scalar=w0, op0=ALU.mult, in1=a[:, o0:o1], op1=ALU.add,
                             r=greg + ["par", ("acc", pr)], w=[("acc", pr)])
                        if hi_ok:
                            o0, o1 = 0, n
                        else:
                            o0, o1 = 0, n - shift
                        S.op("dve", "scalar_tensor_tensor", out=a[:, o0:o1],
                             in0=gflat[:, co + shift:co + shift + (o1 - o0)],
                             scalar=w2, op0=ALU.mult, in1=a[:, o0:o1], op1=ALU.add,
                             r=greg + ["par", ("acc", pr)], w=[("acc", pr)])
                        S.op("act", "activation", out=sil[pr][:, 0:n], in_=a[:, 0:n], func=AF.Silu, bias=bc,
                             r=[("acc", pr), "par"], w=[("sil", pr)])
                        S.op("dve", "tensor_tensor", out=act[jj][:, 0:n], in0=pv[:, 0:n], in1=sil[pr][:, 0:n],
                             op=ALU.mult, r=[("ps", 4 + pr), ("sil", pr)], w=[("act", jj)])
                    for mo in range(8):
                        pt = PS(mo % 4)
                        for jj in range(NJ):
                            S.op("pe", "matmul", pt[:, 0:n], lhsT=wdn[:, jj, mo * 128:(mo + 1) * 128],
                                 rhs=act[jj][:, 0:n], start=(jj == 0), stop=(jj == NJ - 1),
                                 r=[("wdn", 0), ("wdn", 11), ("act", jj)], w=[("ps", mo % 4)])
                        resid(outs[mo], pt[:, 0:n], MODL[:, 5, mo, which:which + 1], xs[mo][:, co:co + n],
                              r=[("ps", mo % 4), "mod", xregs[mo]], w=[oregs[mo]])

                if do_ctx:
                    tile([ctx[k][:, :] for k in range(8)], [("ctx", k) for k in range(8)], TC, 0, TC, 1, 1,
                         [ctx[k][:, :] for k in range(8)], [("ctx", k) for k in range(8)])
                xw2 = sb(es, "xw2", [128, 8, NW], F32)
                xws = [xw, xw2]

                def load(i):
                    r0 = i * 8
                    wlo, whi = max(0, r0 - 1), min(NROW, r0 + 9)
                    nw = (whi - wlo) * GW
                    co = (r0 - wlo) * GW
                    xb, reg = xws[i % 2], ("xw", i % 2)
                    S.dma("sp", xb[:, :, 0:nw], src[:, :, wlo * GW:whi * GW].rearrange("k p t -> p k t"), w=[reg])
                    if r0 + 9 > NROW:
                        S.op("act", "activation", out=xb[:, :, nw:nw + GW], in_=halo[:], func=AF.Identity,
                             r=["hrecv"], w=[reg])
                        nw += GW
                    return xb, reg, nw, co
                nt = NROW // 8
                nxt = load(0)
                for i in range(nt):
                    r0 = i * 8
                    xb, reg, nw, co = nxt
                    if i + 1 < nt:
                        nxt = load(i + 1)
                    tile([xb[:, k, 0:nw] for k in range(8)], [reg] * 8, nw, co, 512, GW, 0,
                         [xo[:, k, :] for k in range(8)], ["xo"] * 8)
                    S.dma("sp", dst[:, :, r0 * GW:r0 * GW + 512].rearrange("k p t -> p k t"), xo[:], r=["xo"])
                S.flush()

        def phase_A1(l, src, emit_ctx):
            j = l // 2
            with ExitStack() as es:
                xw = sb(es, "xw", [128, 8, 512], F32)
                hx = [sb(es, "hx%d" % k, [128, 512], BF16) for k in range(8)]
                tmp = mk_tmp(es, 512)
                win = sb(es, "win", [128, 8, APROJ], BF16)
                qkb = sb(es, "qkb", [128, 8, 512], BF16)
                kt_sb = [sb(es, "kt%d" % i, [128, NH * DK], F32) for i in range(2)]
                va_sb = [sb(es, "va%d" % i, [128, NH * DV], BF16) for i in range(2)]
                so_sb = [sb(es, "so%d" % i, [128, D], F32) for i in range(2)]
                g_sb = [sb(es, "g%d" % i, [128, 16], F32) for i in range(2)]
                sp_sb = [sb(es, "sp%d" % i, [128, 8], F32) for i in range(2)]
                u_sb = [sb(es, "u%d" % i, [128, 8], F32) for i in range(2)]
                sc_sb = [sb(es, "sc%d" % i, [128, 16], F32) for i in range(2)]
                for pc, (c0, c1) in enumerate(((0, 1024), (1024, 2048), (2048, APROJ))):
                    S.dma("pool", win[:, :, c0:c1], Wi[("a_w_in", j)][:, c0:c1].rearrange("(k p) n -> p k n", p=128),
                          w=[("win", pc)])
                wreg = [("win", 0), ("win", 1), ("win", 2)]
                obg, _ = PAR_OFF["bgate"]
                bg = par[:, obg + j * 16: obg + j * 16 + 16]
                stc = [0]
                pendB = []

                def tile(xs, xregs, n, which, ta0, do_o):
                    tr0, tr1 = (C_TRIF, C_TRIB) if which == 1 else (C_TRL0, C_TRL1)
                    norm_mod(xs, xregs, n, which, 1, 0, hx, tmp)
                    for m in range(8):
                        pt = PS(m % 2)
                        for k in range(8):
                            S.op("pe", "matmul", pt[:, 0:n], lhsT=win[:, k, m * 128:(m + 1) * 128], rhs=hx[k][:, 0:n],
                                 start=(k == 0), stop=(k == 7), r=wreg + [("hx", k)], w=[("ps", m % 2)])
                        S.op("act", "activation", out=qkb[:, m, 0:n], in_=pt[:, 0:n], func=AF.Identity,
                             scale=(1.0 if m < 4 else DK ** -0.5), r=[("ps", m % 2)], w=["qkb"])
                    S.dma("sp", QK[:, :, ta0:ta0 + n].rearrange("k p t -> p k t"), qkb[:, :, 0:n], r=["qkb"])
                    for s in range(n // 128):
                        st = stc[0] % 2
                        stc[0] += 1
                        tsl = slice(s * 128, (s + 1) * 128)
                        row0 = ta0 + s * 128

                        def proj(bank, c0, cn):
                            for k in range(8):
                                S.op("pe", "matmul", PS(bank)[:, 0:cn], lhsT=hx[k][:, tsl], rhs=win[:, k, c0:c0 + cn],
                                     start=(k == 0), stop=(k == 7), r=wreg + [("hx", k)], w=[("ps", bank)])
                        proj(2, 512, 512)
                        S.op("act", "activation", out=kt_sb[st][:], in_=PS(2)[:, :], func=AF.Identity,
                             scale=DK ** -0.5, r=[("ps", 2)], w=[("kt", st)])
                        S.dma("sp", KT[row0:row0 + 128, :], kt_sb[st][:], r=[("kt", st)])
                        for pi in range(2):
                            proj(3 + pi, 1024 + pi * 512, 512)
                            S.op("act", "activation", out=va_sb[st][:, pi * 512:(pi + 1) * 512], in_=PS(3 + pi)[:, :],
                                 func=AF.Identity, r=[("ps", 3 + pi)], w=[("va", st)])
                        S.dma("sp", VA[row0:row0 + 128, :], va_sb[st][:], r=[("va", st)])
                        if do_o:
                            for pi in range(2):
                                proj(2 + 2 * pi, 2048 + pi * 512, 512)
                                S.op("act", "activation", out=so_sb[st][:, pi * 512:(pi + 1) * 512],
                                     in_=PS(2 + 2 * pi)[:, :], func=AF.Sigmoid, r=[("ps", 2 + 2 * pi)],
                                     w=[("so", st)])
                            S.dma("sp", SO[row0:row0 + 128, :], so_sb[st][:], r=[("so", st)])
                        proj(5, 3072, 16)
                        while pendB:
                            pendB.pop(0)()
                        S.op("dve", "tensor_tensor", out=g_sb[st][:], in0=PS(5)[:, 0:16], in1=bg, op=ALU.add,
                             r=[("ps", 5), "par"], w=[("g", st)])
                        S.op("act", "activation", out=sp_sb[st][:], in_=g_sb[st][:, 8:16], func=AF.Exp, scale=-1.0,
                             r=[("g", st)], w=[("sp", st)])
                        S.op("act", "activation", out=sp_sb[st][:], in_=sp_sb[st][:], func=AF.Ln, bias=1.0, scale=1.0,
                             r=[("sp", st)], w=[("sp", st)])
                        def partB(st=st, row0=row0, tr0=tr0, tr1=tr1):
                            S.op("pe", "matmul", PS(6)[:, 32:36], lhsT=cst[:, tr0:tr0 + 128], rhs=sp_sb[st][:, 0:4],
                                 start=True, stop=True, r=[("sp", st), "cst"], w=[("ps", 6)])
                            S.op("pe", "matmul", PS(6)[:, 36:40], lhsT=cst[:, tr1:tr1 + 128], rhs=sp_sb[st][:, 4:8],
                                 start=True, stop=True, r=[("sp", st), "cst"], w=[("ps", 6)])
                            S.op("pe", "matmul", PS(6)[:, 64:72], lhsT=cst[:, C_BLK0:C_BLK0 + 128], rhs=sp_sb[st][:, 0:8],
                                 start=True, stop=True, r=[("sp", st), "cst"], w=[("ps", 6)])
                            S.op("pe", "matmul", PS(6)[:, 72:80], lhsT=cst[:, C_BLK1:C_BLK1 + 128], rhs=sp_sb[st][:, 0:8],
                                 start=True, stop=True, r=[("sp", st), "cst"], w=[("ps", 6)])
                            S.op("dve", "tensor_tensor", out=u_sb[st][:], in0=PS(6)[:, 32:40], in1=g_sb[st][:, 0:8],
                                 op=ALU.add, r=[("ps", 6), ("g", st)], w=[("u", st)])
                            S.op("act", "activation", out=sc_sb[st][:, 0:8], in_=u_sb[st][:], func=AF.Exp,
                                 r=[("u", st)], w=[("sc", st)])
                            S.op("act", "activation", out=sc_sb[st][:, 8:16], in_=PS(6)[:, 32:40], func=AF.Exp,
                                 r=[("ps", 6)], w=[("sc", st)])
                            ch0 = row0 // CH
                            S.op("act", "activation", out=EG[:, ch0:ch0 + 2, :].rearrange("p c g -> p (c g)"),
                                 in_=PS(6)[:, 64:80], func=AF.Exp, scale=-1.0, r=[("ps", 6)], w=["eg"])
                            S.dma("sp", SC[row0:row0 + 128, :], sc_sb[st][:], r=[("sc", st)])
                        pendB.append(partB)
                    while pendB:
                        pendB.pop(0)()

                tile([ctx[k][:, :] for k in range(8)], [("ctx", k) for k in range(8)], TC, 1, 0, emit_ctx)
                xw2 = sb(es, "xw2", [128, 8, 512], F32)
                xws = [xw, xw2]

                def load(i):
                    S.dma("sp", xws[i % 2][:], src[:, :, i * 512:(i + 1) * 512].rearrange("k p t -> p k t"),
                          w=[("xw", i % 2)])
                load(0)
                for i in range(T // 512):
                    t0 = i * 512
                    if i + 1 < T // 512:
                        load(i + 1)
                    xb = xws[i % 2]
                    tile([xb[:, k, :] for k in range(8)], [("xw", i % 2)] * 8, 512, 0, TC + t0, True)
                S.flush()

        def phase_scan(d, emit_ctx):
            with ExitStack() as es:
                qk_t = [sb(es, "qkt%d" % i, [128, 8, 512], BF16) for i in range(2)]
                kt_t = [sb(es, "ktt%d" % i, [64, 8, NH * DK], F32) for i in range(2)]
                va_t = [sb(es, "vat%d" % i, [64, 8, NH * DV], BF16) for i in range(2)]
                sc_t = [sb(es, "sct%d" % i, [64, 8, 16], F32) for i in range(2)]
                hout = [sb(es, "hout%d" % i, [64, 8, D], F32) for i in range(2)]
                Ct = sb(es, "Ct", [128, NH, DV], F32)
                nst = sb(es, "nst", [128, NH], F32)
                Cb = [sb(es, "Cb%d" % i, [128, NH, DV], BF16) for i in range(2)]
                nb_ = [sb(es, "nb%d" % i, [128, NH], BF16) for i in range(2)]
                ntmp = sb(es, "ntmp", [128, NH], F32)
                pT = [sb(es, "pT%d" % i, [64, NH, 64], BF16) for i in range(2)]
                ks = [sb(es, "ks%d" % i, [64, NH * DK], BF16) for i in range(2)]
                absb = sb(es, "absb", [64, NH], F32)
                den = sb(es, "den", [64, NH], F32)
                onesb = sb(es, "onesb", [128, 2], BF16)
                S.op("dve", "memset", Ct[:], 0.0, w=[("Ct", h) for h in range(NH)])
                S.op("dve", "memset", nst[:], 0.0, w=["nst"])
                S.op("dve", "memset", Cb[0][:], 0.0, w=[("Cb", 0, h) for h in range(NH)])
                S.op("dve", "memset", nb_[0][:], 0.0, w=[("nb", 0)])
                S.op("dve", "memset", onesb[:], 1.0, w=["onesb"])
                issue_bg_casts()
                mc_ctx = C_MF if d == 0 else C_MB
                mc_lat = C_ML0 if d == 0 else C_ML1
                blocks = [(0, 4, True)] + [(TC + i * 512, 8, False) for i in range(T // 512)]
                if d == 1:
                    blocks = ([blocks[0]] if emit_ctx else []) + blocks[1:][::-1]
                chunks = []
                for bi, (ta0, nb, is_ctx) in enumerate(blocks):
                    order = list(range(nb)) if d == 0 else list(range(nb))[::-1]
                    for ci in order:
                        chunks.append(dict(bi=bi, bf=bi % 2, ta0=ta0, nb=nb, is_ctx=is_ctx, ci=ci,
                                           chg=ta0 // CH + ci, emit=((not is_ctx) or emit_ctx),
                                           first=(ci == order[0]), last=(ci == order[-1])))
                ones4 = cst[:, C_ONES:C_ONES + NH]
                SB = (0, 6)
                st = dict(prev=None, injected=(d == 0))

                def load_block(bi):
                    if bi >= len(blocks):
                        return
                    ta0, nb, _ = blocks[bi]
                    bf, ntok = bi % 2, nb * 64
                    if True:
                        S.dma("sp", qk_t[bf][:, :, 0:ntok], QK[:, :, ta0:ta0 + ntok].rearrange("k p t -> p k t"),
                              w=[("qkt", bf)])
                        S.dma("sp", kt_t[bf][:, 0:nb, :], KT[ta0:ta0 + ntok, :].rearrange("(c s) d -> s c d", s=64),
                              w=[("ktt", bf)])
                        S.dma("sp", va_t[bf][:, 0:nb, :], VA[ta0:ta0 + ntok, :].rearrange("(c s) d -> s c d", s=64),
                              w=[("vat", bf)])
                        S.dma("sp", sc_t[bf][:, 0:nb, :], SC[ta0:ta0 + ntok, :].rearrange("(c s) d -> s c d", s=64),
                              w=[("sct", bf)])

                def P1(ch, q):
                    bf, ci = ch["bf"], ch["ci"]
                    tsl = slice(ci * 64, (ci + 1) * 64)
                    if ch["emit"]:
                        for h in range(NH):
                            S.op("pe", "matmul", PS(SB[q % 2])[0:64, h * 64:(h + 1) * 64], lhsT=qk_t[bf][:, 4 + h, tsl],
                                 rhs=qk_t[bf][:, h, tsl], start=True, stop=True,
                                 r=[("qkt", bf)], w=[("ps", SB[q % 2])])
                    S.op("dve", "tensor_tensor", out=ks[q % 2][:].rearrange("p (h d) -> p h d", h=NH),
                         in0=kt_t[bf][:, ci, :].rearrange("p (h d) -> p h d", h=NH),
                         in1=sc_t[bf][:, ci, d * 4:d * 4 + 4].unsqueeze(2).to_broadcast([64, NH, DK]), op=ALU.mult,
                         r=[("ktt", bf), ("sct", bf)], w=[("ks", q % 2)])

                def P2(ch, q):
                    bf, ci = ch["bf"], ch["ci"]
                    mcol = mc_ctx if ch["is_ctx"] else mc_lat
                    mask = cst[0:64, mcol:mcol + 64]
                    if ch["emit"]:
                        for h in range(NH):
                            S.op("dve", "scalar_tensor_tensor", out=pT[q % 2][:, h, :],
                                 in0=PS(SB[q % 2])[0:64, h * 64:(h + 1) * 64],
                                 scalar=sc_t[bf][:, ci, d * 4 + h:d * 4 + h + 1],
                                 op0=ALU.mult, in1=mask, op1=ALU.mult,
                                 r=[("ps", SB[q % 2]), ("sct", bf), "cst"], w=[("pT", q % 2, h)])
                    for h in range(NH):
                        pu = PS(4 + h // 2)[:, (h % 2) * 256:(h % 2) * 256 + 256]
                        S.op("pe", "matmul", pu, lhsT=ks[q % 2][:, h * DK:(h + 1) * DK],
                             rhs=va_t[bf][:, ci, h * DV:(h + 1) * DV], start=True, stop=True,
                             r=[("ks", q % 2), ("vat", bf)], w=[("ps", 4 + h // 2)])
                    for h in range(NH):
                        S.op("pe", "matmul", PS(3)[:, 8 + h:9 + h], lhsT=ks[q % 2][:, h * DK:(h + 1) * DK],
                             rhs=onesb[0:64, 0:1], start=True, stop=True, r=[("ks", q % 2), "onesb"], w=[("psn",)])

                def P3(ch, q):
                    bf, ci, chg = ch["bf"], ch["ci"], ch["chg"]
                    tsl = slice(ci * 64, (ci + 1) * 64)
                    cur, nxt = q % 2, (q + 1) % 2
                    if not ch["is_ctx"] and not st["injected"]:
                        st["injected"] = True
                        st["prev"] = "ones"
                        S.op("dve", "tensor_copy", out=Ct[:].rearrange("p h v -> p (h v)"), in_=crecv[:, 0:NH * DV],
                             r=["crecv"], w=[("Ct", h) for h in range(NH)])
                        S.op("dve", "tensor_copy", out=nst[:], in_=crecv[:, NH * DV:NST], r=["crecv"], w=["nst"])
                        S.op("act", "activation", out=Cb[cur][:].rearrange("p h v -> p (h v)"),
                             in_=crecv[:, 0:NH * DV], func=AF.Identity, r=["crecv"], w=[("Cb", cur, h) for h in range(NH)])
                        S.op("act", "activation", out=nb_[cur][:], in_=crecv[:, NH * DV:NST], func=AF.Identity,
                             r=["crecv"], w=[("nb", cur)])
                    use_ones = (st["prev"] == "ones")
                    pgl = chg if (st["prev"] is None or use_ones) else st["prev"]
                    st["prev"] = chg
                    if ch["emit"]:
                        for h in range(NH):
                            pa = PS(1 + h // 2)[0:64, (h % 2) * 256:(h % 2) * 256 + 256]
                            S.op("pe", "matmul", pa, lhsT=pT[cur][:, h, :], rhs=va_t[bf][:, ci, h * DV:(h + 1) * DV],
                                 start=True, stop=False, r=[("pT", cur, h), ("vat", bf)], w=[("ps", 1 + h // 2)])
                            S.op("pe", "matmul", pa, lhsT=qk_t[bf][:, h, tsl], rhs=Cb[cur][:, h, :],
                                 start=False, stop=True, r=[("qkt", bf), ("Cb", cur, h)], w=[("ps", 1 + h // 2)])
                        for h in range(NH):
                            pbn = PS(3)[0:64, h:h + 1]
                            S.op("pe", "matmul", pbn, lhsT=pT[cur][:, h, :], rhs=onesb[0:64, 0:1],
                                 start=True, stop=False, r=[("pT", cur, h), "onesb"], w=[("psb4",)])
                            S.op("pe", "matmul", pbn, lhsT=qk_t[bf][:, h, tsl], rhs=nb_[cur][:, h:h + 1],
                                 start=False, stop=True, r=[("qkt", bf), ("nb", cur)], w=[("psb4",)])
                    egp = ones4 if use_ones else EG[:, pgl, d * 4:d * 4 + 4]
                    egc = EG[:, chg, d * 4:d * 4 + 4]
                    for h in range(NH):
                        pu = PS(4 + h // 2)[:, (h % 2) * 256:(h % 2) * 256 + 256]
                        S.op("dve", "scalar_tensor_tensor", out=Ct[:, h, :], in0=Ct[:, h, :],
                             scalar=(cst[:, C_ONES:C_ONES + 1] if use_ones else EG[:, pgl, d * 4 + h:d * 4 + h + 1]),
                             op0=ALU.mult, in1=pu, op1=ALU.add,
                             r=[("Ct", h), "eg", ("ps", 4 + h // 2)], w=[("Ct", h)])
                        S.op("act", "activation", out=Cb[nxt][:, h, :], in_=Ct[:, h, :], func=AF.Identity,
                             scale=EG[:, chg, d * 4 + h:d * 4 + h + 1], r=[("Ct", h), "eg"], w=[("Cb", nxt, h)])
                    S.op("dve", "tensor_tensor", out=ntmp[:], in0=nst[:], in1=egp, op=ALU.mult,
                         r=["nst", "eg"], w=["ntmp"])
                    S.op("dve", "tensor_tensor", out=nst[:], in0=PS(3)[:, 8:8 + NH], in1=ntmp[:], op=ALU.add,
                         r=[("psn",), "ntmp"], w=["nst"])
                    S.op("dve", "tensor_tensor", out=nb_[nxt][:], in0=nst[:], in1=egc, op=ALU.mult,
                         r=["nst", "eg"], w=[("nb", nxt)])

                def P4(ch, q):
                    bf, ci = ch["bf"], ch["ci"]
                    if not ch["emit"]:
                        return
                    ho = hout[ch["bi"] % 2]
                    S.op("act", "activation", out=absb[:], in_=PS(3)[0:64, 0:NH], func=AF.Abs,
                         r=[("psb4",)], w=["absb"])
                    S.op("dve", "tensor_tensor", out=den[:], in0=absb[:], in1=sc_t[bf][:, ci, 8 + d * 4:12 + d * 4],
                         op=ALU.max, r=["absb", ("sct", bf)], w=["den"])
                    S.op("dve", "reciprocal", out=den[:], in_=den[:], r=["den"], w=["den"])
                    for h in range(NH):
                        pa = PS(1 + h // 2)[0:64, (h % 2) * 256:(h % 2) * 256 + 256]
                        S.op("act", "activation", out=ho[:, ci, h * DV:(h + 1) * DV], in_=pa, func=AF.Identity,
                             scale=den[:, h:h + 1], r=[("ps", 1 + h // 2), "den"], w=[("hout", ch["bi"] % 2, h)])
                    if ch["last"]:
                        ta0, ntok, nb = ch["ta0"], ch["nb"] * 64, ch["nb"]
                        S.dma("sp", HD[d][ta0:ta0 + ntok, :].rearrange("(c s) v -> s c v", s=64), ho[:, 0:nb, :],
                              r=[("hout", ch["bi"] % 2, h) for h in range(NH)])

                load_block(0)
                load_block(1)
                P1(chunks[0], 0)
                P2(chunks[0], 0)
                for q, ch in enumerate(chunks):
                    P3(ch, q)
                    if q + 1 < len(chunks):
                        P1(chunks[q + 1], q + 1)
                    P4(ch, q)
                    if ch["last"]:
                        load_block(ch["bi"] + 2)
                    if q + 1 < len(chunks):
                        P2(chunks[q + 1], q + 1)
                if d == 0:
                    csend = sb(es, "csend", [128, NST], F32)
                    lastc = st["prev"]
                    for h in range(NH):
                        S.op("act", "activation", out=csend[:, h * DV:(h + 1) * DV], in_=Ct[:, h, :], func=AF.Identity,
                             scale=EG[:, lastc, d * 4 + h:d * 4 + h + 1], r=[("Ct", h), "eg"], w=["csend"])
                    S.op("dve", "tensor_tensor", out=csend[:, NH * DV:NST], in0=nst[:],
                         in1=EG[:, lastc, d * 4:d * 4 + 4], op=ALU.mult, r=["nst", "eg"], w=["csend"])
                    S.dma("sp", csrc.ap(), csend[:], r=["csend"], w=["csrc"])
                S.flush()
            if d == 0:
                exchange(csrc, cdst, NST, lambda a: None, crecv[:], "c")

        def phase_A5(l, src, dst, do_ctx):
            j = l // 2
            with ExitStack() as es:
                xw = sb(es, "xw", [128, 8, 512], F32)
                xo = sb(es, "xo", [128, 8, 512], F32)
                hf = [sb(es, "hf%d" % i, [128, D], F32) for i in range(4)]
                hb = [sb(es, "hb%d" % i, [128, D], F32) for i in range(4)]
                so = [sb(es, "so%d" % i, [128, D], F32) for i in range(4)]
                hs = sb(es, "hs", [128, D], F32)
                junk = sb(es, "junk", [128, DV], BF16)
                ss = sb(es, "ss", [128, NH], F32)
                yb = [sb(es, "yb%d" % i, [128, D], BF16) for i in range(2)]
                gain = sb(es, "gaint", [128, D], F32)
                yT = sb(es, "yT", [128, 8, 512], BF16)
                wout = sb(es, "wout", [128, 8, D], BF16)
                S.dma("sp", gain[:], gain_in[:, j, :], w=["gain"])
                S.dma("pool", wout[:], Wi[("a_w_out", j)].rearrange("(k p) n -> p k n", p=128), w=["wout"])
                stc = [0]

                def tile(xs, xregs, n, which, ta0, outs, oregs):
                    for s in range(n // 128):
                        st = stc[0] % 4
                        sy = stc[0] % 2
                        stc[0] += 1
                        row0 = ta0 + s * 128
                        S.dma("sp", hf[st][:], HD[0][row0:row0 + 128, :], w=[("hf", st)])
                        S.dma("sp", hb[st][:], HD[1][row0:row0 + 128, :], w=[("hb", st)])
                        S.dma("sp", so[st][:], SO[row0:row0 + 128, :], w=[("so", st)])
                        S.op("pool", "tensor_tensor", out=hs[:], in0=hf[st][:], in1=hb[st][:], op=ALU.add,
                             r=[("hf", st), ("hb", st)], w=["hs"])
                        for h in range(NH):
                            S.op("act", "activation", out=junk[:], in_=hs[:, h * DV:(h + 1) * DV], func=AF.Square,
                                 accum_out=ss[:, h:h + 1], r=["hs"], w=["junk", "ss"])
                        S.op("act", "activation", out=ss[:], in_=ss[:], func=AF.Sqrt, scale=1.0 / DV, bias=EPS,
                             r=["ss"], w=["ss"])
                        S.op("dve", "reciprocal", out=ss[:], in_=ss[:], r=["ss"], w=["ss"])
                        S.op("pool", "tensor_tensor", out=so[st][:], in0=so[st][:], in1=gain[:], op=ALU.mult,
                             r=[("so", st), "gain"], w=[("so", st)])
                        for h in range(NH):
                            S.op("dve", "scalar_tensor_tensor", out=yb[sy][:, h * DV:(h + 1) * DV],
                                 in0=hs[:, h * DV:(h + 1) * DV], scalar=ss[:, h:h + 1], op0=ALU.mult,
                                 in1=so[st][:, h * DV:(h + 1) * DV], op1=ALU.mult,
                                 r=["hs", "ss", ("so", st)], w=[("yb", sy)])
                        for c in range(8):
                            S.op("pe", "transpose", psb[:, c * 128:(c + 1) * 128], yb[sy][:, c * 128:(c + 1) * 128],
                                 identb[:], r=[("yb", sy), "identb"], w=["psb"])
                        S.op("act", "activation", out=yT[:, :, s * 128:(s + 1) * 128],
                             in_=psb[:].rearrange("p (c t) -> p c t", c=8), func=AF.Identity, r=["psb"], w=["yT"])
                    for mo in range(8):
                        pt = PS(mo % 4)
                        for c in range(8):
                            S.op("pe", "matmul", pt[:, 0:n], lhsT=wout[:, c, mo * 128:(mo + 1) * 128], rhs=yT[:, c, 0:n],
                                 start=(c == 0), stop=(c == 7), r=["wout", "yT"], w=[("ps", mo % 4)])
                        resid(outs[mo], pt[:, 0:n], MODL[:, 2, mo, which:which + 1], xs[mo],
                              r=[("ps", mo % 4), "mod", xregs[mo]], w=[oregs[mo]])

                if do_ctx:
                    tile([ctx[k][:, :] for k in range(8)], [("ctx", k) for k in range(8)], TC, 1, 0,
                         [ctx[k][:, :] for k in range(8)], [("ctx", k) for k in range(8)])
                for i in range(T // 512):
                    t0 = i * 512
                    S.dma("sp", xw[:], src[:, :, t0:t0 + 512].rearrange("k p t -> p k t"), w=["xw"])
                    tile([xw[:, k, :] for k in range(8)], ["xw"] * 8, 512, 0, TC + t0,
                         [xo[:, k, :] for k in range(8)], ["xo"] * 8)
                    S.dma("sp", dst[:, :, t0:t0 + 512].rearrange("k p t -> p k t"), xo[:], r=["xo"])
                S.flush()

        def phase_final(src):
            with ExitStack() as es:
                xw = sb(es, "xw", [128, 8, 512], F32)
                xo = sb(es, "xo", [128, 8, 512], F32)
                tmp = mk_tmp(es, 512)
                og, _ = PAR_OFF["gfin"]
                for i in range(T // 512):
                    t0 = i * 512
                    S.dma("sp", xw[:], src[:, :, t0:t0 + 512].rearrange("k p t -> p k t"), w=["xw"])
                    for k in range(8):
                        sq = tmp["sq%d" % (k % 2)]
                        S.op("act", "activation", out=sq[:], in_=xw[:, k, :], func=AF.Square, r=["xw"], w=[("sq", k % 2)])
                        S.op("pe", "matmul", PS(6)[:, :], lhsT=ones_f, rhs=sq[:], start=(k == 0), stop=(k == 7),
                             r=[("sq", k % 2), "cst"], w=[("ps", 6)])
                    std = tmp["std"]
                    S.op("act", "activation", out=std[:], in_=PS(6)[:, :], func=AF.Sqrt, scale=1.0 / D, bias=EPS,
                         r=[("ps", 6)], w=["std"])
                    S.op("dve", "reciprocal", out=std[:], in_=std[:], r=["std"], w=["std"])
                    for k in range(8):
                        S.op("dve", "scalar_tensor_tensor", out=xo[:, k, :], in0=xw[:, k, :],
                             scalar=par[:, og + k:og + k + 1], op0=ALU.mult, in1=std[:], op1=ALU.mult,
                             r=["xw", "par", "std"], w=["xo"])
                    S.dma("sp", outT[:, :, t0:t0 + 512].rearrange("k p t -> p k t"), xo[:], r=["xo"])
                S.flush()

        def halo_exchange(xsrc):
            def load(a):
                S.dma("sp", a.rearrange("p (k t) -> p k t", k=8),
                      xsrc[:, :, T - GW:T].rearrange("k p t -> p k t"), w=["hsrc"])
            exchange(hsrc, hdst, 8 * GW, load, halo[:].rearrange("p k t -> p (k t)"), "h")

        cur = xT_in
        last_mod = None
        for kind, l in plan:
            if last_mod != l:
                phase_mod(l)
                last_mod = l
            ctx_live = l < 2
            if kind == "A":
                phase_A1(l, cur, ctx_live)
                phase_scan(0, ctx_live)
                phase_scan(1, ctx_live)
                phase_A5(l, cur, XB, ctx_live)
                cur = XB
            elif kind == "B":
                phase_B(l, cur, XB, ctx_live)
                cur = XB
            elif kind == "F":
                need_weights(("f", l))
                halo_exchange(cur)
                phase_F(l, cur, XA, ctx_live)
                cur = XA
            elif kind == "M":
                pass
        if final:
            phase_final(cur)
        else:
            for k in range(8):
                S.dma("sp", outT[k], cur[k])
        for k in range(8):
            S.dma("sp", ctx_out[k], ctx[k][:], r=[("ctx", k)])
        S.flush()
        S.finish("sp")
        print("sched: ins=%d waits=%d cnt=%s" % (S.n_ins, S.n_wait, S.cnt))
    return nc


FULL_PLAN = [("A", 0), ("F", 0), ("B", 1), ("F", 1), ("A", 2), ("F", 2), ("B", 3), ("F", 3)]
_NC_CACHE = {}


def pack_par(b, odd, c, c_ctx, b_mod, g_mix, g_ffn, g_final, b_w_conv, f_w_conv, f_b_conv, a_b_gate):
    par = np.zeros((128, NPAR), np.float32)

    def put(name, arr):
        o, n = PAR_OFF[name]
        a = np.asarray(arr, np.float32).reshape(128, -1)
        assert a.shape[1] == n, (name, a.shape, n)
        par[:, o:o + n] = a
    bwc = np.asarray(b_w_conv, np.float32)
    fwc = np.asarray(f_w_conv, np.float32)
    put("cc", np.stack([fm(c[b]), fm(c_ctx)], axis=-1))
    put("bmod", fm(b_mod))
    put("gmix", fm(g_mix))
    put("gffn", fm(g_ffn))
    put("gfin", fm(g_final))
    put("bwc_l", fm(bwc))
    put("bwc_c", fm(bwc[:, ::-1] if odd else bwc))
    put("fwc_l", fm(fwc[:, ::-1] if odd else fwc))
    put("fwc_c", fm(fwc[:, ::-1] if odd else fwc))
    put("fbc", fm(f_b_conv))
    put("bgate", np.broadcast_to(gate_perm(np.asarray(a_b_gate, np.float32), odd).reshape(1, 32), (128, 32)))
    put("sel", np.broadcast_to(np.array([[1.0, 0.0]] if odd else [[0.0, 1.0]], np.float32), (128, 2)))
    return par


def gate_perm(g, odd):
    if not odd:
        return g
    sh = g.shape
    return np.ascontiguousarray(g.reshape(sh[:-1] + (2, 2, NH))[..., ::-1, :].reshape(sh))


def core_tokens(x_b, odd):
    r = np.asarray(x_b, np.float32).reshape(TFULL // GW, GW, D)
    r = r[NROW:][::-1] if odd else r[:NROW]
    return np.ascontiguousarray(r.reshape(T, D).T.reshape(8, 128, T))


def make_in_maps(inputs, cores, plan=None):
    maps = []
    gain = np.ascontiguousarray(np.broadcast_to(
        np.asarray(inputs["a_head_gain"], np.float32)[None], (128, 2, D)))
    wcache = {}
    for b, odd in cores:
        cx = np.asarray(inputs["ctx"][b], np.float32)
        if odd:
            cx = cx[::-1]
        m = {
            "xT": core_tokens(inputs["x"][b], odd),
            "ctxT": np.ascontiguousarray(cx.T.reshape(8, 128, TC)),
            "par": pack_par(b, odd, inputs["c"], inputs["c_ctx"], inputs["b_mod"], inputs["g_mix"], inputs["g_ffn"],
                            inputs["g_final"], inputs["b_w_conv"], inputs["f_w_conv"], inputs["f_b_conv"],
                            inputs["a_b_gate"]),
            "consts": make_consts(odd),
            "gain": gain,
        }
        for kind, l in (plan if plan is not None else FULL_PLAN):
            m["w_mod_%d" % l] = np.ascontiguousarray(inputs["w_mod"][l], np.float32)
            if kind == "A":
                j = l // 2
                key = ("awin", j, odd)
                if key not in wcache:
                    w = np.array(inputs["a_w_in"][j], np.float32)
                    w[:, 3072:] = gate_perm(w[:, 3072:], odd)
                    wcache[key] = w
                m["a_w_in_%d" % j] = wcache[key]
                m["a_w_out_%d" % j] = np.ascontiguousarray(inputs["a_w_out"][j], np.float32)
            elif kind == "B":
                m["b_w_in_%d" % (l // 2)] = np.ascontiguousarray(inputs["b_w_in"][l // 2], np.float32)
                m["b_w_out_%d" % (l // 2)] = np.ascontiguousarray(inputs["b_w_out"][l // 2], np.float32)
            elif kind == "F":
                m["f_w_up_%d" % l] = np.ascontiguousarray(inputs["f_w_up"][l], np.float32)
                m["f_w_down_%d" % l] = np.ascontiguousarray(inputs["f_w_down"][l], np.float32)
        maps.append(m)
    return maps


def assemble(results, cores, nb):
    out = np.empty((nb, TFULL, D), np.float32)
    for res, (b, odd) in zip(results, cores):
        o = res["outT"].reshape(D, T).T.reshape(NROW, GW, D)
        if odd:
            out[b, T:] = o[::-1].reshape(T, D)
        else:
            out[b, :T] = o.reshape(T, D)
    return out


def kernel(**inputs):
    key = "full"
    if key not in _NC_CACHE:
        _NC_CACHE[key] = build(FULL_PLAN, final=True, ncores=8)
    nc = _NC_CACHE[key]
    cores = [(b, odd) for b in range(4) for odd in (0, 1)]
    in_maps = make_in_maps(inputs, cores)
    res = run_bass_kernel_spmd(nc, in_maps, core_ids=list(range(8)))
    return assemble(res.results, cores, 4)
```

```python
import numpy as np
from contextlib import ExitStack
import ml_dtypes
import concourse.bass as bass
import concourse.mybir as mybir
from concourse.bass_utils import run_bass_kernel_spmd

F32 = mybir.dt.float32
BF16 = mybir.dt.bfloat16
AF = mybir.ActivationFunctionType
ALU = mybir.AluOpType

D = 1024
TFULL = 8192
T = 4096
TC = 256
TA = TC + T
DEPTH = 4
NH = 4
DK = 128
DV = 256
DVA = DV + 1
DFF = 2816
NJ = DFF // 128
GW = 64
CH = 64
NROW = T // GW
APROJ = 3088
EPS = 1e-6
NCH = TA // CH
SAME_ENG_SYNC = True


class Sched:
    ENGS = ("pe", "act", "dve", "pool", "sp")

    def __init__(self, nc, es, n_dma_sems=48, n_bg_sems=8):
        self.nc = nc
        self.eng = {"pe": nc.tensor, "act": nc.scalar, "dve": nc.vector,
                    "pool": nc.gpsimd, "sp": nc.sync}
        self.sems = []
        self.esem = {}
        for e in self.ENGS:
            self.esem[e] = len(self.sems)
            self.sems.append(es.enter_context(nc.semaphore("s_" + e)))
        self.dsem = []
        for i in range(n_dma_sems):
            self.dsem.append(len(self.sems))
            self.sems.append(es.enter_context(nc.semaphore("d%d" % i)))
        self.bsem = []
        for i in range(n_bg_sems):
            self.bsem.append(len(self.sems))
            self.sems.append(es.enter_context(nc.semaphore("b%d" % i)))
        self.brr = 0
        self.btarget = [0] * n_bg_sems
        self.bgev = {}
        self.ops = []
        self.cnt = {e: 0 for e in self.ENGS}
        self.waited = {e: {} for e in self.ENGS}
        self.pending = {e: {} for e in self.ENGS}
        self.rr = 0
        self.target = [0] * n_dma_sems
        self.n_ins = 0
        self.n_wait = 0

    def op(self, eng, meth, *args, r=(), w=(), **kw):
        self.ops.append(dict(eng=eng, meth=meth, args=args, kw=kw, r=tuple(r), w=tuple(w),
                             dma=False, signal=False, ev=None))

    def dma(self, eng, out, in_, r=(), w=(), bg=None):
        self.ops.append(dict(eng=eng, meth="dma_start", args=(), kw=dict(out=out, in_=in_),
                             r=tuple(r), w=tuple(w), dma=True, signal=True, ev=None, inc=16, bg=bg))

    def join_bg(self, group):
        for s, v in self.bgev.pop(group, []):
            for e in self.ENGS:
                if self.pending[e].get(s, 0) < v:
                    self.pending[e][s] = v

    def coll(self, ins, outs, groups, r=(), w=()):
        self.ops.append(dict(eng="pool", meth="collective_compute", args=("AllGather", ALU.bypass),
                             kw=dict(replica_groups=groups, ins=ins, outs=outs),
                             r=tuple(r), w=tuple(w), dma=True, signal=True, ev=None, inc=1))

    def flush(self):
        ops = self.ops
        self.ops = []
        state = {}
        deps = []
        last = {}
        for i, o in enumerate(ops):
            d = {}
            for r in o["r"]:
                st = state.get(r)
                if st is not None and st[0] is not None:
                    d[id(st[0])] = st[0]
            for r in o["w"]:
                st = state.get(r)
                if st is not None:
                    if st[0] is not None:
                        d[id(st[0])] = st[0]
                    for x in st[1].values():
                        d[id(x)] = x
                    for x in st[2]:
                        d[id(x)] = x
            for r in o["r"]:
                st = state.setdefault(r, [None, {}, []])
                if o["dma"]:
                    st[2].append(o)
                else:
                    st[1][o["eng"]] = o
            for r in o["w"]:
                st = state.setdefault(r, [None, {}, []])
                st[0] = o
                st[1] = {}
                st[2] = []
            d.pop(id(o), None)
            dd = []
            for oj in d.values():
                if (not oj["dma"]) and (not o["dma"]) and oj["eng"] == o["eng"] and \
                        (o["eng"] == "pe" or not SAME_ENG_SYNC):
                    continue
                dd.append(oj)
                oj["signal"] = True
            deps.append(dd)
            if not o["dma"]:
                last[o["eng"]] = o
        for o in last.values():
            o["signal"] = True
        nd = len(self.dsem)
        bar = {}
        for i, o in enumerate(ops):
            en = o["eng"]
            E = self.eng[en]
            need = dict(self.pending[en])
            self.pending[en] = {}
            for oj in deps[i]:
                s, v = oj["ev"]
                if need.get(s, 0) < v:
                    need[s] = v
            bgd = o["dma"] and o.get("bg") is not None
            if bgd:
                k = self.brr
                self.brr = (self.brr + 1) % len(self.bsem)
                s = self.bsem[k]
                if self.btarget[k] > 0 and need.get(s, 0) < self.btarget[k]:
                    need[s] = self.btarget[k]
            elif o["dma"]:
                k = self.rr
                self.rr = (self.rr + 1) % nd
                s = self.dsem[k]
                if self.target[k] > 0 and need.get(s, 0) < self.target[k]:
                    need[s] = self.target[k]
            for s, v in need.items():
                if self.waited[en].get(s, 0) < v:
                    E.wait_ge(self.sems[s], v)
                    self.waited[en][s] = v
                    self.n_wait += 1
            ins = getattr(E, o["meth"])(*o["args"], **o["kw"])
            self.n_ins += 1
            if bgd:
                self.btarget[k] += 16
                ins.then_inc(self.sems[self.bsem[k]], 16)
                o["ev"] = (self.bsem[k], self.btarget[k])
                self.bgev.setdefault(o["bg"], []).append(o["ev"])
            elif o["dma"]:
                self.target[k] += o["inc"]
                if o["inc"] == 16:
                    ins.then_inc(self.sems[self.dsem[k]], 16)
                else:
                    ins.then_inc(self.sems[self.dsem[k]])
                o["ev"] = (self.dsem[k], self.target[k])
                bar[self.dsem[k]] = self.target[k]
            elif o["signal"]:
                self.cnt[en] += 1
                ins.then_inc(self.sems[self.esem[en]], 1)
                o["ev"] = (self.esem[en], self.cnt[en])
                bar[self.esem[en]] = self.cnt[en]
        for e in self.ENGS:
            p = self.pending[e]
            for s, v in bar.items():
                if p.get(s, 0) < v:
                    p[s] = v

    def finish(self, eng="sp"):
        E = self.eng[eng]
        for s, v in self.pending[eng].items():
            if self.waited[eng].get(s, 0) < v:
                E.wait_ge(self.sems[s], v)
                self.waited[eng][s] = v


def _par_layout():
    off = {}
    o = 0
    for name, n in (("cc", 16), ("bmod", DEPTH * 48), ("gmix", DEPTH * 8), ("gffn", DEPTH * 8),
                    ("gfin", 8), ("bwc_l", 2 * 3 * 8), ("bwc_c", 2 * 3 * 8), ("fwc_l", DEPTH * 3 * NJ),
                    ("fwc_c", DEPTH * 3 * NJ), ("fbc", DEPTH * NJ), ("bgate", 2 * 16), ("sel", 2)):
        off[name] = (o, n)
        o += n
    return off, o


PAR_OFF, NPAR = _par_layout()
C_ONES, C_TRIF, C_TRIB, C_BLK0, C_BLK1, C_ID, C_MF, C_MB, C_TRL0, C_TRL1, C_ML0, C_ML1, NCONST = \
    0, 128, 256, 384, 512, 640, 768, 832, 896, 1024, 1152, 1216, 1280


def fm(v):
    v = np.asarray(v, np.float32)
    lead = v.shape[:-1]
    k = v.shape[-1] // 128
    a = v.reshape(lead + (k, 128))
    a = np.moveaxis(a, -1, 0)
    return np.ascontiguousarray(a)


def make_consts(odd):
    c = np.zeros((128, NCONST), np.float32)
    c[:, C_ONES:C_ONES + 128] = 1.0
    s = np.arange(128)[:, None]
    t = np.arange(128)[None, :]
    same = (s // 64) == (t // 64)
    c[:, C_TRIF:C_TRIF + 128] = (same & (s <= t))
    c[:, C_TRIB:C_TRIB + 128] = (same & (s >= t))
    c[:, C_BLK0:C_BLK0 + 128] = (s < 64)
    c[:, C_BLK1:C_BLK1 + 128] = (s >= 64)
    c[:, C_ID:C_ID + 128] = (s == t)
    s6 = np.arange(128)[:, None] % 64
    t6 = np.arange(64)[None, :]
    c[:, C_MF:C_MF + 64] = (s6 <= t6)
    c[:, C_MB:C_MB + 64] = (s6 >= t6)
    f0, f1 = (C_TRIB, C_TRIF) if odd else (C_TRIF, C_TRIB)
    c[:, C_TRL0:C_TRL0 + 128] = c[:, f0:f0 + 128]
    c[:, C_TRL1:C_TRL1 + 128] = c[:, f1:f1 + 128]
    m0, m1 = (C_MB, C_MF) if odd else (C_MF, C_MB)
    c[:, C_ML0:C_ML0 + 64] = c[:, m0:m0 + 64]
    c[:, C_ML1:C_ML1 + 64] = c[:, m1:m1 + 64]
    return c


def build(plan, final=True, ncores=8):
    nc = bass.Bass("TRN2", target_bir_lowering=False)

    def dram(name, shape, dtype, kind):
        return nc.dram_tensor(name, list(shape), dtype, kind=kind).ap()

    xT_in = dram("xT", [8, 128, T], F32, "ExternalInput")
    ctxT_in = dram("ctxT", [8, 128, TC], F32, "ExternalInput")
    par_in = dram("par", [128, NPAR], F32, "ExternalInput")
    const_in = dram("consts", [128, NCONST], F32, "ExternalInput")
    gain_in = dram("gain", [128, 2, D], F32, "ExternalInput")
    need_w = set()
    for kind, l in plan:
        need_w.add(("mod", l))
        if kind == "A":
            need_w.add(("a", l // 2))
        elif kind == "B":
            need_w.add(("b", l // 2))
        elif kind == "F":
            need_w.add(("f", l))
    Wi, Wb = {}, {}
    for kind, l in sorted(need_w):
        if kind == "mod":
            specs = [("w_mod", [D, 6 * D])]
        elif kind == "a":
            specs = [("a_w_in", [D, APROJ]), ("a_w_out", [D, D])]
        elif kind == "b":
            specs = [("b_w_in", [D, 3 * D]), ("b_w_out", [D, D])]
        else:
            specs = [("f_w_up", [D, 2 * DFF]), ("f_w_down", [DFF, D])]
        for nm, shp in specs:
            Wi[(nm, l)] = dram("%s_%d" % (nm, l), shp, F32, "ExternalInput")
            if nm == "f_w_up":
                Wb[(nm, l)] = dram("%s_%d_b" % (nm, l), shp, BF16, "Internal")
    outT = dram("outT", [8, 128, T], F32, "ExternalOutput")
    ctx_out = dram("ctx_out", [8, 128, TC], F32, "ExternalOutput")
    XA = dram("XA", [8, 128, T], F32, "Internal")
    XB = dram("XB", [8, 128, T], F32, "Internal")
    QK = dram("QK", [8, 128, TA], BF16, "Internal")
    KT = dram("KT", [TA, NH * DK], F32, "Internal")
    VA = dram("VA", [TA, NH * DV], BF16, "Internal")
    SO = dram("SO", [TA, D], F32, "Internal")
    SC = dram("SC", [TA, 16], F32, "Internal")
    HD = [dram("HF", [TA, D], F32, "Internal"), dram("HB", [TA, D], F32, "Internal")]
    NST = NH * DV + NH
    csrc = nc.dram_tensor("csrc", [128, NST], F32)
    cdst = nc.dram_tensor("cdst", [256, NST], F32)
    hsrc = nc.dram_tensor("hsrc", [128, 8 * GW], F32)
    hdst = nc.dram_tensor("hdst", [256, 8 * GW], F32)
    PAIRS = [[2 * i, 2 * i + 1] for i in range(ncores // 2)]

    ges = ExitStack()
    with ges:
        S = Sched(nc, ges)

        uid = [0]

        def sb(es, name, shape, dtype):
            uid[0] += 1
            return es.enter_context(nc.sbuf_tensor("%s_u%d" % (name, uid[0]), list(shape), dtype))

        par = sb(ges, "par", [128, NPAR], F32)
        cst = sb(ges, "cst", [128, NCONST], F32)
        identb = sb(ges, "identb", [128, 128], BF16)
        MODL = sb(ges, "modl", [128, 6, 8, 2], F32)
        ctx = [sb(ges, "ctx%d" % k, [128, TC], F32) for k in range(8)]
        EG = sb(ges, "eg", [128, NCH, 8], F32)
        halo = sb(ges, "halo", [128, 8, GW], F32)
        crecv = sb(ges, "crecv", [128, NST], F32)
        osel, _ = PAR_OFF["sel"]
        sel0 = par[:, osel:osel + 1]
        sel1 = par[:, osel + 1:osel + 2]

        def exchange(src_d, dst_d, n, load_src, out_ap, tag):
            with ExitStack() as es:
                two = sb(es, "xch2", [128, 2, n], F32)
                tmpx = sb(es, "xcht", [128, n], F32)
                load_src(src_d.ap())
                S.coll([src_d.ap().opt()], [dst_d.ap().opt()], PAIRS, r=[tag + "src"], w=[tag + "dst"])
                S.dma("sp", two[:], dst_d.ap().rearrange("(r p) n -> p r n", p=128), r=[tag + "dst"], w=["xch2"])
                S.op("dve", "tensor_scalar", out=tmpx[:], in0=two[:, 0, :], scalar1=sel0, scalar2=None,
                     op0=ALU.mult, r=["xch2", "par"], w=["xcht"])
                S.op("dve", "scalar_tensor_tensor", out=out_ap, in0=two[:, 1, :], scalar=sel1, op0=ALU.mult,
                     in1=tmpx[:], op1=ALU.add, r=["xch2", "xcht", "par"], w=[tag + "recv"])
                S.flush()
        psf = ges.enter_context(nc.psum_tensor("psf", [128, 7, 512], F32))
        psb = ges.enter_context(nc.psum_tensor("psb", [128, 1024], BF16))

        def PS(b):
            return psf[:, b, :]

        def pcol(name, idx=0):
            o, n = PAR_OFF[name]
            return o + idx

        onesb128 = sb(ges, "onesb128", [128, 128], BF16)
        ones_f = onesb128[:]

        S.dma("sp", par[:], par_in[:, :], w=["par"])
        S.dma("sp", cst[:], const_in[:, :], w=["cst"])
        for k in range(8):
            S.dma("sp", ctx[k][:], ctxT_in[k], w=[("ctx", k)])
        S.op("dve", "tensor_copy", out=identb[:], in_=cst[:, C_ID:C_ID + 128], r=["cst"], w=["identb"])
        S.op("dve", "tensor_copy", out=onesb128[:], in_=cst[:, C_ONES:C_ONES + 128], r=["cst"], w=["onesb128"])
        def cast_rows(dst, src, nrows, bg):
            for r0 in range(0, nrows, 128):
                S.dma("pool", dst[r0:r0 + 128, :], src[r0:r0 + 128, :], bg=bg)

        fgroups = []
        for kind, l in plan:
            if kind == "F" and ("f", l) not in fgroups:
                fgroups.append(("f", l))
        joined = set()

        def need_weights(g):
            if g[0] == "f" and g not in joined:
                issue_bg_casts()
                joined.add(g)
                S.join_bg(g)

        S.flush()
        bg_done = [False]

        def issue_bg_casts():
            if bg_done[0]:
                return
            bg_done[0] = True
            for g in fgroups:
                key = ("f_w_up", g[1])
                cast_rows(Wb[key], Wi[key], Wi[key].shape[0], g)

        def mk_tmp(es, n):
            t = {nm: sb(es, nm, [128, n], F32) for nm in ("std", "nt0", "nt1")}
            t["sq0"] = sb(es, "sq0", [128, n], BF16)
            t["sq1"] = sb(es, "sq1", [128, n], BF16)
            return t

        def norm_mod(xs, xregs, n, which, s_gs, s_sh, hx, tmp):
            pieces = [(c0, min(512, n - c0)) for c0 in range(0, n, 512)]
            for k in range(8):
                sq = tmp["sq%d" % (k % 2)]
                S.op("act", "activation", out=sq[:, 0:n], in_=xs[k], func=AF.Square,
                     r=[xregs[k]], w=[("sq", k % 2)])
                for pi, (c0, cn) in enumerate(pieces):
                    S.op("pe", "matmul", PS(6 - pi)[:, 0:cn], lhsT=ones_f, rhs=sq[:, c0:c0 + cn],
                         start=(k == 0), stop=(k == 7), r=[("sq", k % 2), "cst"], w=[("ps", 6 - pi)])
            std = tmp["std"]
            for pi, (c0, cn) in enumerate(pieces):
                S.op("act", "activation", out=std[:, c0:c0 + cn], in_=PS(6 - pi)[:, 0:cn], func=AF.Sqrt,
                     scale=1.0 / D, bias=EPS, r=[("ps", 6 - pi)], w=["std"])
            S.op("dve", "reciprocal", out=std[:, 0:n], in_=std[:, 0:n], r=["std"], w=["std"])
            for k in range(8):
                tt = tmp["nt%d" % (k % 2)]
                S.op("dve", "tensor_tensor", out=tt[:, 0:n], in0=xs[k], in1=std[:, 0:n], op=ALU.mult,
                     r=[xregs[k], "std"], w=[("nt", k % 2)])
                S.op("act", "activation", out=hx[k][:, 0:n], in_=tt[:, 0:n], func=AF.Identity,
                     scale=MODL[:, s_gs, k, which:which + 1], bias=MODL[:, s_sh, k, which:which + 1],
                     r=[("nt", k % 2), "mod"], w=[("hx", k)])

        def resid(out_ap, ps_ap, gcol, x_ap, r, w):
            S.op("dve", "scalar_tensor_tensor", out=out_ap, in0=ps_ap, scalar=gcol, op0=ALU.mult,
                 in1=x_ap, op1=ALU.add, r=r, w=w)

        def v3(ap, rowlen):
            return ap.rearrange("p (a b) -> p a b", b=rowlen)

        def phase_mod(l):
            with ExitStack() as es:
                scb = sb(es, "scb", [128, 8, 2], BF16)
                wm = [sb(es, "wm%d" % i, [128, 8, D], BF16) for i in range(6)]
                t1 = sb(es, "modt1", [128, 8, 2], F32)
                o, _ = PAR_OFF["cc"]
                S.op("act", "activation", out=scb[:].rearrange("p k w -> p (k w)"), in_=par[:, o:o + 16],
                     func=AF.Silu, r=["par"], w=["scb"])
                for s in range(6):
                    S.dma("pool", wm[s][:], Wi[("w_mod", l)][:, s * D:(s + 1) * D].rearrange("(k p) n -> p k n", p=128),
                          w=[("wm", s)])
                for s in range(6):
                    wt = wm[s]
                    pst = PS(s % 2)
                    for m in range(8):
                        for k in range(8):
                            S.op("pe", "matmul", pst[:, m * 2:m * 2 + 2], lhsT=wt[:, k, m * 128:(m + 1) * 128],
                                 rhs=scb[:, k, :], start=(k == 0), stop=(k == 7),
                                 r=[("wm", s), "scb"], w=[("ps", s % 2)])
                    ob, _ = PAR_OFF["bmod"]
                    bcol = par[:, ob + l * 48 + s * 8: ob + l * 48 + s * 8 + 8]
                    S.op("dve", "tensor_tensor", out=MODL[:, s, :, :],
                         in0=pst[:, 0:16].rearrange("p (m w) -> p m w", w=2),
                         in1=bcol.unsqueeze(2).to_broadcast([128, 8, 2]), op=ALU.add,
                         r=[("ps", s % 2), "par"], w=["mod"])
                for s, gname in ((1, "gmix"), (4, "gffn")):
                    og, _ = PAR_OFF[gname]
                    gcol = par[:, og + l * 8: og + l * 8 + 8]
                    S.op("dve", "tensor_scalar", out=t1[:], in0=MODL[:, s, :, :], scalar1=1.0, scalar2=None,
                         op0=ALU.add, r=["mod"], w=["modt1"])
                    S.op("dve", "tensor_tensor", out=MODL[:, s, :, :], in0=t1[:],
                         in1=gcol.unsqueeze(2).to_broadcast([128, 8, 2]), op=ALU.mult,
                         r=["modt1", "par"], w=["mod"])
                S.flush()

        def phase_B(l, src, dst, do_ctx):
            j = l // 2
            with ExitStack() as es:
                xw = sb(es, "xw", [128, 8, 512], F32)
                xo = sb(es, "xo", [128, 8, 512], F32)
                hx = [sb(es, "hx%d" % k, [128, 512], BF16) for k in range(8)]
                tmp = mk_tmp(es, 512)
                win = sb(es, "win", [128, 8, 3 * D], BF16)
                wout = sb(es, "wout", [128, 8, D], BF16)
                xv_sb = [sb(es, "xv%d" % i, [128, 512], F32) for i in range(2)]
                m_sb = [sb(es, "m%d" % i, [128, 512], F32) for i in range(2)]
                acc = [sb(es, "acc%d" % i, [128, 512], F32) for i in range(2)]
                z = [sb(es, "z%d" % k, [128, 512], BF16) for k in range(8)]
                for pc in range(3):
                    S.dma("pool", win[:, :, pc * D:(pc + 1) * D],
                          Wi[("b_w_in", j)][:, pc * D:(pc + 1) * D].rearrange("(k p) n -> p k n", p=128), w=[("win", pc)])
                S.dma("pool", wout[:], Wi[("b_w_out", j)].rearrange("(k p) n -> p k n", p=128), w=["wout"])
                def tile(xs, xregs, n, rowlen, which, outs, oregs):
                    ow, _ = PAR_OFF["bwc_c" if which == 1 else "bwc_l"]
                    norm_mod(xs, xregs, n, which, 1, 0, hx, tmp)
                    for c in range(8):
                        pr = c % 2
                        pb, pc_, pv = PS(3 * pr), PS(3 * pr + 1), PS(3 * pr + 2)
                        for gi, pt in enumerate((pb, pc_, pv)):
                            for k in range(8):
                                S.op("pe", "matmul", pt[:, 0:n],
                                     lhsT=win[:, k, gi * D + c * 128: gi * D + (c + 1) * 128], rhs=hx[k][:, 0:n],
                                     start=(k == 0), stop=(k == 7),
                                     r=[("win", gi), ("hx", k)], w=[("ps", 3 * pr + gi)])
                        S.op("act", "activation", out=xv_sb[pr][:, 0:n], in_=pv[:, 0:n], func=AF.Identity,
                             r=[("ps", 3 * pr + 2)], w=[("xv", pr)])
                        S.op("dve", "tensor_tensor", out=m_sb[pr][:, 0:n], in0=pc_[:, 0:n], in1=xv_sb[pr][:, 0:n],
                             op=ALU.mult, r=[("ps", 3 * pr + 1), ("xv", pr)], w=[("m", pr)])
                        w0 = par[:, ow + (j * 3 + 0) * 8 + c: ow + (j * 3 + 0) * 8 + c + 1]
                        w1 = par[:, ow + (j * 3 + 1) * 8 + c: ow + (j * 3 + 1) * 8 + c + 1]
                        w2 = par[:, ow + (j * 3 + 2) * 8 + c: ow + (j * 3 + 2) * 8 + c + 1]
                        S.op("act", "activation", out=acc[pr][:, 0:n], in_=m_sb[pr][:, 0:n], func=AF.Identity,
                             scale=w1, r=[("m", pr), "par"], w=[("acc", pr)])
                        a3 = v3(acc[pr][:, 0:n], rowlen)
                        m3 = v3(m_sb[pr][:, 0:n], rowlen)
                        S.op("dve", "scalar_tensor_tensor", out=a3[:, :, 1:rowlen], in0=m3[:, :, 0:rowlen - 1],
                             scalar=w0, op0=ALU.mult, in1=a3[:, :, 1:rowlen], op1=ALU.add,
                             r=[("m", pr), ("acc", pr), "par"], w=[("acc", pr)])
                        S.op("dve", "scalar_tensor_tensor", out=a3[:, :, 0:rowlen - 1], in0=m3[:, :, 1:rowlen],
                             scalar=w2, op0=ALU.mult, in1=a3[:, :, 0:rowlen - 1], op1=ALU.add,
                             r=[("m", pr), ("acc", pr), "par"], w=[("acc", pr)])
                        S.op("dve", "tensor_tensor", out=z[c][:, 0:n], in0=pb[:, 0:n], in1=acc[pr][:, 0:n],
                             op=ALU.mult, r=[("ps", 3 * pr), ("acc", pr)], w=[("z", c)])
                    for mo in range(8):
                        pt = PS(mo % 6)
                        for c in range(8):
                            S.op("pe", "matmul", pt[:, 0:n], lhsT=wout[:, c, mo * 128:(mo + 1) * 128],
                                 rhs=z[c][:, 0:n], start=(c == 0), stop=(c == 7),
                                 r=["wout", ("z", c)], w=[("ps", mo % 6)])
                        resid(outs[mo], pt[:, 0:n], MODL[:, 2, mo, which:which + 1], xs[mo],
                              r=[("ps", mo % 6), "mod", xregs[mo]], w=[oregs[mo]])

                if do_ctx:
                    tile([ctx[k][:, :] for k in range(8)], [("ctx", k) for k in range(8)], TC, TC, 1,
                         [ctx[k][:, :] for k in range(8)], [("ctx", k) for k in range(8)])
                xw2 = sb(es, "xw2", [128, 8, 512], F32)
                xws = [xw, xw2]

                def load(i):
                    S.dma("sp", xws[i % 2][:], src[:, :, i * 512:(i + 1) * 512].rearrange("k p t -> p k t"),
                          w=[("xw", i % 2)])
                load(0)
                for i in range(T // 512):
                    t0 = i * 512
                    if i + 1 < T // 512:
                        load(i + 1)
                    xb = xws[i % 2]
                    tile([xb[:, k, :] for k in range(8)], [("xw", i % 2)] * 8, 512, GW, 0,
                         [xo[:, k, :] for k in range(8)], ["xo"] * 8)
                    S.dma("sp", dst[:, :, t0:t0 + 512].rearrange("k p t -> p k t"), xo[:], r=["xo"])
                S.flush()

        def phase_F(l, src, dst, do_ctx, fin=False):
            with ExitStack() as es:
                NW = 640
                xw = sb(es, "xw", [128, 8, NW], F32)
                xo = sb(es, "xo", [128, 8, 512], F32)
                hx = [sb(es, "hx%d" % k, [128, NW], BF16) for k in range(8)]
                tmp = mk_tmp(es, NW)
                wg = [sb(es, "wg%d" % i, [128, 8, 256], BF16) for i in range(2)]
                wv = [sb(es, "wv%d" % i, [128, 8, 256], BF16) for i in range(2)]
                wdn = sb(es, "wdn", [128, NJ, D], BF16)
                acc = [sb(es, "acc%d" % i, [128, 512], F32) for i in range(2)]
                sil = [sb(es, "sil%d" % i, [128, 512], F32) for i in range(2)]
                act = [sb(es, "act%d" % jj, [128, 512], BF16) for jj in range(NJ)]
                for jb in range(0, NJ, 11):
                    S.dma("pool", wdn[:, jb:jb + 11, :],
                          Wi[("f_w_down", l)][jb * 128:(jb + 11) * 128, :].rearrange("(j p) n -> p j n", p=128),
                          w=[("wdn", jb)])
                obc, _ = PAR_OFF["fbc"]

                def tile(xs, xregs, nw, co, n, shift, which, outs, oregs):
                    owc, _ = PAR_OFF["fwc_c" if which == 1 else "fwc_l"]
                    lo_ok = co >= shift
                    hi_ok = nw >= co + n + shift
                    norm_mod(xs, xregs, nw, which, 4, 3, hx, tmp)
                    gp = [(c0, min(512, nw - c0)) for c0 in range(0, nw, 512)]
                    for jj in range(NJ):
                        pr = jj % 2
                        if jj % 2 == 0:
                            wb = (jj // 2) % 2
                            S.dma("sp", wg[wb][:], Wb[("f_w_up", l)][:, jj * 128: jj * 128 + 256].rearrange(
                                "(k p) n -> p k n", p=128), w=[("wg", wb)])
                            S.dma("sp", wv[wb][:], Wb[("f_w_up", l)][:, DFF + jj * 128: DFF + jj * 128 + 256].rearrange(
                                "(k p) n -> p k n", p=128), w=[("wv", wb)])
                        wb = (jj // 2) % 2
                        wo_ = (jj % 2) * 128
                        gflat = psf[:, 2 * pr:2 * pr + 2, :].rearrange("p b c -> p (b c)")
                        pv = PS(4 + pr)
                        for pi, (c0, cn) in enumerate(gp):
                            for k in range(8):
                                S.op("pe", "matmul", PS(2 * pr + pi)[:, 0:cn], lhsT=wg[wb][:, k, wo_:wo_ + 128],
                                     rhs=hx[k][:, c0:c0 + cn], start=(k == 0), stop=(k == 7),
                                     r=[("wg", wb), ("hx", k)], w=[("ps", 2 * pr + pi)])
                        for k in range(8):
                            S.op("pe", "matmul", pv[:, 0:n], lhsT=wv[wb][:, k, wo_:wo_ + 128],
                                 rhs=hx[k][:, co:co + n], start=(k == 0), stop=(k == 7),
                                 r=[("wv", wb), ("hx", k)], w=[("ps", 4 + pr)])
                        greg = [("ps", 2 * pr), ("ps", 2 * pr + 1)]
                        w0 = par[:, owc + (l * 3 + 0) * NJ + jj: owc + (l * 3 + 0) * NJ + jj + 1]
                        w1 = par[:, owc + (l * 3 + 1) * NJ + jj: owc + (l * 3 + 1) * NJ + jj + 1]
                        w2 = par[:, owc + (l * 3 + 2) * NJ + jj: owc + (l * 3 + 2) * NJ + jj + 1]
                        bc = par[:, obc + l * NJ + jj: obc + l * NJ + jj + 1]
                        a = acc[pr]
                        S.op("dve", "tensor_scalar", out=a[:, 0:n], in0=gflat[:, co:co + n], scalar1=w1, scalar2=None,
                             op0=ALU.mult, r=greg + ["par"], w=[("acc", pr)])
                        if lo_ok:
                            o0, o1, s0 = 0, n, co - shift
                        else:
                            o0, o1, s0 = shift, n, co
                        S.op("dve", "scalar_tensor_tensor", out=a[:, o0:o1], in0=gflat[:, s0:s0 + (o1 - o0)],
                             scalar=w0, op0=ALU.mult, in1=a[:, o0:o1], op1=ALU.add,
                             r=greg + ["par", ("acc", pr)], w=[("acc", pr)])
                        if hi_ok:
                            o0, o1 = 0, n
                        else:
                            o0, o1 = 0, n - shift
                        S.op("dve", "scalar_tensor_tensor", out=a[:, o0:o1],
                             in0=gflat[:, co + shift:co + shift + (o1 - o0)],
                             scalar=w2, op0=ALU.mult, in1=a[:, o0:o1], op1=ALU.add,
                             r=greg + ["par", ("acc", pr)], w=[("acc", pr)])
                        S.op("act", "activation", out=sil[pr][:, 0:n], in_=a[:, 0:n], func=AF.Silu, bias=bc,
                             r=[("acc", pr), "par"], w=[("sil", pr)])
                        S.op("dve", "tensor_tensor", out=act[jj][:, 0:n], in0=pv[:, 0:n], in1=sil[pr][:, 0:n],
                             op=ALU.mult, r=[("ps", 4 + pr), ("sil", pr)], w=[("act", jj)])
                    for mo in range(8):
                        pt = PS(mo % 4)
                        for jj in range(NJ):
                            S.op("pe", "matmul", pt[:, 0:n], lhsT=wdn[:, jj, mo * 128:(mo + 1) * 128],
                                 rhs=act[jj][:, 0:n], start=(jj == 0), stop=(jj == NJ - 1),
                                 r=[("wdn", 0), ("wdn", 11), ("act", jj)], w=[("ps", mo % 4)])
                        resid(outs[mo], pt[:, 0:n], MODL[:, 5, mo, which:which + 1], xs[mo][:, co:co + n],
                              r=[("ps", mo % 4), "mod", xregs[mo]], w=[oregs[mo]])

                if do_ctx:
                    tile([ctx[k][:, :] for k in range(8)], [("ctx", k) for k in range(8)], TC, 0, TC, 1, 1,
                         [ctx[k][:, :] for k in range(8)], [("ctx", k) for k in range(8)])
                xw2 = sb(es, "xw2", [128, 8, NW], F32)
                xws = [xw, xw2]

                def load(i):
                    r0 = i * 8
                    wlo, whi = max(0, r0 - 1), min(NROW, r0 + 9)
                    nw = (whi - wlo) * GW
                    co = (r0 - wlo) * GW
                    xb, reg = xws[i % 2], ("xw", i % 2)
                    S.dma("sp", xb[:, :, 0:nw], src[:, :, wlo * GW:whi * GW].rearrange("k p t -> p k t"), w=[reg])
                    if r0 + 9 > NROW:
                        S.op("act", "activation", out=xb[:, :, nw:nw + GW], in_=halo[:], func=AF.Identity,
                             r=["hrecv"], w=[reg])
                        nw += GW
                    return xb, reg, nw, co
                nt = NROW // 8
                nxt = load(0)
                for i in range(nt):
                    r0 = i * 8
                    xb, reg, nw, co = nxt
                    if i + 1 < nt:
                        nxt = load(i + 1)
                    tile([xb[:, k, 0:nw] for k in range(8)], [reg] * 8, nw, co, 512, GW, 0,
                         [xo[:, k, :] for k in range(8)], ["xo"] * 8)
                    if fin:
                        ogf, _ = PAR_OFF["gfin"]
                        for k in range(8):
                            sq = tmp["sq%d" % (k % 2)]
                            S.op("act", "activation", out=sq[:, 0:512], in_=xo[:, k, :], func=AF.Square,
                                 r=["xo"], w=[("sq", k % 2)])
                            S.op("pe", "matmul", PS(6)[:, :], lhsT=ones_f, rhs=sq[:, 0:512], start=(k == 0),
                                 stop=(k == 7), r=[("sq", k % 2)], w=[("ps", 6)])
                        std = tmp["std"]
                        S.op("act", "activation", out=std[:, 0:512], in_=PS(6)[:, :], func=AF.Sqrt, scale=1.0 / D,
                             bias=EPS, r=[("ps", 6)], w=["std"])
                        S.op("dve", "reciprocal", out=std[:, 0:512], in_=std[:, 0:512], r=["std"], w=["std"])
                        for k in range(8):
                            S.op("dve", "scalar_tensor_tensor", out=xo[:, k, :], in0=xo[:, k, :],
                                 scalar=par[:, ogf + k:ogf + k + 1], op0=ALU.mult, in1=std[:, 0:512], op1=ALU.mult,
                                 r=["xo", "par", "std"], w=["xo"])
                    S.dma("sp", dst[:, :, r0 * GW:r0 * GW + 512].rearrange("k p t -> p k t"), xo[:], r=["xo"])
                S.flush()

        def phase_A1(l, src, emit_ctx):
            j = l // 2
            with ExitStack() as es:
                xw = sb(es, "xw", [128, 8, 512], F32)
                hx = [sb(es, "hx%d" % k, [128, 512], BF16) for k in range(8)]
                tmp = mk_tmp(es, 512)
                win = sb(es, "win", [128, 8, APROJ], BF16)
                qkb = sb(es, "qkb", [128, 8, 512], BF16)
                kt_sb = [sb(es, "kt%d" % i, [128, NH * DK], F32) for i in range(2)]
                va_sb = [sb(es, "va%d" % i, [128, NH * DV], BF16) for i in range(2)]
                so_sb = [sb(es, "so%d" % i, [128, D], F32) for i in range(2)]
                g_sb = [sb(es, "g%d" % i, [128, 16], F32) for i in range(2)]
                sp_sb = [sb(es, "sp%d" % i, [128, 8], F32) for i in range(2)]
                u_sb = [sb(es, "u%d" % i, [128, 8], F32) for i in range(2)]
                sc_sb = [sb(es, "sc%d" % i, [128, 16], F32) for i in range(2)]
                for pc, (c0, c1) in enumerate(((0, 1024), (1024, 2048), (2048, APROJ))):
                    S.dma("pool", win[:, :, c0:c1], Wi[("a_w_in", j)][:, c0:c1].rearrange("(k p) n -> p k n", p=128),
                          w=[("win", pc)])
                wreg = [("win", 0), ("win", 1), ("win", 2)]
                obg, _ = PAR_OFF["bgate"]
                bg = par[:, obg + j * 16: obg + j * 16 + 16]
                stc = [0]
                pendB = []

                def tile(xs, xregs, n, which, ta0, do_o):
                    tr0, tr1 = (C_TRIF, C_TRIB) if which == 1 else (C_TRL0, C_TRL1)
                    norm_mod(xs, xregs, n, which, 1, 0, hx, tmp)
                    for m in range(8):
                        pt = PS(m % 2)
                        for k in range(8):
                            S.op("pe", "matmul", pt[:, 0:n], lhsT=win[:, k, m * 128:(m + 1) * 128], rhs=hx[k][:, 0:n],
                                 start=(k == 0), stop=(k == 7), r=wreg + [("hx", k)], w=[("ps", m % 2)])
                        S.op("act", "activation", out=qkb[:, m, 0:n], in_=pt[:, 0:n], func=AF.Identity,
                             scale=(1.0 if m < 4 else DK ** -0.5), r=[("ps", m % 2)], w=["qkb"])
                    S.dma("sp", QK[:, :, ta0:ta0 + n].rearrange("k p t -> p k t"), qkb[:, :, 0:n], r=["qkb"])
                    for s in range(n // 128):
                        st = stc[0] % 2
                        stc[0] += 1
                        tsl = slice(s * 128, (s + 1) * 128)
                        row0 = ta0 + s * 128

                        def proj(bank, c0, cn):
                            for k in range(8):
                                S.op("pe", "matmul", PS(bank)[:, 0:cn], lhsT=hx[k][:, tsl], rhs=win[:, k, c0:c0 + cn],
                                     start=(k == 0), stop=(k == 7), r=wreg + [("hx", k)], w=[("ps", bank)])
                        proj(2, 512, 512)
                        S.op("act", "activation", out=kt_sb[st][:], in_=PS(2)[:, :], func=AF.Identity,
                             scale=DK ** -0.5, r=[("ps", 2)], w=[("kt", st)])
                        S.dma("sp", KT[row0:row0 + 128, :], kt_sb[st][:], r=[("kt", st)])
                        for pi in range(2):
                            proj(3 + pi, 1024 + pi * 512, 512)
                            S.op("act", "activation", out=va_sb[st][:, pi * 512:(pi + 1) * 512], in_=PS(3 + pi)[:, :],
                                 func=AF.Identity, r=[("ps", 3 + pi)], w=[("va", st)])
                        S.dma("sp", VA[row0:row0 + 128, :], va_sb[st][:], r=[("va", st)])
                        if do_o:
                            for pi in range(2):
                                proj(2 + 2 * pi, 2048 + pi * 512, 512)
                                S.op("act", "activation", out=so_sb[st][:, pi * 512:(pi + 1) * 512],
                                     in_=PS(2 + 2 * pi)[:, :], func=AF.Sigmoid, r=[("ps", 2 + 2 * pi)],
                                     w=[("so", st)])
                            S.dma("sp", SO[row0:row0 + 128, :], so_sb[st][:], r=[("so", st)])
                        proj(5, 3072, 16)
                        while pendB:
                            pendB.pop(0)()
                        S.op("dve", "tensor_tensor", out=g_sb[st][:], in0=PS(5)[:, 0:16], in1=bg, op=ALU.add,
                             r=[("ps", 5), "par"], w=[("g", st)])
                        S.op("act", "activation", out=sp_sb[st][:], in_=g_sb[st][:, 8:16], func=AF.Exp, scale=-1.0,
                             r=[("g", st)], w=[("sp", st)])
                        S.op("act", "activation", out=sp_sb[st][:], in_=sp_sb[st][:], func=AF.Ln, bias=1.0, scale=1.0,
                             r=[("sp", st)], w=[("sp", st)])
                        def partB(st=st, row0=row0, tr0=tr0, tr1=tr1):
                            S.op("pe", "matmul", PS(6)[:, 32:36], lhsT=cst[:, tr0:tr0 + 128], rhs=sp_sb[st][:, 0:4],
                                 start=True, stop=True, r=[("sp", st), "cst"], w=[("ps", 6)])
                            S.op("pe", "matmul", PS(6)[:, 36:40], lhsT=cst[:, tr1:tr1 + 128], rhs=sp_sb[st][:, 4:8],
                                 start=True, stop=True, r=[("sp", st), "cst"], w=[("ps", 6)])
                            S.op("pe", "matmul", PS(6)[:, 64:72], lhsT=cst[:, C_BLK0:C_BLK0 + 128], rhs=sp_sb[st][:, 0:8],
                                 start=True, stop=True, r=[("sp", st), "cst"], w=[("ps", 6)])
                            S.op("pe", "matmul", PS(6)[:, 72:80], lhsT=cst[:, C_BLK1:C_BLK1 + 128], rhs=sp_sb[st][:, 0:8],
                                 start=True, stop=True, r=[("sp", st), "cst"], w=[("ps", 6)])
                            S.op("dve", "tensor_tensor", out=u_sb[st][:], in0=PS(6)[:, 32:40], in1=g_sb[st][:, 0:8],
                                 op=ALU.add, r=[("ps", 6), ("g", st)], w=[("u", st)])
                            S.op("act", "activation", out=sc_sb[st][:, 0:8], in_=u_sb[st][:], func=AF.Exp,
                                 r=[("u", st)], w=[("sc", st)])
                            S.op("act", "activation", out=sc_sb[st][:, 8:16], in_=PS(6)[:, 32:40], func=AF.Exp,
                                 r=[("ps", 6)], w=[("sc", st)])
                            ch0 = row0 // CH
                            S.op("act", "activation", out=EG[:, ch0:ch0 + 2, :].rearrange("p c g -> p (c g)"),
                                 in_=PS(6)[:, 64:80], func=AF.Exp, scale=-1.0, r=[("ps", 6)], w=["eg"])
                            S.dma("sp", SC[row0:row0 + 128, :], sc_sb[st][:], r=[("sc", st)])
                        pendB.append(partB)
                    while pendB:
                        pendB.pop(0)()

                tile([ctx[k][:, :] for k in range(8)], [("ctx", k) for k in range(8)], TC, 1, 0, emit_ctx)
                xw2 = sb(es, "xw2", [128, 8, 512], F32)
                xws = [xw, xw2]

                def load(i):
                    S.dma("sp", xws[i % 2][:], src[:, :, i * 512:(i + 1) * 512].rearrange("k p t -> p k t"),
                          w=[("xw", i % 2)])
                load(0)
                for i in range(T // 512):
                    t0 = i * 512
                    if i + 1 < T // 512:
                        load(i + 1)
                    xb = xws[i % 2]
                    tile([xb[:, k, :] for k in range(8)], [("xw", i % 2)] * 8, 512, 0, TC + t0, True)
                S.flush()

        def phase_scan(d, emit_ctx):
            with ExitStack() as es:
                qk_t = [sb(es, "qkt%d" % i, [128, 8, 512], BF16) for i in range(2)]
                kt_t = [sb(es, "ktt%d" % i, [64, 8, NH * DK], F32) for i in range(2)]
                va_t = [sb(es, "vat%d" % i, [64, 8, NH * DV], BF16) for i in range(2)]
                sc_t = [sb(es, "sct%d" % i, [64, 8, 16], F32) for i in range(2)]
                hout = [sb(es, "hout%d" % i, [64, 8, D], F32) for i in range(2)]
                Ct = sb(es, "Ct", [128, NH, DV], F32)
                nst = sb(es, "nst", [128, NH], F32)
                Cb = [sb(es, "Cb%d" % i, [128, NH, DV], BF16) for i in range(2)]
                nb_ = [sb(es, "nb%d" % i, [128, NH], BF16) for i in range(2)]
                ntmp = sb(es, "ntmp", [128, NH], F32)
                pT = [sb(es, "pT%d" % i, [64, NH, 64], BF16) for i in range(2)]
                ks = [sb(es, "ks%d" % i, [64, NH * DK], BF16) for i in range(2)]
                absb = sb(es, "absb", [64, NH], F32)
                den = sb(es, "den", [64, NH], F32)
                onesb = sb(es, "onesb", [128, 2], BF16)
                S.op("dve", "memset", Ct[:], 0.0, w=[("Ct", h) for h in range(NH)])
                S.op("dve", "memset", nst[:], 0.0, w=["nst"])
                S.op("dve", "memset", Cb[0][:], 0.0, w=[("Cb", 0, h) for h in range(NH)])
                S.op("dve", "memset", nb_[0][:], 0.0, w=[("nb", 0)])
                S.op("dve", "memset", onesb[:], 1.0, w=["onesb"])
                issue_bg_casts()
                mc_ctx = C_MF if d == 0 else C_MB
                mc_lat = C_ML0 if d == 0 else C_ML1
                blocks = [(0, 4, True)] + [(TC + i * 512, 8, False) for i in range(T // 512)]
                if d == 1:
                    blocks = ([blocks[0]] if emit_ctx else []) + blocks[1:][::-1]
                chunks = []
                for bi, (ta0, nb, is_ctx) in enumerate(blocks):
                    order = list(range(nb)) if d == 0 else list(range(nb))[::-1]
                    for ci in order:
                        chunks.append(dict(bi=bi, bf=bi % 2, ta0=ta0, nb=nb, is_ctx=is_ctx, ci=ci,
                                           chg=ta0 // CH + ci, emit=((not is_ctx) or emit_ctx),
                                           first=(ci == order[0]), last=(ci == order[-1])))
                ones4 = cst[:, C_ONES:C_ONES + NH]
                SB = (0, 6)
                st = dict(prev=None, injected=(d == 0))

                def load_block(bi):
                    if bi >= len(blocks):
                        return
                    ta0, nb, _ = blocks[bi]
                    bf, ntok = bi % 2, nb * 64
                    if True:
                        S.dma("sp", qk_t[bf][:, :, 0:ntok], QK[:, :, ta0:ta0 + ntok].rearrange("k p t -> p k t"),
                              w=[("qkt", bf)])
                        S.dma("sp", kt_t[bf][:, 0:nb, :], KT[ta0:ta0 + ntok, :].rearrange("(c s) d -> s c d", s=64),
                              w=[("ktt", bf)])
                        S.dma("sp", va_t[bf][:, 0:nb, :], VA[ta0:ta0 + ntok, :].rearrange("(c s) d -> s c d", s=64),
                              w=[("vat", bf)])
                        S.dma("sp", sc_t[bf][:, 0:nb, :], SC[ta0:ta0 + ntok, :].rearrange("(c s) d -> s c d", s=64),
                              w=[("sct", bf)])

                def P1(ch, q):
                    bf, ci = ch["bf"], ch["ci"]
                    tsl = slice(ci * 64, (ci + 1) * 64)
                    if ch["emit"]:
                        for h in range(NH):
                            S.op("pe", "matmul", PS(SB[q % 2])[0:64, h * 64:(h + 1) * 64], lhsT=qk_t[bf][:, 4 + h, tsl],
                                 rhs=qk_t[bf][:, h, tsl], start=True, stop=True,
                                 r=[("qkt", bf)], w=[("ps", SB[q % 2])])
                    S.op("dve", "tensor_tensor", out=ks[q % 2][:].rearrange("p (h d) -> p h d", h=NH),
                         in0=kt_t[bf][:, ci, :].rearrange("p (h d) -> p h d", h=NH),
                         in1=sc_t[bf][:, ci, d * 4:d * 4 + 4].unsqueeze(2).to_broadcast([64, NH, DK]), op=ALU.mult,
                         r=[("ktt", bf), ("sct", bf)], w=[("ks", q % 2)])

                def P2(ch, q):
                    bf, ci = ch["bf"], ch["ci"]
                    mcol = mc_ctx if ch["is_ctx"] else mc_lat
                    mask = cst[0:64, mcol:mcol + 64]
                    if ch["emit"]:
                        for h in range(NH):
                            S.op("dve", "scalar_tensor_tensor", out=pT[q % 2][:, h, :],
                                 in0=PS(SB[q % 2])[0:64, h * 64:(h + 1) * 64],
                                 scalar=sc_t[bf][:, ci, d * 4 + h:d * 4 + h + 1],
                                 op0=ALU.mult, in1=mask, op1=ALU.mult,
                                 r=[("ps", SB[q % 2]), ("sct", bf), "cst"], w=[("pT", q % 2, h)])
                    for h in range(NH):
                        pu = PS(4 + h // 2)[:, (h % 2) * 256:(h % 2) * 256 + 256]
                        S.op("pe", "matmul", pu, lhsT=ks[q % 2][:, h * DK:(h + 1) * DK],
                             rhs=va_t[bf][:, ci, h * DV:(h + 1) * DV], start=True, stop=True,
                             r=[("ks", q % 2), ("vat", bf)], w=[("ps", 4 + h // 2)])
                    for h in range(NH):
                        S.op("pe", "matmul", PS(3)[:, 8 + h:9 + h], lhsT=ks[q % 2][:, h * DK:(h + 1) * DK],
                             rhs=onesb[0:64, 0:1], start=True, stop=True, r=[("ks", q % 2), "onesb"], w=[("psn",)])

                def P3(ch, q):
                    bf, ci, chg = ch["bf"], ch["ci"], ch["chg"]
                    tsl = slice(ci * 64, (ci + 1) * 64)
                    cur, nxt = q % 2, (q + 1) % 2
                    if not ch["is_ctx"] and not st["injected"]:
                        st["injected"] = True
                        st["prev"] = "ones"
                        S.op("dve", "tensor_copy", out=Ct[:].rearrange("p h v -> p (h v)"), in_=crecv[:, 0:NH * DV],
                             r=["crecv"], w=[("Ct", h) for h in range(NH)])
                        S.op("dve", "tensor_copy", out=nst[:], in_=crecv[:, NH * DV:NST], r=["crecv"], w=["nst"])
                        S.op("act", "activation", out=Cb[cur][:].rearrange("p h v -> p (h v)"),
                             in_=crecv[:, 0:NH * DV], func=AF.Identity, r=["crecv"], w=[("Cb", cur, h) for h in range(NH)])
                        S.op("act", "activation", out=nb_[cur][:], in_=crecv[:, NH * DV:NST], func=AF.Identity,
                             r=["crecv"], w=[("nb", cur)])
                    use_ones = (st["prev"] == "ones")
                    pgl = chg if (st["prev"] is None or use_ones) else st["prev"]
                    st["prev"] = chg
                    if ch["emit"]:
                        for h in range(NH):
                            pa = PS(1 + h // 2)[0:64, (h % 2) * 256:(h % 2) * 256 + 256]
                            S.op("pe", "matmul", pa, lhsT=pT[cur][:, h, :], rhs=va_t[bf][:, ci, h * DV:(h + 1) * DV],
                                 start=True, stop=False, r=[("pT", cur, h), ("vat", bf)], w=[("ps", 1 + h // 2)])
                            S.op("pe", "matmul", pa, lhsT=qk_t[bf][:, h, tsl], rhs=Cb[cur][:, h, :],
                                 start=False, stop=True, r=[("qkt", bf), ("Cb", cur, h)], w=[("ps", 1 + h // 2)])
                        for h in range(NH):
                            pbn = PS(3)[0:64, h:h + 1]
                            S.op("pe", "matmul", pbn, lhsT=pT[cur][:, h, :], rhs=onesb[0:64, 0:1],
                                 start=True, stop=False, r=[("pT", cur, h), "onesb"], w=[("psb4",)])
                            S.op("pe", "matmul", pbn, lhsT=qk_t[bf][:, h, tsl], rhs=nb_[cur][:, h:h + 1],
                                 start=False, stop=True, r=[("qkt", bf), ("nb", cur)], w=[("psb4",)])
                    egp = ones4 if use_ones else EG[:, pgl, d * 4:d * 4 + 4]
                    egc = EG[:, chg, d * 4:d * 4 + 4]
                    for h in range(NH):
                        pu = PS(4 + h // 2)[:, (h % 2) * 256:(h % 2) * 256 + 256]
                        S.op("dve", "scalar_tensor_tensor", out=Ct[:, h, :], in0=Ct[:, h, :],
                             scalar=(cst[:, C_ONES:C_ONES + 1] if use_ones else EG[:, pgl, d * 4 + h:d * 4 + h + 1]),
                             op0=ALU.mult, in1=pu, op1=ALU.add,
                             r=[("Ct", h), "eg", ("ps", 4 + h // 2)], w=[("Ct", h)])
                        S.op("act", "activation", out=Cb[nxt][:, h, :], in_=Ct[:, h, :], func=AF.Identity,
                             scale=EG[:, chg, d * 4 + h:d * 4 + h + 1], r=[("Ct", h), "eg"], w=[("Cb", nxt, h)])
                    S.op("dve", "tensor_tensor", out=ntmp[:], in0=nst[:], in1=egp, op=ALU.mult,
                         r=["nst", "eg"], w=["ntmp"])
                    S.op("dve", "tensor_tensor", out=nst[:], in0=PS(3)[:, 8:8 + NH], in1=ntmp[:], op=ALU.add,
                         r=[("psn",), "ntmp"], w=["nst"])
                    S.op("dve", "tensor_tensor", out=nb_[nxt][:], in0=nst[:], in1=egc, op=ALU.mult,
                         r=["nst", "eg"], w=[("nb", nxt)])

                def P4(ch, q):
                    bf, ci = ch["bf"], ch["ci"]
                    if not ch["emit"]:
                        return
                    ho = hout[ch["bi"] % 2]
                    S.op("act", "activation", out=absb[:], in_=PS(3)[0:64, 0:NH], func=AF.Abs,
                         r=[("psb4",)], w=["absb"])
                    S.op("dve", "tensor_tensor", out=den[:], in0=absb[:], in1=sc_t[bf][:, ci, 8 + d * 4:12 + d * 4],
                         op=ALU.max, r=["absb", ("sct", bf)], w=["den"])
                    S.op("dve", "reciprocal", out=den[:], in_=den[:], r=["den"], w=["den"])
                    for h in range(NH):
                        pa = PS(1 + h // 2)[0:64, (h % 2) * 256:(h % 2) * 256 + 256]
                        S.op("act", "activation", out=ho[:, ci, h * DV:(h + 1) * DV], in_=pa, func=AF.Identity,
                             scale=den[:, h:h + 1], r=[("ps", 1 + h // 2), "den"], w=[("hout", ch["bi"] % 2, h)])
                    if ch["last"]:
                        ta0, ntok, nb = ch["ta0"], ch["nb"] * 64, ch["nb"]
                        S.dma("sp", HD[d][ta0:ta0 + ntok, :].rearrange("(c s) v -> s c v", s=64), ho[:, 0:nb, :],
                              r=[("hout", ch["bi"] % 2, h) for h in range(NH)])

                load_block(0)
                load_block(1)
                P1(chunks[0], 0)
                P2(chunks[0], 0)
                for q, ch in enumerate(chunks):
                    P3(ch, q)
                    if q + 1 < len(chunks):
                        P1(chunks[q + 1], q + 1)
                    P4(ch, q)
                    if ch["last"]:
                        load_block(ch["bi"] + 2)
                    if q + 1 < len(chunks):
                        P2(chunks[q + 1], q + 1)
                if d == 0:
                    csend = sb(es, "csend", [128, NST], F32)
                    lastc = st["prev"]
                    for h in range(NH):
                        S.op("act", "activation", out=csend[:, h * DV:(h + 1) * DV], in_=Ct[:, h, :], func=AF.Identity,
                             scale=EG[:, lastc, d * 4 + h:d * 4 + h + 1], r=[("Ct", h), "eg"], w=["csend"])
                    S.op("dve", "tensor_tensor", out=csend[:, NH * DV:NST], in0=nst[:],
                         in1=EG[:, lastc, d * 4:d * 4 + 4], op=ALU.mult, r=["nst", "eg"], w=["csend"])
                    S.dma("sp", csrc.ap(), csend[:], r=["csend"], w=["csrc"])
                S.flush()
            if d == 0:
                exchange(csrc, cdst, NST, lambda a: None, crecv[:], "c")

        def phase_A5(l, src, dst, do_ctx):
            j = l // 2
            with ExitStack() as es:
                xw = sb(es, "xw", [128, 8, 512], F32)
                xo = sb(es, "xo", [128, 8, 512], F32)
                hf = [sb(es, "hf%d" % i, [128, D], F32) for i in range(4)]
                hb = [sb(es, "hb%d" % i, [128, D], F32) for i in range(4)]
                so = [sb(es, "so%d" % i, [128, D], F32) for i in range(4)]
                hs = sb(es, "hs", [128, D], F32)
                junk = sb(es, "junk", [128, DV], BF16)
                ss = sb(es, "ss", [128, NH], F32)
                yb = [sb(es, "yb%d" % i, [128, D], BF16) for i in range(2)]
                gain = sb(es, "gaint", [128, D], F32)
                yT = sb(es, "yT", [128, 8, 512], BF16)
                wout = sb(es, "wout", [128, 8, D], BF16)
                S.dma("sp", gain[:], gain_in[:, j, :], w=["gain"])
                S.dma("pool", wout[:], Wi[("a_w_out", j)].rearrange("(k p) n -> p k n", p=128), w=["wout"])
                stc = [0]

                def tile(xs, xregs, n, which, ta0, outs, oregs):
                    for s in range(n // 128):
                        st = stc[0] % 4
                        sy = stc[0] % 2
                        stc[0] += 1
                        row0 = ta0 + s * 128
                        S.dma("sp", hf[st][:], HD[0][row0:row0 + 128, :], w=[("hf", st)])
                        S.dma("sp", hb[st][:], HD[1][row0:row0 + 128, :], w=[("hb", st)])
                        S.dma("sp", so[st][:], SO[row0:row0 + 128, :], w=[("so", st)])
                        S.op("pool", "tensor_tensor", out=hs[:], in0=hf[st][:], in1=hb[st][:], op=ALU.add,
                             r=[("hf", st), ("hb", st)], w=["hs"])
                        for h in range(NH):
                            S.op("act", "activation", out=junk[:], in_=hs[:, h * DV:(h + 1) * DV], func=AF.Square,
                                 accum_out=ss[:, h:h + 1], r=["hs"], w=["junk", "ss"])
                        S.op("act", "activation", out=ss[:], in_=ss[:], func=AF.Sqrt, scale=1.0 / DV, bias=EPS,
                             r=["ss"], w=["ss"])
                        S.op("dve", "reciprocal", out=ss[:], in_=ss[:], r=["ss"], w=["ss"])
                        S.op("pool", "tensor_tensor", out=so[st][:], in0=so[st][:], in1=gain[:], op=ALU.mult,
                             r=[("so", st), "gain"], w=[("so", st)])
                        for h in range(NH):
                            S.op("dve", "scalar_tensor_tensor", out=yb[sy][:, h * DV:(h + 1) * DV],
                                 in0=hs[:, h * DV:(h + 1) * DV], scalar=ss[:, h:h + 1], op0=ALU.mult,
                                 in1=so[st][:, h * DV:(h + 1) * DV], op1=ALU.mult,
                                 r=["hs", "ss", ("so", st)], w=[("yb", sy)])
                        for c in range(8):
                            S.op("pe", "transpose", psb[:, c * 128:(c + 1) * 128], yb[sy][:, c * 128:(c + 1) * 128],
                                 identb[:], r=[("yb", sy), "identb"], w=["psb"])
                        S.op("act", "activation", out=yT[:, :, s * 128:(s + 1) * 128],
                             in_=psb[:].rearrange("p (c t) -> p c t", c=8), func=AF.Identity, r=["psb"], w=["yT"])
                    for mo in range(8):
                        pt = PS(mo % 4)
                        for c in range(8):
                            S.op("pe", "matmul", pt[:, 0:n], lhsT=wout[:, c, mo * 128:(mo + 1) * 128], rhs=yT[:, c, 0:n],
                                 start=(c == 0), stop=(c == 7), r=["wout", "yT"], w=[("ps", mo % 4)])
                        resid(outs[mo], pt[:, 0:n], MODL[:, 2, mo, which:which + 1], xs[mo],
                              r=[("ps", mo % 4), "mod", xregs[mo]], w=[oregs[mo]])

                if do_ctx:
                    tile([ctx[k][:, :] for k in range(8)], [("ctx", k) for k in range(8)], TC, 1, 0,
                         [ctx[k][:, :] for k in range(8)], [("ctx", k) for k in range(8)])
                for i in range(T // 512):
                    t0 = i * 512
                    S.dma("sp", xw[:], src[:, :, t0:t0 + 512].rearrange("k p t -> p k t"), w=["xw"])
                    tile([xw[:, k, :] for k in range(8)], ["xw"] * 8, 512, 0, TC + t0,
                         [xo[:, k, :] for k in range(8)], ["xo"] * 8)
                    S.dma("sp", dst[:, :, t0:t0 + 512].rearrange("k p t -> p k t"), xo[:], r=["xo"])
                S.flush()

        def phase_final(src):
            with ExitStack() as es:
                xw = sb(es, "xw", [128, 8, 512], F32)
                xo = sb(es, "xo", [128, 8, 512], F32)
                tmp = mk_tmp(es, 512)
                og, _ = PAR_OFF["gfin"]
                for i in range(T // 512):
                    t0 = i * 512
                    S.dma("sp", xw[:], src[:, :, t0:t0 + 512].rearrange("k p t -> p k t"), w=["xw"])
                    for k in range(8):
                        sq = tmp["sq%d" % (k % 2)]
                        S.op("act", "activation", out=sq[:], in_=xw[:, k, :], func=AF.Square, r=["xw"], w=[("sq", k % 2)])
                        S.op("pe", "matmul", PS(6)[:, :], lhsT=ones_f, rhs=sq[:], start=(k == 0), stop=(k == 7),
                             r=[("sq", k % 2), "cst"], w=[("ps", 6)])
                    std = tmp["std"]
                    S.op("act", "activation", out=std[:], in_=PS(6)[:, :], func=AF.Sqrt, scale=1.0 / D, bias=EPS,
                         r=[("ps", 6)], w=["std"])
                    S.op("dve", "reciprocal", out=std[:], in_=std[:], r=["std"], w=["std"])
                    for k in range(8):
                        S.op("dve", "scalar_tensor_tensor", out=xo[:, k, :], in0=xw[:, k, :],
                             scalar=par[:, og + k:og + k + 1], op0=ALU.mult, in1=std[:], op1=ALU.mult,
                             r=["xw", "par", "std"], w=["xo"])
                    S.dma("sp", outT[:, :, t0:t0 + 512].rearrange("k p t -> p k t"), xo[:], r=["xo"])
                S.flush()

        def halo_exchange(xsrc):
            def load(a):
                S.dma("sp", a.rearrange("p (k t) -> p k t", k=8),
                      xsrc[:, :, T - GW:T].rearrange("k p t -> p k t"), w=["hsrc"])
            exchange(hsrc, hdst, 8 * GW, load, halo[:].rearrange("p k t -> p (k t)"), "h")

        cur = xT_in
        last_mod = None
        fused_final = False
        for pi_, (kind, l) in enumerate(plan):
            if last_mod != l:
                phase_mod(l)
                last_mod = l
            ctx_live = l < 2
            if kind == "A":
                phase_A1(l, cur, ctx_live)
                phase_scan(0, ctx_live)
                phase_scan(1, ctx_live)
                phase_A5(l, cur, XB, ctx_live)
                cur = XB
            elif kind == "B":
                phase_B(l, cur, XB, ctx_live)
                cur = XB
            elif kind == "F":
                need_weights(("f", l))
                halo_exchange(cur)
                if final and pi_ == len(plan) - 1:
                    phase_F(l, cur, outT, ctx_live, fin=True)
                    fused_final = True
                else:
                    phase_F(l, cur, XA, ctx_live)
                cur = XA
            elif kind == "M":
                pass
        if fused_final:
            pass
        elif final:
            phase_final(cur)
        else:
            for k in range(8):
                S.dma("sp", outT[k], cur[k])
        for k in range(8):
            S.dma("sp", ctx_out[k], ctx[k][:], r=[("ctx", k)])
        S.flush()
        S.finish("sp")
        print("sched: ins=%d waits=%d cnt=%s" % (S.n_ins, S.n_wait, S.cnt))
    return nc


FULL_PLAN = [("A", 0), ("F", 0), ("B", 1), ("F", 1), ("A", 2), ("F", 2), ("B", 3), ("F", 3)]
_NC_CACHE = {}


def pack_par(b, odd, c, c_ctx, b_mod, g_mix, g_ffn, g_final, b_w_conv, f_w_conv, f_b_conv, a_b_gate):
    par = np.zeros((128, NPAR), np.float32)

    def put(name, arr):
        o, n = PAR_OFF[name]
        a = np.asarray(arr, np.float32).reshape(128, -1)
        assert a.shape[1] == n, (name, a.shape, n)
        par[:, o:o + n] = a
    bwc = np.asarray(b_w_conv, np.float32)
    fwc = np.asarray(f_w_conv, np.float32)
    put("cc", np.stack([fm(c[b]), fm(c_ctx)], axis=-1))
    put("bmod", fm(b_mod))
    put("gmix", fm(g_mix))
    put("gffn", fm(g_ffn))
    put("gfin", fm(g_final))
    put("bwc_l", fm(bwc))
    put("bwc_c", fm(bwc[:, ::-1] if odd else bwc))
    put("fwc_l", fm(fwc[:, ::-1] if odd else fwc))
    put("fwc_c", fm(fwc[:, ::-1] if odd else fwc))
    put("fbc", fm(f_b_conv))
    put("bgate", np.broadcast_to(gate_perm(np.asarray(a_b_gate, np.float32), odd).reshape(1, 32), (128, 32)))
    put("sel", np.broadcast_to(np.array([[1.0, 0.0]] if odd else [[0.0, 1.0]], np.float32), (128, 2)))
    return par


def gate_perm(g, odd):
    if not odd:
        return g
    sh = g.shape
    return np.ascontiguousarray(g.reshape(sh[:-1] + (2, 2, NH))[..., ::-1, :].reshape(sh))


def core_tokens(x_b, odd):
    r = np.asarray(x_b, np.float32).reshape(TFULL // GW, GW, D)
    r = r[NROW:][::-1] if odd else r[:NROW]
    return np.ascontiguousarray(r.reshape(T, D).T.reshape(8, 128, T))


def make_in_maps(inputs, cores, plan=None):
    maps = []
    gain = np.ascontiguousarray(np.broadcast_to(
        np.asarray(inputs["a_head_gain"], np.float32)[None], (128, 2, D)))
    wcache = {}
    for b, odd in cores:
        cx = np.asarray(inputs["ctx"][b], np.float32)
        if odd:
            cx = cx[::-1]
        m = {
            "xT": core_tokens(inputs["x"][b], odd),
            "ctxT": np.ascontiguousarray(cx.T.reshape(8, 128, TC)),
            "par": pack_par(b, odd, inputs["c"], inputs["c_ctx"], inputs["b_mod"], inputs["g_mix"], inputs["g_ffn"],
                            inputs["g_final"], inputs["b_w_conv"], inputs["f_w_conv"], inputs["f_b_conv"],
                            inputs["a_b_gate"]),
            "consts": make_consts(odd),
            "gain": gain,
        }
        for kind, l in (plan if plan is not None else FULL_PLAN):
            m["w_mod_%d" % l] = np.ascontiguousarray(inputs["w_mod"][l], np.float32)
            if kind == "A":
                j = l // 2
                key = ("awin", j, odd)
                if key not in wcache:
                    w = np.array(inputs["a_w_in"][j], np.float32)
                    w[:, 3072:] = gate_perm(w[:, 3072:], odd)
                    wcache[key] = w
                m["a_w_in_%d" % j] = wcache[key]
                m["a_w_out_%d" % j] = np.ascontiguousarray(inputs["a_w_out"][j], np.float32)
            elif kind == "B":
                m["b_w_in_%d" % (l // 2)] = np.ascontiguousarray(inputs["b_w_in"][l // 2], np.float32)
                m["b_w_out_%d" % (l // 2)] = np.ascontiguousarray(inputs["b_w_out"][l // 2], np.float32)
            elif kind == "F":
                m["f_w_up_%d" % l] = np.ascontiguousarray(inputs["f_w_up"][l], np.float32)
                m["f_w_down_%d" % l] = np.ascontiguousarray(inputs["f_w_down"][l], np.float32)
        maps.append(m)
    return maps


def assemble(results, cores, nb):
    out = np.empty((nb, TFULL, D), np.float32)
    for res, (b, odd) in zip(results, cores):
        o = res["outT"].reshape(D, T).T.reshape(NROW, GW, D)
        if odd:
            out[b, T:] = o[::-1].reshape(T, D)
        else:
            out[b, :T] = o.reshape(T, D)
    return out


def kernel(**inputs):
    key = "full"
    if key not in _NC_CACHE:
        _NC_CACHE[key] = build(FULL_PLAN, final=True, ncores=8)
    nc = _NC_CACHE[key]
    cores = [(b, odd) for b in range(4) for odd in (0, 1)]
    in_maps = make_in_maps(inputs, cores)
    res = run_bass_kernel_spmd(nc, in_maps, core_ids=list(range(8)))
    return assemble(res.results, cores, 4)
```
